# Optimizing a Trainium2 kernel written in Bass

```python
import math
import jax, jax.numpy as jnp
from jax import lax
import numpy as np

D_MODEL = 2048
BATCH = 4
SEQ = 2048
DEPTH = 1

CHUNK = 64
Q_BLOCK = 128
HEAD_DIM = 128
SB_HEADS = 8
RET_HEADS = 8
SB_WIDTH = SB_HEADS * HEAD_DIM
RET_WIDTH = RET_HEADS * HEAD_DIM
MIX_WIDTH = SB_WIDTH + RET_WIDTH
IN_WIDTH = 3 * SB_WIDTH + 4 * RET_WIDTH
ROPE_BASE = 10000.0
N_EXPERTS = 32
TOP_K = 4
D_FF = D_MODEL
SWIGLU_LIMIT = 7.0
SWIGLU_ALPHA = 1.702
MOE_BLOCK = 128
LN_EPS = 1e-5
GN_EPS = 1e-5
DEEPNORM_ALPHA = (2 * DEPTH) ** 0.25
DEEPNORM_BETA = (8 * DEPTH) ** -0.25

kernel_name = "hybrid_stickbreak_retention_moe_deepnorm"


def layer_norm(x, gain, bias):
    xf = x.astype(jnp.float32)
    mu = jnp.mean(xf, axis=-1, keepdims=True)
    var = jnp.mean(jnp.square(xf - mu), axis=-1, keepdims=True)
    y = (xf - mu) * lax.rsqrt(var + LN_EPS)
    return (y * gain.astype(jnp.float32) + bias.astype(jnp.float32)).astype(x.dtype)


def rotary(x):
    s, d = x.shape[1], x.shape[3]
    inv_freq = ROPE_BASE ** (-jnp.arange(0, d, 2, dtype=jnp.float32) / d)
    ang = jnp.arange(s, dtype=jnp.float32)[:, None] * inv_freq[None, :]
    cos = jnp.cos(ang)[None, :, None, :]
    sin = jnp.sin(ang)[None, :, None, :]
    xf = x.astype(jnp.float32)
    x1, x2 = xf[..., : d // 2], xf[..., d // 2:]
    return jnp.concatenate([x1 * cos - x2 * sin, x1 * sin + x2 * cos], axis=-1).astype(x.dtype)


def stick_breaking_attention(q, k, v):
    b, h, s, d = q.shape
    nq = s // Q_BLOCK
    q_blocks = q.reshape(b, h, nq, Q_BLOCK, d).transpose(2, 0, 1, 3, 4)
    starts = jnp.arange(nq, dtype=jnp.int32) * Q_BLOCK
    kf = k.astype(jnp.float32)
    vf = v.astype(jnp.float32)
    key_pos = jnp.arange(s, dtype=jnp.int32)
    scale = 1.0 / math.sqrt(d)

    def one_block(args):
        q_blk, start = args
        z = jnp.einsum('bhqd,bhkd->bhqk', q_blk.astype(jnp.float32), kf) * scale
        q_pos = start + jnp.arange(Q_BLOCK, dtype=jnp.int32)
        mask = key_pos[None, :] < q_pos[:, None]
        log_not_beta = jnp.where(mask, jax.nn.log_sigmoid(-z), 0.0)
        later = lax.cumsum(log_not_beta, axis=3, reverse=True) - log_not_beta
        weights = jnp.where(mask, jnp.exp(jax.nn.log_sigmoid(z) + later), 0.0)
        return jnp.einsum('bhqk,bhkd->bhqd', weights, vf)

    o = lax.map(one_block, (q_blocks, starts))
    return o.transpose(1, 0, 3, 2, 4).reshape(b, s, h * d).astype(q.dtype)


def chunk_retention(q, k, v, gn_gain):
    b, s, h, d = q.shape
    n = s // CHUNK
    log_gamma = jnp.log1p(-jnp.exp2(-5.0 - jnp.arange(h, dtype=jnp.float32)))
    to_chunks = lambda a: a.astype(jnp.float32).reshape(b, n, CHUNK, h, d).transpose(0, 3, 1, 2, 4)
    qf = to_chunks(q)
    kf = to_chunks(k) * (d ** -0.5)
    vf = to_chunks(v)
    i = jnp.arange(CHUNK, dtype=jnp.float32)
    intra_decay = jnp.exp(log_gamma[:, None, None] * jnp.abs(i[:, None] - i[None, :]))
    scores = jnp.einsum('bhncd,bhnjd->bhncj', qf, kf) * intra_decay[:, None]
    o_intra = jnp.einsum('bhncj,bhnje->bhnce', scores, vf)
    k_decay = jnp.exp(log_gamma[:, None] * (CHUNK - 1 - i))
    chunk_kv = jnp.einsum('bhnjd,bhnje->nbhde', kf * k_decay[:, None, :, None], vf)
    chunk_decay = jnp.exp(log_gamma * CHUNK)[None, :, None, None]

    def step(state, kv):
        return state * chunk_decay + kv, state

    _, prev_states = lax.scan(step, jnp.zeros((b, h, d, d), jnp.float32), chunk_kv)
    q_decay = jnp.exp(log_gamma[:, None] * (i + 1.0))
    o_cross = jnp.einsum('bhncd,nbhde->bhnce', qf * q_decay[:, None, :, None], prev_states)
    o = o_intra + o_cross
    mu = jnp.mean(o, axis=-1, keepdims=True)
    var = jnp.mean(jnp.square(o - mu), axis=-1, keepdims=True)
    o = (o - mu) * lax.rsqrt(var + GN_EPS)
    o = o.transpose(0, 2, 3, 1, 4).reshape(b, s, h * d) * gn_gain.astype(jnp.float32)
    return o.astype(q.dtype)


def clamped_swiglu(hid):
    gate, up = hid[..., :D_FF], hid[..., D_FF:]
    gate = jnp.minimum(gate, SWIGLU_LIMIT)
    up = jnp.clip(up, -SWIGLU_LIMIT, SWIGLU_LIMIT)
    return (up + 1.0) * (gate * jax.nn.sigmoid(SWIGLU_ALPHA * gate))


def moe_ffn(x, w_router, b_router, w_gate_up, b_gate_up, w_down, b_down):
    b, s, dm = x.shape
    t = b * s
    xt = x.reshape(t, dm)
    logits = (xt @ w_router + b_router).astype(jnp.float32)
    top_vals, top_idx = lax.top_k(logits, TOP_K)
    gates = jax.nn.softmax(top_vals, axis=-1)
    tk = t * TOP_K
    flat_e = top_idx.reshape(tk).astype(jnp.int32)
    flat_tok = jnp.arange(tk, dtype=jnp.int32) // TOP_K
    order = jnp.argsort(flat_e)
    sorted_e = flat_e[order]
    sorted_tok = flat_tok[order]
    counts = jnp.bincount(flat_e, length=N_EXPERTS).astype(jnp.int32)
    padded = (counts + MOE_BLOCK - 1) // MOE_BLOCK * MOE_BLOCK
    un_start = jnp.cumsum(counts) - counts
    pad_end = jnp.cumsum(padded)
    pad_start = pad_end - padded
    dest = pad_start[sorted_e] + jnp.arange(tk, dtype=jnp.int32) - un_start[sorted_e]
    n_rows = (tk + MOE_BLOCK - 1) // MOE_BLOCK * MOE_BLOCK + N_EXPERTS * MOE_BLOCK
    n_blocks = n_rows // MOE_BLOCK
    row_tok = jnp.full((n_rows,), t, jnp.int32).at[dest].set(sorted_tok)
    x_pad = jnp.concatenate([xt, jnp.zeros((1, dm), xt.dtype)], axis=0)
    x_rows = x_pad[row_tok].reshape(n_blocks, MOE_BLOCK, dm)
    block_e = jnp.minimum(
        jnp.searchsorted(pad_end, jnp.arange(n_blocks, dtype=jnp.int32) * MOE_BLOCK, side='right'),
        N_EXPERTS - 1)

    def expert_block(args):
        xb, e = args
        hid = xb @ w_gate_up[e] + b_gate_up[e]
        return clamped_swiglu(hid) @ w_down[e] + b_down[e]

    y_rows = lax.map(expert_block, (x_rows, block_e)).reshape(n_rows, dm)
    dest_tok = jnp.zeros((tk,), jnp.int32).at[order].set(dest).reshape(t, TOP_K)
    y = jnp.einsum('tk,tkd->td', gates.astype(y_rows.dtype), y_rows[dest_tok])
    return y.reshape(b, s, dm)


def setup_inputs(seed: int = 0) -> dict:
    key = jax.random.key(seed)
    ks = jax.random.split(key, 16)
    f32 = jnp.float32
    nrm = lambda k, shape: jax.random.normal(k, shape, f32)
    return {
        "x": nrm(ks[0], (BATCH, SEQ, D_MODEL)),
        "w_in": nrm(ks[1], (DEPTH, D_MODEL, IN_WIDTH)) * D_MODEL ** -0.5,
        "ret_gn_gain": 1.0 + 0.02 * nrm(ks[2], (DEPTH, RET_WIDTH)),
        "w_out": nrm(ks[3], (DEPTH, MIX_WIDTH, D_MODEL)) * (MIX_WIDTH ** -0.5 * DEEPNORM_BETA),
        "ln1_gain": 1.0 + 0.02 * nrm(ks[4], (DEPTH, D_MODEL)),
        "ln1_bias": 0.02 * nrm(ks[5], (DEPTH, D_MODEL)),
        "w_router": nrm(ks[6], (DEPTH, D_MODEL, N_EXPERTS)) * D_MODEL ** -0.5,
        "b_router": 0.01 * nrm(ks[7], (DEPTH, N_EXPERTS)),
        "w_gate_up": nrm(ks[8], (DEPTH, N_EXPERTS, D_MODEL, 2 * D_FF)) * D_MODEL ** -0.5,
        "b_gate_up": 0.01 * nrm(ks[9], (DEPTH, N_EXPERTS, 2 * D_FF)),
        "w_down": nrm(ks[10], (DEPTH, N_EXPERTS, D_FF, D_MODEL)) * (D_FF ** -0.5 * DEEPNORM_BETA),
        "b_down": 0.01 * nrm(ks[11], (DEPTH, N_EXPERTS, D_MODEL)),
        "ln2_gain": 1.0 + 0.02 * nrm(ks[12], (DEPTH, D_MODEL)),
        "ln2_bias": 0.02 * nrm(ks[13], (DEPTH, D_MODEL)),
    }


def reference(x, w_in, ret_gn_gain, w_out, ln1_gain, ln1_bias, w_router, b_router,
              w_gate_up, b_gate_up, w_down, b_down, ln2_gain, ln2_bias):
    b, s, _ = x.shape
    splits = [SB_WIDTH, 2 * SB_WIDTH, 3 * SB_WIDTH,
              3 * SB_WIDTH + RET_WIDTH, 3 * SB_WIDTH + 2 * RET_WIDTH, 3 * SB_WIDTH + 3 * RET_WIDTH]
    for layer in range(DEPTH):
        proj = x @ w_in[layer]
        sb_q, sb_k, sb_v, r_q, r_k, r_v, r_g = jnp.split(proj, splits, axis=-1)
        sb_heads = lambda a: a.reshape(b, s, SB_HEADS, HEAD_DIM).transpose(0, 2, 1, 3)
        sb_out = stick_breaking_attention(sb_heads(sb_q), sb_heads(sb_k), sb_heads(sb_v))
        ret_heads = lambda a: a.reshape(b, s, RET_HEADS, HEAD_DIM)
        ret_out = chunk_retention(rotary(ret_heads(r_q)), rotary(ret_heads(r_k)), ret_heads(r_v),
                                  ret_gn_gain[layer]) * jax.nn.silu(r_g)
        mix = jnp.concatenate([sb_out.astype(x.dtype), ret_out.astype(x.dtype)], axis=-1) @ w_out[layer]
        x = layer_norm(DEEPNORM_ALPHA * x + mix, ln1_gain[layer], ln1_bias[layer])
        ffn = moe_ffn(x, w_router[layer], b_router[layer], w_gate_up[layer], b_gate_up[layer],
                      w_down[layer], b_down[layer])
        x = layer_norm(DEEPNORM_ALPHA * x + ffn.astype(x.dtype), ln2_gain[layer], ln2_bias[layer])
    return x
```

```python
import math
from contextlib import ExitStack

import numpy as np
import concourse.bass as bass
import concourse.mybir as mybir
from concourse.bass_utils import run_bass_kernel_spmd

F32 = mybir.dt.float32
BF16 = mybir.dt.bfloat16
AF = mybir.ActivationFunctionType
ALU = mybir.AluOpType

D = 2048
SEQ = 2048
NB = 4
HD = 128
NH = 8
NE = 32
CAP = 256
ALPHA = 2.0 ** 0.25
LN_EPS = 1e-5
GN_EPS = 1e-5
QSCALE = 1.0 / math.sqrt(HD)
WEXT = 9216
EPOCH = 2000

C_SBQ, C_SBK, C_SBV, C_RQX, C_RKX, C_RV, C_RG = 0, 1024, 2048, 3072, 5120, 7168, 8192


class Sched:
    ENGS = ("pe", "act", "dve", "pool", "sp")

    def __init__(self, nc):
        self.nc = nc
        self.ops = {e: [] for e in self.ENGS}
        self.count = {e: 0 for e in self.ENGS}
        self.known = {e: {} for e in self.ENGS}
        self.last_w = {}
        self.readers = {}
        self.dma_slots = {}
        self.n_dma_sems = 0

    def _need(self, eng, key, val, waits):
        if key == ("eng", "pe") and eng == "pe":
            return
        if self.known[eng].get(key, 0) >= val:
            return
        self.known[eng][key] = val
        waits[key] = max(waits.get(key, 0), val)

    def _deps(self, eng, reads, writes):
        waits = {}
        for r in reads:
            t = self.last_w.get(r)
            if t is not None:
                self._need(eng, t[0], t[1], waits)
        for w in writes:
            t = self.last_w.get(w)
            if t is not None:
                self._need(eng, t[0], t[1], waits)
            for k, v in self.readers.get(w, {}).items():
                self._need(eng, k, v, waits)
        return list(waits.items())

    def _commit(self, tok, reads, writes):
        for r in reads:
            d = self.readers.setdefault(r, {})
            d[tok[0]] = max(d.get(tok[0], 0), tok[1])
        for w in writes:
            self.last_w[w] = tok
            self.readers[w] = {}

    def op(self, eng, method, reads=(), writes=(), **kw):
        fn = (method, kw)
        waits = self._deps(eng, reads, writes)
        idx = self.count[eng]
        self.count[eng] += 1
        tok = (("eng", eng), idx + 1)
        self.ops[eng].append((waits, fn, ("eng", idx)))
        self._commit(tok, reads, writes)

    def dma(self, eng, slot, reads=(), writes=(), **kw):
        fn = ("dma_start", kw)
        waits = self._deps(eng, reads, writes)
        if slot not in self.dma_slots:
            self.dma_slots[slot] = [self.n_dma_sems, 0]
            self.n_dma_sems += 1
        self.dma_slots[slot][1] += 16
        tok = (("dma", slot), self.dma_slots[slot][1])
        self.ops[eng].append((waits, fn, ("dma", slot)))
        self._commit(tok, reads, writes)

    def wait_all(self, eng, regions):
        waits = self._deps(eng, regions, ())
        self.ops[eng].append((waits, None, None))

    def barrier(self):
        toks = [(("eng", e), self.count[e]) for e in self.ENGS if self.count[e] > 0]
        toks += [(("dma", s), v[1]) for s, v in self.dma_slots.items()]
        for e in self.ENGS:
            waits = {}
            for k, v in toks:
                self._need(e, k, v, waits)
            self.ops[e].append((list(waits.items()), None, None))
        self.last_w.clear()
        self.readers.clear()

    def emit(self):
        nc = self.nc
        with ExitStack() as st:
            eng_sems = {}
            for e in self.ENGS:
                n_ep = self.count[e] // EPOCH + 1
                eng_sems[e] = [st.enter_context(nc.semaphore(f"s_{e}_{i}")) for i in range(n_ep)]
            dma_sems = [st.enter_context(nc.semaphore(f"s_dma_{i}")) for i in range(self.n_dma_sems)]
            block = st.enter_context(nc.Block())

            def run(ename):
                def body(eng):
                    for waits, fn, inc in self.ops[ename]:
                        for key, val in waits:
                            if key[0] == "eng":
                                idx = val - 1
                                eng.wait_ge(eng_sems[key[1]][idx // EPOCH], idx % EPOCH + 1)
                            else:
                                eng.wait_ge(dma_sems[self.dma_slots[key[1]][0]], val)
                        if fn is None:
                            continue
                        ins = getattr(eng, fn[0])(**fn[1])
                        if inc[0] == "eng":
                            idx = inc[1]
                            ins.then_inc(eng_sems[ename][idx // EPOCH], 1)
                        else:
                            ins.then_inc(dma_sems[self.dma_slots[inc[1]][0]], 16)
                return body

            block.tensor(run("pe"))
            block.scalar(run("act"))
            block.vector(run("dve"))
            block.gpsimd(run("pool"))
            block.sync(run("sp"))


class Arena:
    def __init__(self, t, words):
        self.t = t
        self.words = words
        self.off = 0

    def mark(self):
        return self.off

    def release(self, m):
        self.off = m

    def _take(self, nwords):
        a = self.off
        self.off += nwords
        assert self.off <= self.words, f"SBUF arena overflow {self.off} > {self.words}"
        return self.t[:, a:a + nwords]

    def f32(self, *dims):
        n = int(np.prod(dims))
        v = self._take(n)
        return self._shape(v, dims)

    def bf16(self, *dims):
        n = int(np.prod(dims))
        assert n % 2 == 0
        v = self._take(n // 2).bitcast(BF16)
        return self._shape(v, dims)

    @staticmethod
    def _shape(v, dims):
        if len(dims) == 1:
            return v
        if len(dims) == 2:
            return v.rearrange("p (a b) -> p a b", a=dims[0])
        if len(dims) == 3:
            return v.rearrange("p (a b c) -> p a b c", a=dims[0], b=dims[1])
        raise ValueError(dims)


def build_program(stage="full", ne_run=NE):
    nc = bass.Bass("TRN2", target_bir_lowering=False)
    dt = nc.dram_tensor
    with_moe = stage in ("full", "router")
    xT3 = dt("xT3", [3, D, 1024], F32, kind="ExternalInput").ap()
    x_own = dt("x_own", [1024, D], F32, kind="ExternalInput").ap()
    w_ext = dt("w_ext", [D, WEXT], F32, kind="ExternalInput").ap()
    w_out = dt("w_out", [D, D], F32, kind="ExternalInput").ap()
    vecs = dt("vecs", [5, D], F32, kind="ExternalInput").ap()
    cst = dt("cst", [128, 1024], F32, kind="ExternalInput").ap()
    rmask = dt("rmask", [128, 3 * NH * 128], F32, kind="ExternalInput").ap()
    rot = dt("rot", [3, 2, 128, 1024], F32, kind="ExternalInput").ap()
    if with_moe:
        w_router = dt("w_router", [D, NE], F32, kind="ExternalInput").ap()
        b_router = dt("b_router", [1, NE], F32, kind="ExternalInput").ap()
        b_gu = dt("b_gu", [NE, 128, 32], F32, kind="ExternalInput").ap()
        if stage == "full":
            w_gu = dt("w_gu", [ne_run, D, 2 * D], F32, kind="ExternalInput").ap()
            w_dn = dt("w_dn", [ne_run, D, D], F32, kind="ExternalInput").ap()
        b_dn = dt("b_dn", [NE, D], F32, kind="ExternalInput").ap()
    y = dt("y", [1024, D], F32, kind="ExternalOutput").ap()

    AW = 53000
    with ExitStack() as st:
        arena_t = st.enter_context(nc.sbuf_tensor("arena", [128, AW], F32))
        ps = st.enter_context(nc.psum_tensor("ps", [128, 4096], F32))
        A = Arena(arena_t, AW)
        S = Sched(nc)
        OP = S.op

        def bank(b, n=512, off=0):
            return ps[:, b * 512 + off: b * 512 + off + n]

        def mm(out, lhsT, rhs, start, stop, reads, writes):
            OP("pe", "matmul", reads, writes, out=out, lhsT=lhsT, rhs=rhs, start=start, stop=stop)

        evac_rr = [0]

        def evac_copy(dst, src, reads, writes, eng=None):
            if eng is None:
                eng = ("act", "dve")[evac_rr[0] % 2]
                evac_rr[0] += 1
            if eng == "act":
                OP("act", "activation", reads, writes, out=dst, in_=src, func=AF.Copy)
            else:
                OP("dve", "tensor_copy", reads, writes, out=dst, in_=src)

        cst_f = A.f32(1024)
        ident_f = cst_f[:, 0:128]
        maskA = cst_f[:, 512:640]
        maskB = cst_f[:, 640:768]
        iota_f = cst_f[:, 768:1024]
        cst_b = A.bf16(4, 128)
        ident_b, triU_b, triL_b, ones_b = (cst_b[:, i, :] for i in range(4))
        S.dma("sp", "cst", [], ["cst"], out=cst_f, in_=cst)
        OP("dve", "tensor_copy", ["cst"], ["cstb"], out=cst_b,
           in_=cst_f[:, 0:512].rearrange("p (a b) -> p a b", a=4))
        mix_raw = A._take(8192)
        mixT = mix_raw.bitcast(BF16).rearrange("p (a b) -> p a b", a=16)
        mix_hi_f32 = mix_raw[:, 4096:8192]
        m_phaseA = A.mark()

        wp_n = [0]

        def load_piece(wbufs, src_ap, tag):
            i = wp_n[0] % len(wbufs)
            wp_n[0] += 1
            key = (tag, i)
            S.dma("pool", f"{tag}{i}", [], [key], out=wbufs[i], in_=src_ap.rearrange("(k q) c -> q k c", q=128))
            return wbufs[i], key

        def load_xT(xT, p):
            S.dma("pool", "xT", [], ["xT"], out=xT, in_=xT3[p].rearrange("(k q) t -> q k t", q=128))

        pbank = [0]

        def next_banks(n):
            b = pbank[0]
            pbank[0] = (pbank[0] + n) % 4
            return [b + i for i in range(n)]

        def proj_feature_major(wbuf, wkey, col, xT, dst_fn, dkey_fn):
            bA, bB = next_banks(2)
            for k in range(16):
                for hf, b in ((0, bA), (1, bB)):
                    mm(bank(b), wbuf[:, k, col:col + 128], xT[:, k, hf * 512:(hf + 1) * 512],
                       k == 0, k == 15, [wkey, "xT"], [("ps", b)])
            for hf, b in ((0, bA), (1, bB)):
                evac_copy(dst_fn(hf), bank(b), [("ps", b)], [dkey_fn(hf)])

        def proj_token_major(wbuf, wkey, xT, tb, consume):
            (b,) = next_banks(1)
            for k in range(16):
                mm(bank(b), xT[:, k, tb * 128:(tb + 1) * 128], wbuf[:, k, :], k == 0, k == 15,
                   [wkey, "xT"], [("ps", b)])
            consume(b)

        KT = A.bf16(NH, SEQ)
        V = A.bf16(16, 1024)
        QT = A.bf16(NH, 1024)
        m_sbattn = A.mark()
        xT = A.bf16(16, 1024)
        wbufs = [A.bf16(16, 512) for _ in range(3)]

        for p in range(3):
            load_xT(xT, p)
            if p < 2:
                for pc in range(2):
                    wb, wk = load_piece(wbufs, w_ext[:, C_SBK + pc * 512: C_SBK + (pc + 1) * 512], "wp")
                    for hh in range(4):
                        h = pc * 4 + hh
                        proj_feature_major(
                            wb, wk, hh * 128, xT,
                            lambda hf, h=h, p=p: KT[:, h, p * 1024 + hf * 512: p * 1024 + (hf + 1) * 512],
                            lambda hf, h=h, p=p: ("KT", h, p * 2 + hf))
                for pc in range(2):
                    wb, wk = load_piece(wbufs, w_ext[:, C_SBV + pc * 512: C_SBV + (pc + 1) * 512], "wp")
                    for tb in range(8):
                        blk = p * 8 + tb
                        proj_token_major(
                            wb, wk, xT, tb,
                            lambda b, blk=blk, pc=pc: evac_copy(V[:, blk, pc * 512:(pc + 1) * 512], bank(b),
                                                                [("ps", b)], [("V", blk, pc)]))
            else:
                for pc in range(2):
                    wb, wk = load_piece(wbufs, w_ext[:, C_SBQ + pc * 512: C_SBQ + (pc + 1) * 512], "wp")
                    for hh in range(4):
                        h = pc * 4 + hh
                        proj_feature_major(
                            wb, wk, hh * 128, xT,
                            lambda hf, h=h: QT[:, h, hf * 512:(hf + 1) * 512],
                            lambda hf, h=h: ("QT", h, hf))

        S.barrier()
        A.release(m_sbattn)
        e_buf = [A.f32(512) for _ in range(2)]
        sp_buf = [A.bf16(512) for _ in range(2)]
        t1_buf = [A.f32(512) for _ in range(2)]
        w_buf = [A.bf16(512) for _ in range(2)]
        acc_buf = [[A.bf16(512) for _ in range(3)] for _ in range(2)]
        iters = []
        for h in range(NH):
            for q in range(2):
                u = h * 2 + q
                jmax = 8 * q + 7
                for j in range(jmax, -1, -1):
                    iters.append((u, h, q, j, jmax))

        def sb_info(q, j):
            r0 = max(0, j // 2 - 4 * q)
            ms = j // 2 - 4 * q
            return r0, (ms if ms >= 0 else None)

        def sb_s1(n):
            u, h, q, j, jmax = iters[n]
            r0, ms = sb_info(q, j)
            lo = r0 * 128
            zb = n % 2
            pb = n % 2
            z = bank(zb)[:, lo:512]
            mm(z, KT[:, h, j * 128:(j + 1) * 128], QT[:, h, q * 512 + lo: (q + 1) * 512], True, True,
               [("KT", h, j // 4), ("QT", h, q)], [("ps", zb)])
            eb, spb, t1b = e_buf[pb][:, lo:512], sp_buf[pb][:, lo:512], t1_buf[pb][:, lo:512]
            OP("act", "activation", [("ps", zb)], [("e", pb)], out=eb, in_=z, func=AF.Exp, scale=QSCALE)
            OP("act", "activation", [("e", pb)], [("sp", pb)], out=spb, in_=eb, func=AF.Ln, bias=1.0, scale=1.0)
            if ms is not None:
                mk = maskA if j % 2 == 0 else maskB
                spm = sp_buf[pb][:, ms * 128:(ms + 1) * 128]
                OP("dve", "tensor_tensor", [("sp", pb), "cst"], [("sp", pb)], out=spm, in0=spm, in1=mk, op=ALU.mult)
            OP("dve", "scalar_tensor_tensor", [("ps", zb), ("sp", pb)], [("t1", pb)],
               out=t1b, in0=z, scalar=QSCALE, in1=spb, op0=ALU.mult, op1=ALU.subtract)
            ua = acc_buf[u % 2]
            k = jmax - j
            if j == jmax:
                for i3 in range(3):
                    OP("pool", "memset", [], [("acc", u % 2, i3)], ap=ua[i3], constant=0.0)
            if j > 0:
                src, dst = ua[k % 3], ua[(k + 1) % 3]
                OP("pool", "tensor_tensor", [("acc", u % 2, k % 3), ("sp", pb)], [("acc", u % 2, (k + 1) % 3)],
                   out=dst[:, lo:512], in0=src[:, lo:512], in1=spb, op=ALU.add)

        def sb_s2(n):
            u, h, q, j, jmax = iters[n]
            r0, ms = sb_info(q, j)
            lo = r0 * 128
            pb = n % 2
            Pb = 2 + n % 2
            k = jmax - j
            P = bank(Pb)[:, lo:512]
            spb, t1b, wb = sp_buf[pb][:, lo:512], t1_buf[pb][:, lo:512], w_buf[pb][:, lo:512]
            mm(P, triU_b, spb, True, False, [("sp", pb), "cstb"], [("ps", Pb)])
            mm(P, ones_b, acc_buf[u % 2][k % 3][:, lo:512], False, True, [("acc", u % 2, k % 3), "cstb"], [("ps", Pb)])
            OP("dve", "tensor_tensor", [("t1", pb), ("ps", Pb)], [("t1", pb)], out=t1b, in0=t1b, in1=P, op=ALU.subtract)
            OP("act", "activation", [("t1", pb)], [("w", pb)], out=wb, in_=t1b, func=AF.Exp)
            if ms is not None:
                mk = maskA if j % 2 == 0 else maskB
                wm = w_buf[pb][:, ms * 128:(ms + 1) * 128]
                OP("dve", "tensor_tensor", [("w", pb), "cst"], [("w", pb)], out=wm, in0=wm, in1=mk, op=ALU.mult)

        def sb_s3(n):
            u, h, q, j, jmax = iters[n]
            r0, ms = sb_info(q, j)
            pb = n % 2
            Ob = 4 + u % 2
            vv = V[:, j, h * 128:(h + 1) * 128]
            vkey = ("V", j, h // 4)
            mm(bank(Ob)[:, r0 * 128:512], vv, w_buf[pb][:, r0 * 128:512], j == jmax, j == 0, [vkey, ("w", pb)],
               [("ps", Ob)])
            if j == 0:
                evac_copy(mixT[:, h, q * 512:(q + 1) * 512], bank(Ob), [("ps", Ob)], [("mixT", h, q)])

        NIT = len(iters)
        import os as _os
        _nit = int(_os.environ.get("SB_NIT", NIT))
        if _os.environ.get("SB_PIPE", "1") == "1":
            for step in range(_nit + 2):
                if step < _nit:
                    sb_s1(step)
                if 0 <= step - 1 < _nit:
                    sb_s2(step - 1)
                if 0 <= step - 2 < _nit:
                    sb_s3(step - 2)
        else:
            for step in range(_nit):
                sb_s1(step)
                sb_s2(step)
                sb_s3(step)

        if stage == "sb":
            return _finish_debug(nc, S, A, y, mixT, 8)

        S.barrier()
        A.release(m_phaseA)
        rKT = A.bf16(NH, SEQ)
        rV = A.bf16(16, 1024)
        rQT = A.bf16(NH, 1024)
        G = A.bf16(8, 1024)
        m_retattn = A.mark()
        xT = A.bf16(16, 1024)
        wbufs = [A.bf16(16, 512) for _ in range(2)]
        tab = mix_hi_f32[:, 0:2048].rearrange("p (a b) -> p a b", a=2)
        tA = [mix_hi_f32[:, 2048 + i * 512: 2048 + (i + 1) * 512] for i in range(2)]
        tB = [mix_hi_f32[:, 3072 + i * 512: 3072 + (i + 1) * 512] for i in range(2)]
        rr = [0]

        def rot_proj(wb, wk, hh, xT, dst_fn, dkey_fn):
            for hf in range(2):
                bA, bB = next_banks(2)
                for k in range(16):
                    mm(bank(bA), wb[:, k, hh * 128:(hh + 1) * 128], xT[:, k, hf * 512:(hf + 1) * 512],
                       k == 0, k == 15, [wk, "xT"], [("ps", bA)])
                for k in range(16):
                    mm(bank(bB), wb[:, k, 256 + hh * 128: 256 + (hh + 1) * 128], xT[:, k, hf * 512:(hf + 1) * 512],
                       k == 0, k == 15, [wk, "xT"], [("ps", bB)])
                i = rr[0] % 2
                rr[0] += 1
                cs, sn = tab[:, 0, hf * 512:(hf + 1) * 512], tab[:, 1, hf * 512:(hf + 1) * 512]
                OP("dve", "tensor_tensor", [("ps", bA), "tab"], [("tA", i)], out=tA[i], in0=bank(bA), in1=cs, op=ALU.mult)
                OP("dve", "tensor_tensor", [("ps", bB), "tab"], [("tB", i)], out=tB[i], in0=bank(bB), in1=sn, op=ALU.mult)
                OP("pool", "tensor_tensor", [("tA", i), ("tB", i)], [dkey_fn(hf)], out=dst_fn(hf), in0=tA[i], in1=tB[i],
                   op=ALU.add)

        for p in range(3):
            load_xT(xT, p)
            S.dma("sp", "tab", [], ["tab"], out=tab, in_=rot[p].rearrange("c d t -> d c t"))
            if p < 2:
                for pc in range(4):
                    wb, wk = load_piece(wbufs, w_ext[:, C_RKX + pc * 512: C_RKX + (pc + 1) * 512], "wr")
                    for hh in range(2):
                        h = pc * 2 + hh
                        rot_proj(wb, wk, hh, xT,
                                 lambda hf, h=h, p=p: rKT[:, h, p * 1024 + hf * 512: p * 1024 + (hf + 1) * 512],
                                 lambda hf, h=h, p=p: ("rKT", h, p * 2 + hf))
                for pc in range(2):
                    wb, wk = load_piece(wbufs, w_ext[:, C_RV + pc * 512: C_RV + (pc + 1) * 512], "wr")
                    for tb in range(8):
                        blk = p * 8 + tb
                        proj_token_major(
                            wb, wk, xT, tb,
                            lambda b, blk=blk, pc=pc: evac_copy(rV[:, blk, pc * 512:(pc + 1) * 512], bank(b),
                                                                [("ps", b)], [("rV", blk, pc)]))
            else:
                for pc in range(4):
                    wb, wk = load_piece(wbufs, w_ext[:, C_RQX + pc * 512: C_RQX + (pc + 1) * 512], "wr")
                    for hh in range(2):
                        h = pc * 2 + hh
                        rot_proj(wb, wk, hh, xT,
                                 lambda hf, h=h: rQT[:, h, hf * 512:(hf + 1) * 512],
                                 lambda hf, h=h: ("rQT", h, hf))
                for pc in range(2):
                    wb, wk = load_piece(wbufs, w_ext[:, C_RG + pc * 512: C_RG + (pc + 1) * 512], "wr")
                    for tb in range(8):
                        def cons(b, tb=tb, pc=pc):
                            OP("act", "activation", [("ps", b)], [("G", tb, pc)],
                               out=G[:, tb, pc * 512:(pc + 1) * 512], in_=bank(b), func=AF.Silu)
                        proj_token_major(wb, wk, xT, tb, cons)

        S.barrier()
        A.release(m_retattn)
        rm = A.f32(3, NH, 128)
        S.dma("sp", "rm", [], ["rm"], out=rm, in_=rmask.rearrange("p (a h c) -> p a h c", a=3, h=NH))
        gg = A.f32(1024)
        S.dma("sp", "gg", [], ["gg"], out=gg, in_=vecs[4, 0:1024].partition_broadcast(128))
        sc_buf = [A.bf16(512) for _ in range(2)]
        st6 = A.f32(4, 6)
        mv = A.f32(4, 2)
        rstd = A.f32(4)
        nmr = A.f32(4)
        nrm = [A.f32(128) for _ in range(2)]
        nrm2 = [A.f32(128) for _ in range(2)]
        rout = [A.bf16(128) for _ in range(2)]
        gam = [1.0 - 2.0 ** (-5.0 - h) for h in range(NH)]
        psT = ps[:, 4 * 512: 5 * 512].bitcast(BF16)

        def ret_s1(n):
            u, h, q, j, jmax = iters[n]
            r0, ms = sb_info(q, j)
            lo = r0 * 128
            sb_ = n % 2
            sT = bank(sb_)
            mm(sT[:, lo:512], rKT[:, h, j * 128:(j + 1) * 128], rQT[:, h, q * 512 + lo:(q + 1) * 512], True, True,
               [("rKT", h, j // 4), ("rQT", h, q)], [("ps", sb_)])
            for r in range(r0, 4):
                i_slot = 4 * q + r
                dstc = sc_buf[sb_][:, r * 128:(r + 1) * 128]
                src = sT[:, r * 128:(r + 1) * 128]
                if ms is not None and r == ms:
                    mk = rm[:, 1 if j % 2 == 0 else 2, h, :]
                    OP("dve", "tensor_tensor", [("ps", sb_), "rm"], [("sc", sb_)], out=dstc, in0=src, in1=mk, op=ALU.mult)
                else:
                    fac = gam[h] ** (128.0 * (2 * i_slot - j))
                    mk = rm[:, 0, h, :]
                    OP("dve", "scalar_tensor_tensor", [("ps", sb_), "rm"], [("sc", sb_)],
                       out=dstc, in0=src, scalar=fac, in1=mk, op0=ALU.mult, op1=ALU.mult)

        def ret_s2(n):
            u, h, q, j, jmax = iters[n]
            r0, ms = sb_info(q, j)
            sb_ = n % 2
            Ob = 2 + u % 2
            for r in range(r0, 4):
                first = (j == jmax)
                mm(bank(Ob)[:, r * 128:(r + 1) * 128], sc_buf[sb_][:, r * 128:(r + 1) * 128],
                   rV[:, j, h * 128:(h + 1) * 128], first, j == 0, [("sc", sb_), ("rV", j, h // 4)], [("ps", Ob)])
            if j == 0:
                for r in range(4):
                    i_slot = 4 * q + r
                    o = bank(Ob)[:, r * 128:(r + 1) * 128]
                    k2 = (u * 4 + r) % 2
                    OP("dve", "bn_stats", [("ps", Ob)], [("st6", r)], out=st6[:, r, :], in_=o)
                    OP("dve", "bn_aggr", [("st6", r)], [("mv", r)], out=mv[:, r, :], in_=st6[:, r, :])
                    OP("act", "activation", [("mv", r)], [("rstd", r)], out=rstd[:, r:r + 1], in_=mv[:, r, 1:2],
                       func=AF.Ln, bias=GN_EPS, scale=1.0)
                    OP("act", "activation", [("rstd", r)], [("rstd", r)], out=rstd[:, r:r + 1], in_=rstd[:, r:r + 1],
                       func=AF.Exp, scale=-0.5)
                    OP("dve", "scalar_tensor_tensor", [("mv", r), ("rstd", r)], [("nmr", r)], out=nmr[:, r:r + 1],
                       in0=mv[:, r, 0:1], scalar=-1.0, in1=rstd[:, r:r + 1], op0=ALU.mult, op1=ALU.mult)
                    OP("act", "activation", [("ps", Ob), ("nmr", r), ("rstd", r)], [("nrm", k2)], out=nrm[k2], in_=o,
                       func=AF.Identity, bias=nmr[:, r:r + 1], scale=rstd[:, r:r + 1])
                    OP("pool", "tensor_tensor", [("nrm", k2), "gg"], [("nrm2", k2)], out=nrm2[k2], in0=nrm[k2],
                       in1=gg[:, h * 128:(h + 1) * 128], op=ALU.mult)
                    OP("pool", "tensor_tensor", [("nrm2", k2), ("G", i_slot, h // 4)], [("rout", k2)], out=rout[k2],
                       in0=nrm2[k2], in1=G[:, i_slot, h * 128:(h + 1) * 128], op=ALU.mult)
                    OP("pe", "transpose", [("rout", k2), "cstb"], [("psT",)], out=psT[:, r * 128:(r + 1) * 128],
                       in_=rout[k2], identity=ident_b)
                evac_copy(mixT[:, 8 + h, q * 512:(q + 1) * 512], psT[:, 0:512], [("psT",)], [("mixT", 8 + h, q)])

        for step in range(NIT + 1):
            if step < NIT:
                ret_s1(step)
            if 0 <= step - 1 < NIT:
                ret_s2(step - 1)

        if stage == "attn":
            return _finish_debug(nc, S, A, y, mixT, 16)

        S.barrier()
        A.release(m_phaseA)
        yacc = A.f32(8, D)
        x1b = A.bf16(8, D)
        m_moe = A.mark()
        wob = [A.bf16(16, 512) for _ in range(2)]
        lng = A.f32(2, D)
        S.dma("sp", "lng", [], ["lng"], out=lng[:, 0, :], in_=vecs[0, :].partition_broadcast(128))
        S.dma("sp", "lng", [], ["lng"], out=lng[:, 1, :], in_=vecs[1, :].partition_broadcast(128))
        S.dma("sp", "xown", [], [("h1", s_) for s_ in range(8)], out=yacc, in_=x_own.rearrange("(s p) f -> p s f", p=128))
        for cp in range(4):
            wb, wk = load_piece(wob, w_out[:, cp * 512:(cp + 1) * 512], "wo")
            for s_ in range(8):
                (b,) = next_banks(1)
                for k in range(16):
                    mm(bank(b), mixT[:, k, s_ * 128:(s_ + 1) * 128], wb[:, k, :], k == 0, k == 15,
                       [wk, ("mixT", k, s_ // 4)], [("ps", b)])
                dst = yacc[:, s_, cp * 512:(cp + 1) * 512]
                OP("dve", "scalar_tensor_tensor", [("ps", b), ("h1", s_)], [("h1", s_)],
                   out=dst, in0=dst, scalar=ALPHA, in1=bank(b), op0=ALU.mult, op1=ALU.add)

        lst = A.f32(8, 24)
        lmv = A.f32(8, 2)
        lrs = A.f32(8)
        lnm = A.f32(8)

        def layer_norm(s_, lng, lst, lmv, lrs, lnm):
            xs = yacc[:, s_, :]
            for c in range(4):
                OP("dve", "bn_stats", [("h1", s_)], [("lst", s_)], out=lst[:, s_, c * 6:(c + 1) * 6],
                   in_=xs[:, c * 512:(c + 1) * 512])
            OP("dve", "bn_aggr", [("lst", s_)], [("lmv", s_)], out=lmv[:, s_, :], in_=lst[:, s_, :])
            OP("act", "activation", [("lmv", s_)], [("lrs", s_)], out=lrs[:, s_:s_ + 1], in_=lmv[:, s_, 1:2],
               func=AF.Ln, bias=LN_EPS, scale=1.0)
            OP("act", "activation", [("lrs", s_)], [("lrs", s_)], out=lrs[:, s_:s_ + 1], in_=lrs[:, s_:s_ + 1],
               func=AF.Exp, scale=-0.5)
            OP("dve", "scalar_tensor_tensor", [("lmv", s_), ("lrs", s_)], [("lnm", s_)], out=lnm[:, s_:s_ + 1],
               in0=lmv[:, s_, 0:1], scalar=-1.0, in1=lrs[:, s_:s_ + 1], op0=ALU.mult, op1=ALU.mult)
            OP("act", "activation", [("h1", s_), ("lnm", s_), ("lrs", s_)], [("h1", s_)], out=xs, in_=xs,
               func=AF.Identity, bias=lnm[:, s_:s_ + 1], scale=lrs[:, s_:s_ + 1])
            OP("pool", "tensor_tensor", [("h1", s_), "lng"], [("h1", s_)], out=xs, in0=xs, in1=lng[:, 0, :], op=ALU.mult)
            OP("pool", "tensor_tensor", [("h1", s_), "lng"], [("h1", s_)], out=xs, in0=xs, in1=lng[:, 1, :], op=ALU.add)

        for s_ in range(8):
            layer_norm(s_, lng, lst, lmv, lrs, lnm)
            OP("act", "activation", [("h1", s_)], [("x1b", s_)], out=x1b[:, s_, :], in_=yacc[:, s_, :], func=AF.Copy)

        if stage == "x1":
            S.dma("sp", "yout", [("h1", s_) for s_ in range(8)], ["y"], out=y.rearrange("(s p) f -> p s f", p=128),
                  in_=yacc)
            S.wait_all("sp", ["y"])
            S.emit()
            return nc

        S.barrier()
        A.release(m_moe)
        gates = A.f32(8, NE)
        posm = A.f32(8, NE)
        m_moe2 = A.mark()
        wr = A.f32(16, NE)
        br = A.f32(NE)
        onesrow = A.f32(128)
        bdn = A.f32(D)
        x1T = A.f32(16, 128)
        lg = A.f32(NE)
        mx8 = A.f32(8)
        msk = A.f32(8, NE)
        mskb = A.bf16(8, NE)
        msum = A.f32(NE)
        msumb = A.bf16(NE)
        ex = A.f32(NE)
        ssum = A.f32(1)
        nmx = A.f32(1)
        gT = A.f32(128)
        S.dma("sp", "wr", [], ["wr"], out=wr, in_=w_router.rearrange("(k q) n -> q k n", q=128))
        S.dma("sp", "br", [], ["br"], out=br[0:1, :], in_=b_router)
        S.dma("sp", "bdn", [], ["bdn"], out=bdn[0:NE, :], in_=b_dn)
        OP("dve", "memset", [], ["onesrow"], ap=onesrow[0:1, :], constant=1.0)
        OP("dve", "memset", [], ["msum"], ap=msum, constant=0.0)
        for s_ in range(8):
            for g4 in range(4):
                for c in range(4):
                    k = g4 * 4 + c
                    OP("pe", "transpose", [("h1", s_), "cst"], [("ps", g4)], out=bank(g4)[:, c * 128:(c + 1) * 128],
                       in_=yacc[:, s_, k * 128:(k + 1) * 128], identity=ident_f)
                evac_copy(x1T[:, g4 * 4:(g4 + 1) * 4, :], bank(g4).rearrange("p (a b) -> p a b", a=4),
                          [("ps", g4)], [("x1T", g4)])
            lgp = bank(4)[:, 0:NE]
            for k in range(16):
                mm(lgp, x1T[:, k, :], wr[:, k, :], k == 0, False, [("x1T", k // 4), "wr"], [("ps", 4)])
            mm(lgp, onesrow[0:1, :], br[0:1, :], False, True, ["onesrow", "br"], [("ps", 4)])
            OP("dve", "tensor_copy", [("ps", 4)], ["lg"], out=lg, in_=lgp)
            OP("dve", "max", ["lg"], ["mx8"], out=mx8, in_=lg)
            OP("dve", "tensor_scalar", ["lg", "mx8"], [("msk", s_)], out=msk[:, s_, :], in0=lg, scalar1=mx8[:, 3:4],
               scalar2=None, op0=ALU.is_ge)
            OP("dve", "tensor_scalar", ["mx8"], ["nmx"], out=nmx, in0=mx8[:, 0:1], scalar1=-1.0, scalar2=None, op0=ALU.mult)
            OP("act", "activation", ["lg", "nmx"], ["ex"], out=ex, in_=lg, func=AF.Exp, bias=nmx, scale=1.0)
            OP("dve", "tensor_tensor", ["ex", ("msk", s_)], ["ex"], out=ex, in0=ex, in1=msk[:, s_, :], op=ALU.mult)
            OP("dve", "reduce_sum", ["ex"], ["ssum"], out=ssum, in_=ex, axis=mybir.AxisListType.X)
            OP("dve", "reciprocal", ["ssum"], ["ssum"], out=ssum, in_=ssum)
            OP("dve", "tensor_scalar", ["ex", "ssum"], [("gates", s_)], out=gates[:, s_, :], in0=ex, scalar1=ssum[:, 0:1],
               scalar2=None, op0=ALU.mult)
            OP("dve", "tensor_copy", [("msk", s_)], [("mskb", s_)], out=mskb[:, s_, :], in_=msk[:, s_, :])
            OP("dve", "tensor_copy", ["msum"], ["msumb"], out=msumb, in_=msum)
            pp = bank(5)[:, 0:NE]
            mm(pp, triL_b, mskb[:, s_, :], True, False, [("mskb", s_), "cstb"], [("ps", 5)])
            mm(pp, ones_b, msumb, False, True, ["msumb", "cstb"], [("ps", 5)])
            OP("dve", "scalar_tensor_tensor", [("ps", 5), ("msk", s_)], [("posm", s_)], out=posm[:, s_, :], in0=pp,
               scalar=1.0, in1=msk[:, s_, :], op0=ALU.add, op1=ALU.mult)
            OP("dve", "tensor_scalar", [("posm", s_)], [("posm", s_)], out=posm[:, s_, :], in0=posm[:, s_, :],
               scalar1=-1.0, scalar2=None, op0=ALU.add)
            OP("dve", "tensor_tensor", ["msum", ("msk", s_)], ["msum"], out=msum, in0=msum, in1=msk[:, s_, :], op=ALU.add)
            OP("pe", "transpose", [("gates", s_), "cst"], [("ps", 6)], out=bank(6)[0:NE, 0:128], in_=gates[:, s_, :],
               identity=ident_f)
            OP("act", "activation", [("ps", 6)], ["gT"], out=gT[0:NE, :], in_=bank(6)[0:NE, 0:128], func=AF.Copy)
            for cp in range(4):
                bb = bank(7)
                mm(bb, gT[0:NE, :], bdn[0:NE, cp * 512:(cp + 1) * 512], True, True, ["gT", "bdn"], [("ps", 7)])
                dst = yacc[:, s_, cp * 512:(cp + 1) * 512]
                OP("dve", "scalar_tensor_tensor", [("ps", 7), ("h1", s_)], [("h1", s_)], out=dst, in0=dst, scalar=ALPHA,
                   in1=bb, op0=ALU.mult, op1=ALU.add)

        if stage == "router":
            S.barrier()
            yv = y.rearrange("(s p) f -> p s f", p=128)
            S.dma("sp", "yout", [], ["y"], out=yv[:, :, 0:NE], in_=gates)
            S.dma("sp", "yout", [], ["y"], out=yv[:, :, NE:2 * NE], in_=posm)
            S.dma("sp", "yout", [], ["y"], out=yv[:, :, 64:64 + 1024], in_=yacc[:, :, 0:1024])
            S.wait_all("sp", ["y"])
            S.emit()
            return nc

        S.barrier()
        A.release(m_moe2)
        wmb = [mix_raw[:, i * 4096:(i + 1) * 4096].bitcast(BF16).rearrange("p (a b) -> p a b", a=16) for i in range(2)]
        wmb.append(A.bf16(16, 512))
        sel = [A.bf16(8, CAP) for _ in range(2)]
        selT = [A.bf16(2, 1024) for _ in range(2)]
        XeT = [A.bf16(16, CAP) for _ in range(2)]
        actT = A.bf16(16, CAP)
        gsb = A.f32(4, CAP)
        utmp = [A.f32(CAP)] * 2
        gtmp = [A.f32(CAP)] * 2
        yrows = A.bf16(2, D)
        bgu = [A.f32(32) for _ in range(2)]
        psT7 = ps[:, 7 * 512: 8 * 512].bitcast(BF16)

        hb_n = [0]
        sc_n = [0]
        for ex_ in range(ne_run):
            eb = ex_ % 2
            S.dma("sp", f"bgu{eb}", [], [("bgu", eb)], out=bgu[eb], in_=b_gu[ex_])
            for s_ in range(8):
                eng = "dve" if s_ % 2 == 0 else "pool"
                OP(eng, "tensor_scalar", [("posm", s_), "cst"], [("sel", eb, s_)], out=sel[eb][:, s_, :], in0=iota_f,
                   scalar1=posm[:, s_, ex_:ex_ + 1], scalar2=None, op0=ALU.is_equal)
            for rc in range(2):
                for s_ in range(8):
                    OP("pe", "transpose", [("sel", eb, s_), "cstb"], [("ps", 7)], out=psT7[:, s_ * 128:(s_ + 1) * 128],
                       in_=sel[eb][:, s_, rc * 128:(rc + 1) * 128], identity=ident_b)
                evac_copy(selT[eb][:, rc, :], psT7, [("ps", 7)], [("selT", eb, rc)])
            for c in range(16):
                gb = (0, 7)[c % 2]
                for s_ in range(8):
                    mm(bank(gb, CAP), x1b[:, s_, c * 128:(c + 1) * 128], sel[eb][:, s_, :], s_ == 0, s_ == 7,
                       [("x1b", s_), ("sel", eb, s_)], [("ps", gb)])
                evac_copy(XeT[eb][:, c, :], bank(gb, CAP), [("ps", gb)], [("XeT", eb, c)])
            for p4 in range(4):
                for gu in range(2):
                    col0 = gu * D + p4 * 512
                    wb, wk = load_piece(wmb, w_gu[ex_][:, col0:col0 + 512], "wm")
                    for cc in range(4):
                        hbi = hb_n[0] % 2
                        hb_n[0] += 1
                        hbk = ("ps", 1 + hbi)
                        hp_ = bank(1 + hbi, CAP)
                        for k in range(16):
                            mm(hp_, wb[:, k, cc * 128:(cc + 1) * 128], XeT[eb][:, k, :], k == 0, k == 15,
                               [wk, ("XeT", eb, k)], [hbk])
                        fch = gu * 16 + p4 * 4 + cc
                        bcol = bgu[eb][:, fch:fch + 1]
                        ti = 0
                        if gu == 0:
                            gt = gtmp[ti]
                            OP("dve", "tensor_scalar", [hbk, ("bgu", eb)], [("gtmp", ti)], out=gt, in0=hp_, scalar1=bcol,
                               scalar2=7.0, op0=ALU.add, op1=ALU.min)
                            OP("act", "activation", [("gtmp", ti)], [("gsb", cc)], out=gsb[:, cc, :], in_=gt,
                               func=AF.Sigmoid, scale=1.702)
                            OP("pool", "tensor_tensor", [("gtmp", ti), ("gsb", cc)], [("gsb", cc)], out=gsb[:, cc, :],
                               in0=gsb[:, cc, :], in1=gt, op=ALU.mult)
                        else:
                            ut = utmp[ti]
                            OP("dve", "tensor_scalar", [hbk, ("bgu", eb)], [("utmp", ti)], out=ut, in0=hp_, scalar1=bcol,
                               scalar2=7.0, op0=ALU.add, op1=ALU.min)
                            OP("dve", "tensor_scalar", [("utmp", ti)], [("utmp", ti)], out=ut, in0=ut, scalar1=-7.0,
                               scalar2=1.0, op0=ALU.max, op1=ALU.add)
                            ach = p4 * 4 + cc
                            OP("pool", "tensor_tensor", [("utmp", ti), ("gsb", cc)], [("actT", ach)], out=actT[:, ach, :],
                               in0=ut, in1=gsb[:, cc, :], op=ALU.mult)
            for cp in range(4):
                wb, wk = load_piece(wmb, w_dn[ex_][:, cp * 512:(cp + 1) * 512], "wm")
                for rc in range(2):
                    yb = 3 + rc
                    for k in range(16):
                        mm(bank(yb), actT[:, k, rc * 128:(rc + 1) * 128], wb[:, k, :], k == 0, k == 15,
                           [wk, ("actT", k)], [("ps", yb)])
                    evac_copy(yrows[:, rc, cp * 512:(cp + 1) * 512], bank(yb), [("ps", yb)], [("yrows", rc, cp)])
                for s_ in range(8):
                    sb_ = 5 + sc_n[0] % 2
                    sc_n[0] += 1
                    for rc in range(2):
                        mm(bank(sb_), selT[eb][:, rc, s_ * 128:(s_ + 1) * 128], yrows[:, rc, cp * 512:(cp + 1) * 512],
                           rc == 0, rc == 1, [("selT", eb, rc), ("yrows", rc, cp)], [("ps", sb_)])
                    dst = yacc[:, s_, cp * 512:(cp + 1) * 512]
                    OP("dve", "scalar_tensor_tensor", [("ps", sb_), ("gates", s_), ("h1", s_, cp)], [("h1", s_, cp)],
                       out=dst, in0=bank(sb_), scalar=gates[:, s_, ex_:ex_ + 1], in1=dst, op0=ALU.mult, op1=ALU.add)

        S.barrier()
        A.release(m_moe2)
        lng2 = A.f32(2, D)
        lst2 = A.f32(8, 24)
        lmv2 = A.f32(8, 2)
        lrs2 = A.f32(8)
        lnm2 = A.f32(8)
        S.dma("sp", "lng2", [], ["lng"], out=lng2[:, 0, :], in_=vecs[2, :].partition_broadcast(128))
        S.dma("sp", "lng2", [], ["lng"], out=lng2[:, 1, :], in_=vecs[3, :].partition_broadcast(128))
        yv = y.rearrange("(s p) f -> p s f", p=128)
        for s_ in range(8):
            layer_norm(s_, lng2, lst2, lmv2, lrs2, lnm2)
            S.dma("sp", "yout", [("h1", s_)], [("y", s_)], out=yv[:, s_, :], in_=yacc[:, s_, :])
        S.wait_all("sp", [("y", s_) for s_ in range(8)])
        S.emit()
    return nc


def _finish_debug(nc, S, A, y, mixT, nchunks):
    S.barrier()
    tmp = [A.f32(1024) for _ in range(2)]
    yv = y.rearrange("r (two t) -> (r two) t", two=2).rearrange("(c p) t -> p c t", p=128)
    for c in range(nchunks):
        S.op("dve", "tensor_copy", [], [("tmp", c % 2)], out=tmp[c % 2], in_=mixT[:, c, :])
        S.dma("sp", "dbg", [("tmp", c % 2)], ["ydbg"], out=yv[:, c, :], in_=tmp[c % 2])
    S.wait_all("sp", ["ydbg"])
    S.emit()
    return nc


def _constants(hp):
    p = np.arange(128)
    ident = np.eye(128, dtype=np.float32)
    triU = (p[:, None] > p[None, :]).astype(np.float32)
    triL = (p[:, None] < p[None, :]).astype(np.float32)
    ones = np.ones((128, 128), np.float32)
    strict = (p[:, None] < p[None, :]).astype(np.float32)
    if hp == 0:
        mA, mB = strict, np.zeros((128, 128), np.float32)
    else:
        mA, mB = ones, strict
    iota = np.broadcast_to(np.arange(256, dtype=np.float32)[None, :], (128, 256))
    cst = np.concatenate([ident, triU, triL, ones, mA, mB, iota], axis=1).astype(np.float32)
    sl = p[:, None].astype(np.float64)
    tl = p[None, :].astype(np.float64)
    rm = np.zeros((128, 3, NH, 128), np.float64)
    for h in range(NH):
        lg = math.log1p(-2.0 ** (-5.0 - h))
        gen = np.exp(lg * (128.0 * hp + tl - sl))
        diag = np.where((p[:, None] // 64) <= (p[None, :] // 64), np.exp(lg * np.abs(tl - sl)), 0.0)
        rm[:, 0, h, :] = gen
        if hp == 0:
            rm[:, 1, h, :] = diag
            rm[:, 2, h, :] = 0.0
        else:
            rm[:, 1, h, :] = np.exp(lg * (128.0 + tl - sl))
            rm[:, 2, h, :] = diag
    rm *= HD ** -0.5
    return cst, rm.reshape(128, 3 * NH * 128).astype(np.float32)


def _rot_tables(hp):
    inv = 10000.0 ** (-np.arange(0, HD, 2, dtype=np.float32) / HD)
    own = np.concatenate([np.arange(128) + (2 * i + hp) * 128 for i in range(8)])
    out = np.zeros((3, 2, 128, 1024), np.float32)
    for pi, pos in enumerate([np.arange(0, 1024), np.arange(1024, 2048), own]):
        ang = pos.astype(np.float32)[None, :] * inv[:, None]
        c, s = np.cos(ang), np.sin(ang)
        out[pi, 0] = np.concatenate([c, c], axis=0)
        out[pi, 1] = np.concatenate([-s, s], axis=0)
    return out


def _w_ext(w_in):
    def perm(a):
        return a.reshape(D, NH, 2, 64)[:, :, ::-1, :].reshape(D, 1024)

    def interleave(a, ap):
        a4 = a.reshape(D, 4, 256)
        p4 = ap.reshape(D, 4, 256)
        return np.concatenate([a4, p4], axis=2).reshape(D, 2048)

    sbq, sbk, sbv = w_in[:, 0:1024], w_in[:, 1024:2048], w_in[:, 2048:3072]
    rq, rk, rv, rg = w_in[:, 3072:4096], w_in[:, 4096:5120], w_in[:, 5120:6144], w_in[:, 6144:7168]
    return np.ascontiguousarray(np.concatenate(
        [sbq, sbk, sbv, interleave(rq, perm(rq)), interleave(rk, perm(rk)), rv, rg], axis=1))


_PROG = {}


def _get_prog(stage="full", ne_run=NE):
    if (stage, ne_run) not in _PROG:
        _PROG[(stage, ne_run)] = build_program(stage, ne_run)
    return _PROG[(stage, ne_run)]


def make_in_maps(x, w_in, ret_gn_gain, w_out, ln1_gain, ln1_bias, w_router, b_router,
                 w_gate_up, b_gate_up, w_down, b_down, ln2_gain, ln2_bias, stage="full", ne_run=NE):
    f = lambda a: np.ascontiguousarray(np.asarray(a, dtype=np.float32))
    x = f(x)
    wext = _w_ext(f(w_in)[0])
    vecs = np.zeros((5, D), np.float32)
    vecs[0], vecs[1], vecs[2], vecs[3] = f(ln1_gain)[0], f(ln1_bias)[0], f(ln2_gain)[0], f(ln2_bias)[0]
    vecs[4, :1024] = f(ret_gn_gain)[0]
    shared = {"w_ext": wext, "w_out": f(w_out)[0], "vecs": vecs}
    if stage in ("full", "router"):
        shared.update({
            "w_router": f(w_router)[0], "b_router": f(b_router)[0].reshape(1, NE), "b_dn": f(b_down)[0],
            "b_gu": np.ascontiguousarray(f(b_gate_up)[0].reshape(NE, 32, 128).transpose(0, 2, 1)),
        })
    if stage == "full":
        shared.update({"w_gu": f(w_gate_up)[0][:ne_run], "w_dn": f(w_down)[0][:ne_run]})
    consts = [(_constants(hp), _rot_tables(hp)) for hp in range(2)]
    in_maps = []
    for c in range(8):
        b, hp = c // 2, c % 2
        xb = x[b]
        own = np.concatenate([np.arange(128) + (2 * i + hp) * 128 for i in range(8)])
        xo = np.ascontiguousarray(xb[own])
        xT3 = np.stack([np.ascontiguousarray(xb[0:1024].T), np.ascontiguousarray(xb[1024:2048].T),
                        np.ascontiguousarray(xo.T)])
        (cst, rm), rot = consts[hp]
        m = dict(shared)
        m.update({"xT3": xT3, "x_own": xo, "cst": cst, "rmask": rm, "rot": rot})
        in_maps.append(m)
    return in_maps


def kernel(x, w_in, ret_gn_gain, w_out, ln1_gain, ln1_bias, w_router, b_router,
           w_gate_up, b_gate_up, w_down, b_down, ln2_gain, ln2_bias):
    in_maps = make_in_maps(x, w_in, ret_gn_gain, w_out, ln1_gain, ln1_bias, w_router, b_router,
                           w_gate_up, b_gate_up, w_down, b_down, ln2_gain, ln2_bias)
    nc = _get_prog("full")
    res = run_bass_kernel_spmd(nc, in_maps, core_ids=list(range(8)))
    out = np.zeros((NB, SEQ, D), np.float32)
    for c in range(8):
        b, hp = c // 2, c % 2
        yc = res.results[c]["y"]
        for i in range(8):
            blk = 2 * i + hp
            out[b, blk * 128:(blk + 1) * 128] = yc[i * 128:(i + 1) * 128]
    return out
```

```python
import math
from contextlib import ExitStack

import numpy as np
import concourse.bass as bass
import concourse.mybir as mybir
from concourse.bass_utils import run_bass_kernel_spmd

F32 = mybir.dt.float32
BF16 = mybir.dt.bfloat16
AF = mybir.ActivationFunctionType
ALU = mybir.AluOpType

D = 2048
SEQ = 2048
NB = 4
HD = 128
NH = 8
NE = 32
CAP = 192
RCH = ((0, 128), (128, 64))
ALPHA = 2.0 ** 0.25
LN_EPS = 1e-5
GN_EPS = 1e-5
QSCALE = 1.0 / math.sqrt(HD)
WEXT = 7168
EPOCH = 2000

C_SBQ, C_SBK, C_SBV, C_RQX, C_RKX, C_RV, C_RG = 0, 1024, 2048, 3072, 4096, 5120, 6144


class Sched:
    ENGS = ("pe", "act", "dve", "pool", "sp")

    def __init__(self, nc):
        self.nc = nc
        self.ops = {e: [] for e in self.ENGS}
        self.count = {e: 0 for e in self.ENGS}
        self.known = {e: {} for e in self.ENGS}
        self.last_w = {}
        self.readers = {}
        self.dma_slots = {}
        self.n_dma_sems = 0

    def _need(self, eng, key, val, waits):
        if key == ("eng", "pe") and eng == "pe":
            return
        if self.known[eng].get(key, 0) >= val:
            return
        self.known[eng][key] = val
        waits[key] = max(waits.get(key, 0), val)

    def _deps(self, eng, reads, writes):
        waits = {}
        for r in reads:
            t = self.last_w.get(r)
            if t is not None:
                self._need(eng, t[0], t[1], waits)
        for w in writes:
            t = self.last_w.get(w)
            if t is not None:
                self._need(eng, t[0], t[1], waits)
            for k, v in self.readers.get(w, {}).items():
                self._need(eng, k, v, waits)
        return list(waits.items())

    def _commit(self, tok, reads, writes):
        for r in reads:
            d = self.readers.setdefault(r, {})
            d[tok[0]] = max(d.get(tok[0], 0), tok[1])
        for w in writes:
            self.last_w[w] = tok
            self.readers[w] = {}

    def op(self, eng, method, reads=(), writes=(), **kw):
        fn = (method, kw)
        waits = self._deps(eng, reads, writes)
        idx = self.count[eng]
        self.count[eng] += 1
        tok = (("eng", eng), idx + 1)
        self.ops[eng].append((waits, fn, ("eng", idx)))
        self._commit(tok, reads, writes)

    def dma(self, eng, slot, reads=(), writes=(), **kw):
        fn = ("dma_start", kw)
        waits = self._deps(eng, reads, writes)
        if slot not in self.dma_slots:
            self.dma_slots[slot] = [self.n_dma_sems, 0]
            self.n_dma_sems += 1
        self.dma_slots[slot][1] += 16
        tok = (("dma", slot), self.dma_slots[slot][1])
        self.ops[eng].append((waits, fn, ("dma", slot)))
        self._commit(tok, reads, writes)

    def wait_all(self, eng, regions):
        waits = self._deps(eng, regions, ())
        self.ops[eng].append((waits, None, None))

    def barrier(self):
        toks = [(("eng", e), self.count[e]) for e in self.ENGS if self.count[e] > 0]
        toks += [(("dma", s), v[1]) for s, v in self.dma_slots.items()]
        for e in self.ENGS:
            waits = {}
            for k, v in toks:
                self._need(e, k, v, waits)
            self.ops[e].append((list(waits.items()), None, None))
        self.last_w.clear()
        self.readers.clear()

    def emit(self):
        nc = self.nc
        with ExitStack() as st:
            eng_sems = {}
            for e in self.ENGS:
                n_ep = self.count[e] // EPOCH + 1
                eng_sems[e] = [st.enter_context(nc.semaphore(f"s_{e}_{i}")) for i in range(n_ep)]
            dma_sems = [st.enter_context(nc.semaphore(f"s_dma_{i}")) for i in range(self.n_dma_sems)]
            block = st.enter_context(nc.Block())

            def run(ename):
                def body(eng):
                    for waits, fn, inc in self.ops[ename]:
                        for key, val in waits:
                            if key[0] == "eng":
                                idx = val - 1
                                eng.wait_ge(eng_sems[key[1]][idx // EPOCH], idx % EPOCH + 1)
                            else:
                                eng.wait_ge(dma_sems[self.dma_slots[key[1]][0]], val)
                        if fn is None:
                            continue
                        ins = getattr(eng, fn[0])(**fn[1])
                        if inc[0] == "eng":
                            idx = inc[1]
                            ins.then_inc(eng_sems[ename][idx // EPOCH], 1)
                        else:
                            ins.then_inc(dma_sems[self.dma_slots[inc[1]][0]], 16)
                return body

            block.tensor(run("pe"))
            block.scalar(run("act"))
            block.vector(run("dve"))
            block.gpsimd(run("pool"))
            block.sync(run("sp"))


class Arena:
    def __init__(self, t, words):
        self.t = t
        self.words = words
        self.off = 0

    def mark(self):
        return self.off

    def release(self, m):
        self.off = m

    def _take(self, nwords):
        a = self.off
        self.off += nwords
        assert self.off <= self.words, f"SBUF arena overflow {self.off} > {self.words}"
        return self.t[:, a:a + nwords]

    def f32(self, *dims):
        n = int(np.prod(dims))
        v = self._take(n)
        return self._shape(v, dims)

    def bf16(self, *dims):
        n = int(np.prod(dims))
        assert n % 2 == 0
        v = self._take(n // 2).bitcast(BF16)
        return self._shape(v, dims)

    @staticmethod
    def _shape(v, dims):
        if len(dims) == 1:
            return v
        if len(dims) == 2:
            return v.rearrange("p (a b) -> p a b", a=dims[0])
        if len(dims) == 3:
            return v.rearrange("p (a b c) -> p a b c", a=dims[0], b=dims[1])
        raise ValueError(dims)


def build_program(stage="full", ne_run=NE):
    nc = bass.Bass("TRN2", target_bir_lowering=False)
    dt = nc.dram_tensor
    with_moe = stage in ("full", "router")
    xT3 = dt("xT3", [3, D, 1024], F32, kind="ExternalInput").ap()
    x_own = dt("x_own", [1024, D], F32, kind="ExternalInput").ap()
    w_ext = dt("w_ext", [D, WEXT], F32, kind="ExternalInput").ap()
    w_out = dt("w_out", [D, D], F32, kind="ExternalInput").ap()
    vecs = dt("vecs", [5, D], F32, kind="ExternalInput").ap()
    cst = dt("cst", [128, 1024], F32, kind="ExternalInput").ap()
    rmask = dt("rmask", [128, 3 * NH * 128], F32, kind="ExternalInput").ap()
    rot = dt("rot", [3, 2, 128, 1024], F32, kind="ExternalInput").ap()
    if with_moe:
        w_router = dt("w_router", [D, NE], F32, kind="ExternalInput").ap()
        b_router = dt("b_router", [1, NE], F32, kind="ExternalInput").ap()
        b_gu = dt("b_gu", [NE, 128, 32], F32, kind="ExternalInput").ap()
        if stage == "full":
            w_gu = dt("w_gu", [ne_run, D, 2 * D], F32, kind="ExternalInput").ap()
            w_dn = dt("w_dn", [ne_run, D, D], F32, kind="ExternalInput").ap()
        b_dn = dt("b_dn", [NE, D], F32, kind="ExternalInput").ap()
    y = dt("y", [1024, D], F32, kind="ExternalOutput").ap()

    AW = 53000
    with ExitStack() as st:
        arena_t = st.enter_context(nc.sbuf_tensor("arena", [128, AW], F32))
        ps = st.enter_context(nc.psum_tensor("ps", [128, 4096], F32))
        A = Arena(arena_t, AW)
        S = Sched(nc)
        OP = S.op

        def bank(b, n=512, off=0):
            return ps[:, b * 512 + off: b * 512 + off + n]

        def mm(out, lhsT, rhs, start, stop, reads, writes):
            OP("pe", "matmul", reads, writes, out=out, lhsT=lhsT, rhs=rhs, start=start, stop=stop)

        evac_rr = [0]

        def evac_copy(dst, src, reads, writes, eng=None):
            if eng is None:
                eng = ("act", "dve")[evac_rr[0] % 2]
                evac_rr[0] += 1
            if eng == "act":
                OP("act", "activation", reads, writes, out=dst, in_=src, func=AF.Copy)
            else:
                OP("dve", "tensor_copy", reads, writes, out=dst, in_=src)

        cst_f = A.f32(1024)
        ident_f = cst_f[:, 0:128]
        maskA = cst_f[:, 512:640]
        maskB = cst_f[:, 640:768]
        iota_f = cst_f[:, 768:1024]
        cst_b = A.bf16(4, 128)
        ident_b, triU_b, triL_b, ones_b = (cst_b[:, i, :] for i in range(4))
        S.dma("sp", "cst", [], ["cst"], out=cst_f, in_=cst)
        OP("dve", "tensor_copy", ["cst"], ["cstb"], out=cst_b,
           in_=cst_f[:, 0:512].rearrange("p (a b) -> p a b", a=4))
        swp_b = A.bf16(128)
        OP("dve", "tensor_copy", ["cstb"], ["swp"], out=swp_b[:, 0:64], in_=ident_b[:, 64:128])
        OP("dve", "tensor_copy", ["cstb"], ["swp"], out=swp_b[:, 64:128], in_=ident_b[:, 0:64])
        mb_b = A.bf16(2, 128)
        OP("dve", "tensor_scalar", ["cst"], ["mbb"], out=mb_b, in0=cst_f[:, 512:768].rearrange("p (a b) -> p a b", a=2),
           scalar1=-1.0, scalar2=1.0e4, op0=ALU.add, op1=ALU.mult)
        mix_raw = A._take(8192)
        mixT = mix_raw.bitcast(BF16).rearrange("p (a b) -> p a b", a=16)
        mix_hi_f32 = mix_raw[:, 4096:8192]
        m_phaseA = A.mark()

        wp_n = [0]

        def load_piece(wbufs, src_ap, tag):
            i = wp_n[0] % len(wbufs)
            wp_n[0] += 1
            key = (tag, i)
            S.dma("pool", f"{tag}{i}", [], [key], out=wbufs[i], in_=src_ap.rearrange("(k q) c -> q k c", q=128))
            return wbufs[i], key

        def load_xT(xT, p):
            S.dma("pool", "xT", [], ["xT"], out=xT, in_=xT3[p].rearrange("(k q) t -> q k t", q=128))

        pbank = [0]

        def next_banks(n):
            b = pbank[0]
            pbank[0] = (pbank[0] + n) % 4
            return [b + i for i in range(n)]

        def proj_feature_major(wbuf, wkey, col, xT, dst_fn, dkey_fn):
            bA, bB = next_banks(2)
            for k in range(16):
                for hf, b in ((0, bA), (1, bB)):
                    mm(bank(b), wbuf[:, k, col:col + 128], xT[:, k, hf * 512:(hf + 1) * 512],
                       k == 0, k == 15, [wkey, "xT"], [("ps", b)])
            for hf, b in ((0, bA), (1, bB)):
                evac_copy(dst_fn(hf), bank(b), [("ps", b)], [dkey_fn(hf)])

        def proj_token_major(wbuf, wkey, xT, tb, consume):
            (b,) = next_banks(1)
            for k in range(16):
                mm(bank(b), xT[:, k, tb * 128:(tb + 1) * 128], wbuf[:, k, :], k == 0, k == 15,
                   [wkey, "xT"], [("ps", b)])
            consume(b)

        KT = A.bf16(NH, SEQ)
        V = A.bf16(16, 1024)
        QT = A.bf16(NH, 1024)
        m_sbattn = A.mark()
        xT = A.bf16(16, 1024)
        wbufs = [A.bf16(16, 512) for _ in range(3)]

        for p in range(3):
            load_xT(xT, p)
            if p < 2:
                for pc in range(2):
                    wb, wk = load_piece(wbufs, w_ext[:, C_SBK + pc * 512: C_SBK + (pc + 1) * 512], "wp")
                    for hh in range(4):
                        h = pc * 4 + hh
                        proj_feature_major(
                            wb, wk, hh * 128, xT,
                            lambda hf, h=h, p=p: KT[:, h, p * 1024 + hf * 512: p * 1024 + (hf + 1) * 512],
                            lambda hf, h=h, p=p: ("KT", h, p * 2 + hf))
                for pc in range(2):
                    wb, wk = load_piece(wbufs, w_ext[:, C_SBV + pc * 512: C_SBV + (pc + 1) * 512], "wp")
                    for tb in range(8):
                        blk = p * 8 + tb
                        proj_token_major(
                            wb, wk, xT, tb,
                            lambda b, blk=blk, pc=pc: evac_copy(V[:, blk, pc * 512:(pc + 1) * 512], bank(b),
                                                                [("ps", b)], [("V", blk, pc)]))
            else:
                for pc in range(2):
                    wb, wk = load_piece(wbufs, w_ext[:, C_SBQ + pc * 512: C_SBQ + (pc + 1) * 512], "wp")
                    for hh in range(4):
                        h = pc * 4 + hh
                        proj_feature_major(
                            wb, wk, hh * 128, xT,
                            lambda hf, h=h: QT[:, h, hf * 512:(hf + 1) * 512],
                            lambda hf, h=h: ("QT", h, hf))

        S.barrier()
        A.release(m_sbattn)
        NS = 4
        e_buf = [A.f32(512) for _ in range(NS)]
        sp_buf = [A.bf16(512) for _ in range(NS)]
        t1_buf = [A.f32(512) for _ in range(NS)]
        w_buf = [A.bf16(512) for _ in range(NS)]
        acc_buf = [[A.bf16(512) for _ in range(2)] for _ in range(NS)]
        streams = []
        for s in range(NS):
            its = []
            for h in (s, s + 4):
                for q in range(2):
                    jmax = 8 * q + 7
                    for j in range(jmax, -1, -1):
                        its.append((h * 2 + q, h, q, j, jmax))
            streams.append(its)
        iters = []
        for h in range(NH):
            for q in range(2):
                for j in range(8 * q + 7, -1, -1):
                    iters.append((h * 2 + q, h, q, j, 8 * q + 7))

        def sb_info(q, j):
            r0 = max(0, j // 2 - 4 * q)
            ms = j // 2 - 4 * q
            return r0, (ms if ms >= 0 else None)

        def sb_parts(s, m):
            u, h, q, j, jmax = streams[s][m]
            r0, ms = sb_info(q, j)
            return u, h, q, j, jmax, r0 * 128, ms, jmax - j

        def sb_z(s, m):
            u, h, q, j, jmax, lo, ms, k = sb_parts(s, m)
            mm(bank(2 * s)[:, lo:512], KT[:, h, j * 128:(j + 1) * 128], QT[:, h, q * 512 + lo: (q + 1) * 512], True,
               ms is None, [("KT", h, j // 4), ("QT", h, q)], [("ps", 2 * s)])
            if ms is not None:
                mm(bank(2 * s)[:, ms * 128:(ms + 1) * 128], ident_b, mb_b[:, j % 2, :], False, True, ["mbb", "cstb"],
                   [("ps", 2 * s)])

        def sb_e(s, m):
            u, h, q, j, jmax, lo, ms, k = sb_parts(s, m)
            OP("act", "activation", [("ps", 2 * s)], [("e", s)], out=e_buf[s][:, lo:512], in_=bank(2 * s)[:, lo:512],
               func=AF.Exp, scale=QSCALE)

        def sb_sp(s, m):
            u, h, q, j, jmax, lo, ms, k = sb_parts(s, m)
            OP("act", "activation", [("e", s)], [("sp", s)], out=sp_buf[s][:, lo:512], in_=e_buf[s][:, lo:512],
               func=AF.Ln, bias=1.0, scale=1.0)

        def sb_t1(s, m):
            u, h, q, j, jmax, lo, ms, k = sb_parts(s, m)
            spb = sp_buf[s][:, lo:512]
            OP("dve", "scalar_tensor_tensor", [("ps", 2 * s), ("sp", s)], [("t1", s)],
               out=t1_buf[s][:, lo:512], in0=bank(2 * s)[:, lo:512], scalar=QSCALE, in1=spb, op0=ALU.mult, op1=ALU.subtract)
            ua = acc_buf[s]
            if j == jmax:
                for i2 in range(2):
                    OP("pool", "memset", [], [("acc", s, i2)], ap=ua[i2], constant=0.0)
            if j > 0:
                OP("pool", "tensor_tensor", [("acc", s, k % 2), ("sp", s)], [("acc", s, (k + 1) % 2)],
                   out=ua[(k + 1) % 2][:, lo:512], in0=ua[k % 2][:, lo:512], in1=spb, op=ALU.add)

        def sb_P(s, m):
            u, h, q, j, jmax, lo, ms, k = sb_parts(s, m)
            P = bank(2 * s)[:, lo:512]
            mm(P, triU_b, sp_buf[s][:, lo:512], True, False, [("sp", s), "cstb"], [("ps", 2 * s)])
            mm(P, ones_b, acc_buf[s][k % 2][:, lo:512], False, True, [("acc", s, k % 2), "cstb"], [("ps", 2 * s)])

        def sb_logw(s, m):
            u, h, q, j, jmax, lo, ms, k = sb_parts(s, m)
            t1b = t1_buf[s][:, lo:512]
            OP("dve", "tensor_tensor", [("t1", s), ("ps", 2 * s)], [("t1", s)], out=t1b, in0=t1b,
               in1=bank(2 * s)[:, lo:512], op=ALU.subtract)

        def sb_w(s, m):
            u, h, q, j, jmax, lo, ms, k = sb_parts(s, m)
            OP("act", "activation", [("t1", s)], [("w", s)], out=w_buf[s][:, lo:512], in_=t1_buf[s][:, lo:512],
               func=AF.Exp)

        def sb_av(s, m):
            u, h, q, j, jmax, lo, ms, k = sb_parts(s, m)
            Ob = 2 * s + 1
            mm(bank(Ob)[:, lo:512], V[:, j, h * 128:(h + 1) * 128], w_buf[s][:, lo:512], j == jmax, j == 0,
               [("V", j, h // 4), ("w", s)], [("ps", Ob)])
            if j == 0:
                evac_copy(mixT[:, h, q * 512:(q + 1) * 512], bank(Ob), [("ps", Ob)], [("mixT", h, q)])

        NIT = len(iters)
        nm = len(streams[0])
        grp = ((0, 1), (2, 3))
        for T in range(2 * nm + 1):
            F, mF = grp[T % 2], T // 2
            G, mG = grp[(T + 1) % 2], (T - 1) // 2
            for f1, f2 in ((sb_z, sb_P), (sb_e, sb_logw), (sb_sp, sb_w), (sb_t1, sb_av)):
                if mF < nm:
                    for s in F:
                        f1(s, mF)
                if T >= 1 and mG < nm:
                    for s in G:
                        f2(s, mG)

        if stage == "sb":
            return _finish_debug(nc, S, A, y, mixT, 8)

        S.barrier()
        A.release(m_phaseA)
        rKT = A.bf16(NH, SEQ)
        rV = A.bf16(16, 1024)
        rQT = A.bf16(NH, 1024)
        G = A.bf16(8, 1024)
        m_retattn = A.mark()
        xT = A.bf16(16, 1024)
        wbufs = [A.bf16(16, 512) for _ in range(2)]
        tab = mix_hi_f32[:, 0:2048].rearrange("p (a b) -> p a b", a=2)
        tA = [mix_hi_f32[:, 2048 + i * 512: 2048 + (i + 1) * 512] for i in range(2)]
        tB = [mix_hi_f32[:, 3072 + i * 512: 3072 + (i + 1) * 512] for i in range(2)]
        rr = [0]

        qb = [A.bf16(512) for _ in range(2)]

        def rot_proj(wb, wk, hh, xT, dst_fn, dkey_fn):
            for hf in range(2):
                bA, bB = next_banks(2)
                for k in range(16):
                    mm(bank(bA), wb[:, k, hh * 128:(hh + 1) * 128], xT[:, k, hf * 512:(hf + 1) * 512],
                       k == 0, k == 15, [wk, "xT"], [("ps", bA)])
                i = rr[0] % 2
                rr[0] += 1
                OP("act", "activation", [("ps", bA)], [("qb", i)], out=qb[i], in_=bank(bA), func=AF.Copy)
                mm(bank(bB), swp_b, qb[i], True, True, [("qb", i), "swp"], [("ps", bB)])
                cs, sn = tab[:, 0, hf * 512:(hf + 1) * 512], tab[:, 1, hf * 512:(hf + 1) * 512]
                OP("dve", "tensor_tensor", [("ps", bA), "tab", ("qb", i)], [("tA", i)], out=tA[i], in0=bank(bA), in1=cs,
                   op=ALU.mult)
                OP("dve", "tensor_tensor", [("ps", bB), "tab"], [("tB", i)], out=tB[i], in0=bank(bB), in1=sn, op=ALU.mult)
                OP("dve", "tensor_tensor", [("tA", i), ("tB", i)], [dkey_fn(hf)], out=dst_fn(hf), in0=tA[i], in1=tB[i],
                   op=ALU.add)

        for p in range(3):
            load_xT(xT, p)
            S.dma("sp", "tab", [], ["tab"], out=tab, in_=rot[p].rearrange("c d t -> d c t"))
            if p < 2:
                for pc in range(2):
                    wb, wk = load_piece(wbufs, w_ext[:, C_RKX + pc * 512: C_RKX + (pc + 1) * 512], "wr")
                    for hh in range(4):
                        h = pc * 4 + hh
                        rot_proj(wb, wk, hh, xT,
                                 lambda hf, h=h, p=p: rKT[:, h, p * 1024 + hf * 512: p * 1024 + (hf + 1) * 512],
                                 lambda hf, h=h, p=p: ("rKT", h, p * 2 + hf))
                for pc in range(2):
                    wb, wk = load_piece(wbufs, w_ext[:, C_RV + pc * 512: C_RV + (pc + 1) * 512], "wr")
                    for tb in range(8):
                        blk = p * 8 + tb
                        proj_token_major(
                            wb, wk, xT, tb,
                            lambda b, blk=blk, pc=pc: evac_copy(rV[:, blk, pc * 512:(pc + 1) * 512], bank(b),
                                                                [("ps", b)], [("rV", blk, pc)]))
            else:
                for pc in range(2):
                    wb, wk = load_piece(wbufs, w_ext[:, C_RQX + pc * 512: C_RQX + (pc + 1) * 512], "wr")
                    for hh in range(4):
                        h = pc * 4 + hh
                        rot_proj(wb, wk, hh, xT,
                                 lambda hf, h=h: rQT[:, h, hf * 512:(hf + 1) * 512],
                                 lambda hf, h=h: ("rQT", h, hf))
                for pc in range(2):
                    wb, wk = load_piece(wbufs, w_ext[:, C_RG + pc * 512: C_RG + (pc + 1) * 512], "wr")
                    for tb in range(8):
                        def cons(b, tb=tb, pc=pc):
                            OP("act", "activation", [("ps", b)], [("G", tb, pc)],
                               out=G[:, tb, pc * 512:(pc + 1) * 512], in_=bank(b), func=AF.Silu)
                        proj_token_major(wb, wk, xT, tb, cons)

        S.barrier()
        A.release(m_retattn)
        rm = A.f32(3, NH, 128)
        S.dma("sp", "rm", [], ["rm"], out=rm, in_=rmask.rearrange("p (a h c) -> p a h c", a=3, h=NH))
        gg = A.f32(1024)
        S.dma("sp", "gg", [], ["gg"], out=gg, in_=vecs[4, 0:1024].partition_broadcast(128))
        sc_buf = [A.bf16(512) for _ in range(2)]
        st6 = A.f32(4, 6)
        mv = A.f32(4, 2)
        rstd = A.f32(4)
        nmr = A.f32(4)
        nrm = [A.f32(128) for _ in range(2)]
        nrm2 = [A.f32(128) for _ in range(2)]
        rout = [A.bf16(128) for _ in range(2)]
        gam = [1.0 - 2.0 ** (-5.0 - h) for h in range(NH)]
        psT = ps[:, 4 * 512: 5 * 512].bitcast(BF16)

        def ret_s1(n):
            u, h, q, j, jmax = iters[n]
            r0, ms = sb_info(q, j)
            lo = r0 * 128
            sb_ = n % 2
            sT = bank(sb_)
            mm(sT[:, lo:512], rKT[:, h, j * 128:(j + 1) * 128], rQT[:, h, q * 512 + lo:(q + 1) * 512], True, True,
               [("rKT", h, j // 4), ("rQT", h, q)], [("ps", sb_)])
            for r in range(r0, 4):
                i_slot = 4 * q + r
                dstc = sc_buf[sb_][:, r * 128:(r + 1) * 128]
                src = sT[:, r * 128:(r + 1) * 128]
                if ms is not None and r == ms:
                    mk = rm[:, 1 if j % 2 == 0 else 2, h, :]
                    OP("dve", "tensor_tensor", [("ps", sb_), "rm"], [("sc", sb_)], out=dstc, in0=src, in1=mk, op=ALU.mult)
                else:
                    fac = gam[h] ** (128.0 * (2 * i_slot - j))
                    mk = rm[:, 0, h, :]
                    OP("dve", "scalar_tensor_tensor", [("ps", sb_), "rm"], [("sc", sb_)],
                       out=dstc, in0=src, scalar=fac, in1=mk, op0=ALU.mult, op1=ALU.mult)

        def ret_s2(n):
            u, h, q, j, jmax = iters[n]
            r0, ms = sb_info(q, j)
            sb_ = n % 2
            Ob = 2 + u % 2
            for r in range(r0, 4):
                first = (j == jmax)
                mm(bank(Ob)[:, r * 128:(r + 1) * 128], sc_buf[sb_][:, r * 128:(r + 1) * 128],
                   rV[:, j, h * 128:(h + 1) * 128], first, j == 0, [("sc", sb_), ("rV", j, h // 4)], [("ps", Ob)])
            if j == 0:
                for r in range(4):
                    o = bank(Ob)[:, r * 128:(r + 1) * 128]
                    OP("dve", "bn_stats", [("ps", Ob)], [("st6", r)], out=st6[:, r, :], in_=o)
                    OP("dve", "bn_aggr", [("st6", r)], [("mv",)], out=mv[:, r, :], in_=st6[:, r, :])
                OP("act", "activation", [("mv",)], [("rstd",)], out=rstd, in_=mv[:, :, 1], func=AF.Ln, bias=GN_EPS,
                   scale=1.0)
                OP("act", "activation", [("rstd",)], [("rstd",)], out=rstd, in_=rstd, func=AF.Exp, scale=-0.5)
                OP("dve", "scalar_tensor_tensor", [("mv",), ("rstd",)], [("nmr",)], out=nmr, in0=mv[:, :, 0], scalar=-1.0,
                   in1=rstd, op0=ALU.mult, op1=ALU.mult)
                for r in range(4):
                    i_slot = 4 * q + r
                    o = bank(Ob)[:, r * 128:(r + 1) * 128]
                    k2 = (u * 4 + r) % 2
                    OP("act", "activation", [("ps", Ob), ("nmr",), ("rstd",)], [("nrm", k2)], out=nrm[k2], in_=o,
                       func=AF.Identity, bias=nmr[:, r:r + 1], scale=rstd[:, r:r + 1])
                    OP("pool", "tensor_tensor", [("nrm", k2), "gg"], [("nrm2", k2)], out=nrm2[k2], in0=nrm[k2],
                       in1=gg[:, h * 128:(h + 1) * 128], op=ALU.mult)
                    OP("pool", "tensor_tensor", [("nrm2", k2), ("G", i_slot, h // 4)], [("rout", k2)], out=rout[k2],
                       in0=nrm2[k2], in1=G[:, i_slot, h * 128:(h + 1) * 128], op=ALU.mult)
                    OP("pe", "transpose", [("rout", k2), "cstb"], [("psT",)], out=psT[:, r * 128:(r + 1) * 128],
                       in_=rout[k2], identity=ident_b)
                evac_copy(mixT[:, 8 + h, q * 512:(q + 1) * 512], psT[:, 0:512], [("psT",)], [("mixT", 8 + h, q)])

        for step in range(NIT + 1):
            if step < NIT:
                ret_s1(step)
            if 0 <= step - 1 < NIT:
                ret_s2(step - 1)

        if stage == "attn":
            return _finish_debug(nc, S, A, y, mixT, 16)

        S.barrier()
        A.release(m_phaseA)
        yacc = A.f32(8, D)
        x1b = A.bf16(8, D)
        m_moe = A.mark()
        wob = [A.bf16(16, 512) for _ in range(2)]
        lng = A.f32(2, D)
        S.dma("sp", "lng", [], ["lng"], out=lng[:, 0, :], in_=vecs[0, :].partition_broadcast(128))
        S.dma("sp", "lng", [], ["lng"], out=lng[:, 1, :], in_=vecs[1, :].partition_broadcast(128))
        S.dma("sp", "xown", [], [("h1", s_) for s_ in range(8)], out=yacc, in_=x_own.rearrange("(s p) f -> p s f", p=128))
        for cp in range(4):
            wb, wk = load_piece(wob, w_out[:, cp * 512:(cp + 1) * 512], "wo")
            for s_ in range(8):
                (b,) = next_banks(1)
                for k in range(16):
                    mm(bank(b), mixT[:, k, s_ * 128:(s_ + 1) * 128], wb[:, k, :], k == 0, k == 15,
                       [wk, ("mixT", k, s_ // 4)], [("ps", b)])
                dst = yacc[:, s_, cp * 512:(cp + 1) * 512]
                OP("dve", "scalar_tensor_tensor", [("ps", b), ("h1", s_)], [("h1", s_)],
                   out=dst, in0=dst, scalar=ALPHA, in1=bank(b), op0=ALU.mult, op1=ALU.add)

        lst = A.f32(8, 24)
        lmv = A.f32(8, 2)
        lrs = A.f32(8)
        lnm = A.f32(8)

        def layer_norm(s_, lng, lst, lmv, lrs, lnm):
            xs = yacc[:, s_, :]
            for c in range(4):
                OP("dve", "bn_stats", [("h1", s_)], [("lst", s_)], out=lst[:, s_, c * 6:(c + 1) * 6],
                   in_=xs[:, c * 512:(c + 1) * 512])
            OP("dve", "bn_aggr", [("lst", s_)], [("lmv", s_)], out=lmv[:, s_, :], in_=lst[:, s_, :])
            OP("act", "activation", [("lmv", s_)], [("lrs", s_)], out=lrs[:, s_:s_ + 1], in_=lmv[:, s_, 1:2],
               func=AF.Ln, bias=LN_EPS, scale=1.0)
            OP("act", "activation", [("lrs", s_)], [("lrs", s_)], out=lrs[:, s_:s_ + 1], in_=lrs[:, s_:s_ + 1],
               func=AF.Exp, scale=-0.5)
            OP("dve", "scalar_tensor_tensor", [("lmv", s_), ("lrs", s_)], [("lnm", s_)], out=lnm[:, s_:s_ + 1],
               in0=lmv[:, s_, 0:1], scalar=-1.0, in1=lrs[:, s_:s_ + 1], op0=ALU.mult, op1=ALU.mult)
            OP("act", "activation", [("h1", s_), ("lnm", s_), ("lrs", s_)], [("h1", s_)], out=xs, in_=xs,
               func=AF.Identity, bias=lnm[:, s_:s_ + 1], scale=lrs[:, s_:s_ + 1])
            OP("pool", "tensor_tensor", [("h1", s_), "lng"], [("h1", s_)], out=xs, in0=xs, in1=lng[:, 0, :], op=ALU.mult)
            OP("pool", "tensor_tensor", [("h1", s_), "lng"], [("h1", s_)], out=xs, in0=xs, in1=lng[:, 1, :], op=ALU.add)

        for s_ in range(8):
            layer_norm(s_, lng, lst, lmv, lrs, lnm)
            OP("act", "activation", [("h1", s_)], [("x1b", s_)], out=x1b[:, s_, :], in_=yacc[:, s_, :], func=AF.Copy)

        if stage == "x1":
            S.dma("sp", "yout", [("h1", s_) for s_ in range(8)], ["y"], out=y.rearrange("(s p) f -> p s f", p=128),
                  in_=yacc)
            S.wait_all("sp", ["y"])
            S.emit()
            return nc

        S.barrier()
        A.release(m_moe)
        gates = A.f32(8, NE)
        posm = A.f32(8, NE)
        m_moe2 = A.mark()
        wr = A.f32(16, NE)
        br = A.f32(NE)
        onesrow = A.f32(128)
        bdn = A.f32(D)
        x1T = A.f32(16, 128)
        lg = A.f32(NE)
        mx8 = A.f32(8)
        msk = A.f32(8, NE)
        mskb = A.bf16(8, NE)
        msum = A.f32(NE)
        msumb = A.bf16(NE)
        ex = A.f32(NE)
        ssum = A.f32(1)
        nmx = A.f32(1)
        gT = A.f32(128)
        S.dma("sp", "wr", [], ["wr"], out=wr, in_=w_router.rearrange("(k q) n -> q k n", q=128))
        S.dma("sp", "br", [], ["br"], out=br[0:1, :], in_=b_router)
        S.dma("sp", "bdn", [], ["bdn"], out=bdn[0:NE, :], in_=b_dn)
        OP("dve", "memset", [], ["onesrow"], ap=onesrow[0:1, :], constant=1.0)
        OP("dve", "memset", [], ["msum"], ap=msum, constant=0.0)
        for s_ in range(8):
            for g4 in range(4):
                for c in range(4):
                    k = g4 * 4 + c
                    OP("pe", "transpose", [("h1", s_), "cst"], [("ps", g4)], out=bank(g4)[:, c * 128:(c + 1) * 128],
                       in_=yacc[:, s_, k * 128:(k + 1) * 128], identity=ident_f)
                evac_copy(x1T[:, g4 * 4:(g4 + 1) * 4, :], bank(g4).rearrange("p (a b) -> p a b", a=4),
                          [("ps", g4)], [("x1T", g4)])
            lgp = bank(4)[:, 0:NE]
            for k in range(16):
                mm(lgp, x1T[:, k, :], wr[:, k, :], k == 0, False, [("x1T", k // 4), "wr"], [("ps", 4)])
            mm(lgp, onesrow[0:1, :], br[0:1, :], False, True, ["onesrow", "br"], [("ps", 4)])
            OP("dve", "tensor_copy", [("ps", 4)], ["lg"], out=lg, in_=lgp)
            OP("dve", "max", ["lg"], ["mx8"], out=mx8, in_=lg)
            OP("dve", "tensor_scalar", ["lg", "mx8"], [("msk", s_)], out=msk[:, s_, :], in0=lg, scalar1=mx8[:, 3:4],
               scalar2=None, op0=ALU.is_ge)
            OP("dve", "tensor_scalar", ["mx8"], ["nmx"], out=nmx, in0=mx8[:, 0:1], scalar1=-1.0, scalar2=None, op0=ALU.mult)
            OP("act", "activation", ["lg", "nmx"], ["ex"], out=ex, in_=lg, func=AF.Exp, bias=nmx, scale=1.0)
            OP("dve", "tensor_tensor", ["ex", ("msk", s_)], ["ex"], out=ex, in0=ex, in1=msk[:, s_, :], op=ALU.mult)
            OP("dve", "reduce_sum", ["ex"], ["ssum"], out=ssum, in_=ex, axis=mybir.AxisListType.X)
            OP("dve", "reciprocal", ["ssum"], ["ssum"], out=ssum, in_=ssum)
            OP("dve", "tensor_scalar", ["ex", "ssum"], [("gates", s_)], out=gates[:, s_, :], in0=ex, scalar1=ssum[:, 0:1],
               scalar2=None, op0=ALU.mult)
            OP("dve", "tensor_copy", [("msk", s_)], [("mskb", s_)], out=mskb[:, s_, :], in_=msk[:, s_, :])
            OP("dve", "tensor_copy", ["msum"], ["msumb"], out=msumb, in_=msum)
            pp = bank(5)[:, 0:NE]
            mm(pp, triL_b, mskb[:, s_, :], True, False, [("mskb", s_), "cstb"], [("ps", 5)])
            mm(pp, ones_b, msumb, False, True, ["msumb", "cstb"], [("ps", 5)])
            OP("dve", "scalar_tensor_tensor", [("ps", 5), ("msk", s_)], [("posm", s_)], out=posm[:, s_, :], in0=pp,
               scalar=1.0, in1=msk[:, s_, :], op0=ALU.add, op1=ALU.mult)
            OP("dve", "tensor_scalar", [("posm", s_)], [("posm", s_)], out=posm[:, s_, :], in0=posm[:, s_, :],
               scalar1=-1.0, scalar2=None, op0=ALU.add)
            OP("dve", "tensor_tensor", ["msum", ("msk", s_)], ["msum"], out=msum, in0=msum, in1=msk[:, s_, :], op=ALU.add)
            OP("pe", "transpose", [("gates", s_), "cst"], [("ps", 6)], out=bank(6)[0:NE, 0:128], in_=gates[:, s_, :],
               identity=ident_f)
            OP("act", "activation", [("ps", 6)], ["gT"], out=gT[0:NE, :], in_=bank(6)[0:NE, 0:128], func=AF.Copy)
            for cp in range(4):
                bb = bank(7)
                mm(bb, gT[0:NE, :], bdn[0:NE, cp * 512:(cp + 1) * 512], True, True, ["gT", "bdn"], [("ps", 7)])
                dst = yacc[:, s_, cp * 512:(cp + 1) * 512]
                OP("dve", "scalar_tensor_tensor", [("ps", 7), ("h1", s_)], [("h1", s_)], out=dst, in0=dst, scalar=ALPHA,
                   in1=bb, op0=ALU.mult, op1=ALU.add)

        if stage == "router":
            S.barrier()
            yv = y.rearrange("(s p) f -> p s f", p=128)
            S.dma("sp", "yout", [], ["y"], out=yv[:, :, 0:NE], in_=gates)
            S.dma("sp", "yout", [], ["y"], out=yv[:, :, NE:2 * NE], in_=posm)
            S.dma("sp", "yout", [], ["y"], out=yv[:, :, 64:64 + 1024], in_=yacc[:, :, 0:1024])
            S.wait_all("sp", ["y"])
            S.emit()
            return nc

        S.barrier()
        A.release(m_moe2)
        wmb = [mix_raw[:, i * 4096:(i + 1) * 4096].bitcast(BF16).rearrange("p (a b) -> p a b", a=16) for i in range(2)]
        wmb.append(A.bf16(16, 512))
        sel = [A.bf16(8, CAP) for _ in range(2)]
        selT = [A.bf16(2, 1024) for _ in range(2)]
        XeT = [A.bf16(16, CAP) for _ in range(2)]
        actT = A.bf16(16, CAP)
        gsb = A.f32(4, CAP)
        utmp = [A.f32(CAP) for _ in range(2)]
        gtmp = utmp
        yrows = A.bf16(2, D)
        bgu = [A.f32(32) for _ in range(2)]
        psT7 = ps[:, 7 * 512: 8 * 512].bitcast(BF16)

        hb_n = [0]
        sc_n = [0]
        bgu1 = [A.f32(16) for _ in range(2)]

        def moe_front(ex_):
            eb = ex_ % 2
            S.dma("sp", f"bgu{eb}", [], [("bgu", eb)], out=bgu[eb], in_=b_gu[ex_])
            OP("dve", "tensor_scalar", [("bgu", eb)], [("bgu1", eb)], out=bgu1[eb], in0=bgu[eb][:, 16:32], scalar1=1.0,
               scalar2=None, op0=ALU.add)
            for s_ in range(8):
                OP("dve", "tensor_scalar", [("posm", s_), "cst"], [("sel", eb, s_)], out=sel[eb][:, s_, :], in0=iota_f[:, 0:CAP],
                   scalar1=posm[:, s_, ex_:ex_ + 1], scalar2=None, op0=ALU.is_equal)
            for rc, (r0_, rn) in enumerate(RCH):
                for s_ in range(8):
                    OP("pe", "transpose", [("sel", eb, s_), "cstb"], [("ps", 7)], out=psT7[0:rn, s_ * 128:(s_ + 1) * 128],
                       in_=sel[eb][:, s_, r0_:r0_ + rn], identity=ident_b)
                evac_copy(selT[eb][0:rn, rc, :], psT7[0:rn, :], [("ps", 7)], [("selT", eb, rc)], eng="act")
            for c in range(16):
                gb = (0, 7)[c % 2]
                for s_ in range(8):
                    mm(bank(gb, CAP), x1b[:, s_, c * 128:(c + 1) * 128], sel[eb][:, s_, :], s_ == 0, s_ == 7,
                       [("x1b", s_), ("sel", eb, s_)], [("ps", gb)])
                evac_copy(XeT[eb][:, c, :], bank(gb, CAP), [("ps", gb)], [("XeT", eb, c)], eng="act")

        def moe_hidden(ex_):
            eb = ex_ % 2
            for p4 in range(4):
                for gu in range(2):
                    col0 = gu * D + p4 * 512
                    wb, wk = load_piece(wmb, w_gu[ex_][:, col0:col0 + 512], "wm")
                    for cc in range(4):
                        hbi = hb_n[0] % 2
                        hb_n[0] += 1
                        hbk = ("ps", 1 + hbi)
                        hp_ = bank(1 + hbi, CAP)
                        for k in range(16):
                            mm(hp_, wb[:, k, cc * 128:(cc + 1) * 128], XeT[eb][:, k, :], k == 0, k == 15,
                               [wk, ("XeT", eb, k)], [hbk])
                        fch = p4 * 4 + cc
                        if gu == 0:
                            OP("dve", "tensor_scalar", [hbk, ("bgu", eb)], [("gtmp", hbi)], out=gtmp[hbi], in0=hp_,
                               scalar1=bgu[eb][:, fch:fch + 1], scalar2=7.0, op0=ALU.add, op1=ALU.min)
                            OP("act", "activation", [("gtmp", hbi)], [("gsb", cc)], out=gsb[:, cc, :], in_=gtmp[hbi],
                               func=AF.Gelu_apprx_sigmoid)
                        else:
                            OP("dve", "tensor_scalar", [hbk, ("bgu1", eb)], [("utmp", hbi)], out=utmp[hbi], in0=hp_,
                               scalar1=bgu1[eb][:, fch:fch + 1], scalar2=8.0, op0=ALU.add, op1=ALU.min)
                            OP("dve", "scalar_tensor_tensor", [("utmp", hbi), ("gsb", cc)], [("actT", fch)],
                               out=actT[:, fch, :], in0=utmp[hbi], scalar=-6.0, in1=gsb[:, cc, :], op0=ALU.max, op1=ALU.mult)

        def moe_down(ex_):
            eb = ex_ % 2
            for cp in range(4):
                wb, wk = load_piece(wmb, w_dn[ex_][:, cp * 512:(cp + 1) * 512], "wm")
                for rc, (r0_, rn) in enumerate(RCH):
                    yb = 3 + rc
                    for k in range(16):
                        mm(bank(yb)[0:rn, :], actT[:, k, r0_:r0_ + rn], wb[:, k, :], k == 0, k == 15,
                           [wk, ("actT", k)], [("ps", yb)])
                    evac_copy(yrows[0:rn, rc, cp * 512:(cp + 1) * 512], bank(yb)[0:rn, :], [("ps", yb)],
                              [("yrows", rc, cp)], eng="act")
                for s_ in range(8):
                    sb_ = 5 + sc_n[0] % 2
                    sc_n[0] += 1
                    for rc, (r0_, rn) in enumerate(RCH):
                        mm(bank(sb_), selT[eb][0:rn, rc, s_ * 128:(s_ + 1) * 128], yrows[0:rn, rc, cp * 512:(cp + 1) * 512],
                           rc == 0, rc == 1, [("selT", eb, rc), ("yrows", rc, cp)], [("ps", sb_)])
                    dst = yacc[:, s_, cp * 512:(cp + 1) * 512]
                    OP("dve", "scalar_tensor_tensor", [("ps", sb_), ("gates", s_), ("h1", s_, cp)], [("h1", s_, cp)],
                       out=dst, in0=bank(sb_), scalar=gates[:, s_, ex_:ex_ + 1], in1=dst, op0=ALU.mult, op1=ALU.add)

        moe_front(0)
        for ex_ in range(ne_run):
            moe_hidden(ex_)
            if ex_ + 1 < ne_run:
                moe_front(ex_ + 1)
            moe_down(ex_)

        S.barrier()
        A.release(m_moe2)
        lng2 = A.f32(2, D)
        lst2 = A.f32(8, 24)
        lmv2 = A.f32(8, 2)
        lrs2 = A.f32(8)
        lnm2 = A.f32(8)
        S.dma("sp", "lng2", [], ["lng"], out=lng2[:, 0, :], in_=vecs[2, :].partition_broadcast(128))
        S.dma("sp", "lng2", [], ["lng"], out=lng2[:, 1, :], in_=vecs[3, :].partition_broadcast(128))
        yv = y.rearrange("(s p) f -> p s f", p=128)
        for s_ in range(8):
            layer_norm(s_, lng2, lst2, lmv2, lrs2, lnm2)
            S.dma("sp", "yout", [("h1", s_)], [("y", s_)], out=yv[:, s_, :], in_=yacc[:, s_, :])
        S.wait_all("sp", [("y", s_) for s_ in range(8)])
        S.emit()
    return nc


def _finish_debug(nc, S, A, y, mixT, nchunks):
    S.barrier()
    tmp = [A.f32(1024) for _ in range(2)]
    yv = y.rearrange("r (two t) -> (r two) t", two=2).rearrange("(c p) t -> p c t", p=128)
    for c in range(nchunks):
        S.op("dve", "tensor_copy", [], [("tmp", c % 2)], out=tmp[c % 2], in_=mixT[:, c, :])
        S.dma("sp", "dbg", [("tmp", c % 2)], ["ydbg"], out=yv[:, c, :], in_=tmp[c % 2])
    S.wait_all("sp", ["ydbg"])
    S.emit()
    return nc


def _constants(hp):
    p = np.arange(128)
    ident = np.eye(128, dtype=np.float32)
    triU = (p[:, None] > p[None, :]).astype(np.float32)
    triL = (p[:, None] < p[None, :]).astype(np.float32)
    ones = np.ones((128, 128), np.float32)
    strict = (p[:, None] < p[None, :]).astype(np.float32)
    if hp == 0:
        mA, mB = strict, np.zeros((128, 128), np.float32)
    else:
        mA, mB = ones, strict
    iota = np.broadcast_to(np.arange(256, dtype=np.float32)[None, :], (128, 256))
    cst = np.concatenate([ident, triU, triL, ones, mA, mB, iota], axis=1).astype(np.float32)
    sl = p[:, None].astype(np.float64)
    tl = p[None, :].astype(np.float64)
    rm = np.zeros((128, 3, NH, 128), np.float64)
    for h in range(NH):
        lg = math.log1p(-2.0 ** (-5.0 - h))
        gen = np.exp(lg * (128.0 * hp + tl - sl))
        diag = np.where((p[:, None] // 64) <= (p[None, :] // 64), np.exp(lg * np.abs(tl - sl)), 0.0)
        rm[:, 0, h, :] = gen
        if hp == 0:
            rm[:, 1, h, :] = diag
            rm[:, 2, h, :] = 0.0
        else:
            rm[:, 1, h, :] = np.exp(lg * (128.0 + tl - sl))
            rm[:, 2, h, :] = diag
    rm *= HD ** -0.5
    return cst, rm.reshape(128, 3 * NH * 128).astype(np.float32)


def _rot_tables(hp):
    inv = 10000.0 ** (-np.arange(0, HD, 2, dtype=np.float32) / HD)
    own = np.concatenate([np.arange(128) + (2 * i + hp) * 128 for i in range(8)])
    out = np.zeros((3, 2, 128, 1024), np.float32)
    for pi, pos in enumerate([np.arange(0, 1024), np.arange(1024, 2048), own]):
        ang = pos.astype(np.float32)[None, :] * inv[:, None]
        c, s = np.cos(ang), np.sin(ang)
        out[pi, 0] = np.concatenate([c, c], axis=0)
        out[pi, 1] = np.concatenate([-s, s], axis=0)
    return out


def _w_ext(w_in):
    return np.ascontiguousarray(w_in)


_PROG = {}


def _get_prog(stage="full", ne_run=NE):
    if (stage, ne_run) not in _PROG:
        _PROG[(stage, ne_run)] = build_program(stage, ne_run)
    return _PROG[(stage, ne_run)]


def make_in_maps(x, w_in, ret_gn_gain, w_out, ln1_gain, ln1_bias, w_router, b_router,
                 w_gate_up, b_gate_up, w_down, b_down, ln2_gain, ln2_bias, stage="full", ne_run=NE):
    f = lambda a: np.ascontiguousarray(np.asarray(a, dtype=np.float32))
    x = f(x)
    wext = _w_ext(f(w_in)[0])
    vecs = np.zeros((5, D), np.float32)
    vecs[0], vecs[1], vecs[2], vecs[3] = f(ln1_gain)[0], f(ln1_bias)[0], f(ln2_gain)[0], f(ln2_bias)[0]
    vecs[4, :1024] = f(ret_gn_gain)[0]
    shared = {"w_ext": wext, "w_out": f(w_out)[0], "vecs": vecs}
    if stage in ("full", "router"):
        shared.update({
            "w_router": f(w_router)[0], "b_router": f(b_router)[0].reshape(1, NE), "b_dn": f(b_down)[0],
            "b_gu": np.ascontiguousarray(f(b_gate_up)[0].reshape(NE, 32, 128).transpose(0, 2, 1)),
        })
    if stage == "full":
        shared.update({"w_gu": f(w_gate_up)[0][:ne_run], "w_dn": f(w_down)[0][:ne_run]})
    consts = [(_constants(hp), _rot_tables(hp)) for hp in range(2)]
    in_maps = []
    for c in range(8):
        b, hp = c // 2, c % 2
        xb = x[b]
        own = np.concatenate([np.arange(128) + (2 * i + hp) * 128 for i in range(8)])
        xo = np.ascontiguousarray(xb[own])
        xT3 = np.stack([np.ascontiguousarray(xb[0:1024].T), np.ascontiguousarray(xb[1024:2048].T),
                        np.ascontiguousarray(xo.T)])
        (cst, rm), rot = consts[hp]
        m = dict(shared)
        m.update({"xT3": xT3, "x_own": xo, "cst": cst, "rmask": rm, "rot": rot})
        in_maps.append(m)
    return in_maps


def kernel(x, w_in, ret_gn_gain, w_out, ln1_gain, ln1_bias, w_router, b_router,
           w_gate_up, b_gate_up, w_down, b_down, ln2_gain, ln2_bias):
    in_maps = make_in_maps(x, w_in, ret_gn_gain, w_out, ln1_gain, ln1_bias, w_router, b_router,
                           w_gate_up, b_gate_up, w_down, b_down, ln2_gain, ln2_bias)
    nc = _get_prog("full")
    res = run_bass_kernel_spmd(nc, in_maps, core_ids=list(range(8)))
    out = np.zeros((NB, SEQ, D), np.float32)
    for c in range(8):
        b, hp = c // 2, c % 2
        yc = res.results[c]["y"]
        for i in range(8):
            blk = 2 * i + hp
            out[b, blk * 128:(blk + 1) * 128] = yc[i * 128:(i + 1) * 128]
    return out
```

```python
import math
from contextlib import ExitStack

import numpy as np
import concourse.bass as bass
import concourse.mybir as mybir
from concourse.bass_utils import run_bass_kernel_spmd

F32 = mybir.dt.float32
BF16 = mybir.dt.bfloat16
AF = mybir.ActivationFunctionType
ALU = mybir.AluOpType

D = 2048
SEQ = 2048
NB = 4
HD = 128
NH = 8
NE = 32
CAP = 192
RCH = ((0, 128), (128, 64))
ALPHA = 2.0 ** 0.25
LN_EPS = 1e-5
GN_EPS = 1e-5
QSCALE = 1.0 / math.sqrt(HD)
WEXT = 7168
EPOCH = 2000

C_SBQ, C_SBK, C_SBV, C_RQX, C_RKX, C_RV, C_RG = 0, 1024, 2048, 3072, 4096, 5120, 6144


class Sched:
    ENGS = ("pe", "act", "dve", "pool", "sp")

    def __init__(self, nc):
        self.nc = nc
        self.ops = {e: [] for e in self.ENGS}
        self.count = {e: 0 for e in self.ENGS}
        self.known = {e: {} for e in self.ENGS}
        self.last_w = {}
        self.readers = {}
        self.dma_slots = {}
        self.n_dma_sems = 0

    def _need(self, eng, key, val, waits):
        if key == ("eng", "pe") and eng == "pe":
            return
        if self.known[eng].get(key, 0) >= val:
            return
        self.known[eng][key] = val
        waits[key] = max(waits.get(key, 0), val)

    def _deps(self, eng, reads, writes):
        waits = {}
        for r in reads:
            t = self.last_w.get(r)
            if t is not None:
                self._need(eng, t[0], t[1], waits)
        for w in writes:
            t = self.last_w.get(w)
            if t is not None:
                self._need(eng, t[0], t[1], waits)
            for k, v in self.readers.get(w, {}).items():
                self._need(eng, k, v, waits)
        return list(waits.items())

    def _commit(self, tok, reads, writes):
        for r in reads:
            d = self.readers.setdefault(r, {})
            d[tok[0]] = max(d.get(tok[0], 0), tok[1])
        for w in writes:
            self.last_w[w] = tok
            self.readers[w] = {}

    def op(self, eng, method, reads=(), writes=(), **kw):
        fn = (method, kw)
        waits = self._deps(eng, reads, writes)
        idx = self.count[eng]
        self.count[eng] += 1
        tok = (("eng", eng), idx + 1)
        self.ops[eng].append((waits, fn, ("eng", idx)))
        self._commit(tok, reads, writes)

    def dma(self, eng, slot, reads=(), writes=(), **kw):
        fn = ("dma_start", kw)
        waits = self._deps(eng, reads, writes)
        if slot not in self.dma_slots:
            self.dma_slots[slot] = [self.n_dma_sems, 0]
            self.n_dma_sems += 1
        self.dma_slots[slot][1] += 16
        tok = (("dma", slot), self.dma_slots[slot][1])
        self.ops[eng].append((waits, fn, ("dma", slot)))
        self._commit(tok, reads, writes)

    def wait_all(self, eng, regions):
        waits = self._deps(eng, regions, ())
        self.ops[eng].append((waits, None, None))

    def barrier(self):
        toks = [(("eng", e), self.count[e]) for e in self.ENGS if self.count[e] > 0]
        toks += [(("dma", s), v[1]) for s, v in self.dma_slots.items()]
        for e in self.ENGS:
            waits = {}
            for k, v in toks:
                self._need(e, k, v, waits)
            self.ops[e].append((list(waits.items()), None, None))
        self.last_w.clear()
        self.readers.clear()

    def emit(self):
        nc = self.nc
        with ExitStack() as st:
            eng_sems = {}
            for e in self.ENGS:
                n_ep = self.count[e] // EPOCH + 1
                eng_sems[e] = [st.enter_context(nc.semaphore(f"s_{e}_{i}")) for i in range(n_ep)]
            dma_sems = [st.enter_context(nc.semaphore(f"s_dma_{i}")) for i in range(self.n_dma_sems)]
            block = st.enter_context(nc.Block())

            def run(ename):
                def body(eng):
                    for waits, fn, inc in self.ops[ename]:
                        for key, val in waits:
                            if key[0] == "eng":
                                idx = val - 1
                                eng.wait_ge(eng_sems[key[1]][idx // EPOCH], idx % EPOCH + 1)
                            else:
                                eng.wait_ge(dma_sems[self.dma_slots[key[1]][0]], val)
                        if fn is None:
                            continue
                        ins = getattr(eng, fn[0])(**fn[1])
                        if inc[0] == "eng":
                            idx = inc[1]
                            ins.then_inc(eng_sems[ename][idx // EPOCH], 1)
                        else:
                            ins.then_inc(dma_sems[self.dma_slots[inc[1]][0]], 16)
                return body

            block.tensor(run("pe"))
            block.scalar(run("act"))
            block.vector(run("dve"))
            block.gpsimd(run("pool"))
            block.sync(run("sp"))


class Arena:
    def __init__(self, t, words):
        self.t = t
        self.words = words
        self.off = 0

    def mark(self):
        return self.off

    def release(self, m):
        self.off = m

    def _take(self, nwords):
        a = self.off
        self.off += nwords
        assert self.off <= self.words, f"SBUF arena overflow {self.off} > {self.words}"
        return self.t[:, a:a + nwords]

    def f32(self, *dims):
        n = int(np.prod(dims))
        v = self._take(n)
        return self._shape(v, dims)

    def bf16(self, *dims):
        n = int(np.prod(dims))
        assert n % 2 == 0
        v = self._take(n // 2).bitcast(BF16)
        return self._shape(v, dims)

    @staticmethod
    def _shape(v, dims):
        if len(dims) == 1:
            return v
        if len(dims) == 2:
            return v.rearrange("p (a b) -> p a b", a=dims[0])
        if len(dims) == 3:
            return v.rearrange("p (a b c) -> p a b c", a=dims[0], b=dims[1])
        raise ValueError(dims)


def build_program(stage="full", ne_run=NE):
    nc = bass.Bass("TRN2", target_bir_lowering=False)
    dt = nc.dram_tensor
    with_moe = stage in ("full", "router")
    xT3 = dt("xT3", [3, D, 1024], F32, kind="ExternalInput").ap()
    x_own = dt("x_own", [1024, D], F32, kind="ExternalInput").ap()
    w_ext = dt("w_ext", [D, WEXT], F32, kind="ExternalInput").ap()
    w_out = dt("w_out", [D, D], F32, kind="ExternalInput").ap()
    vecs = dt("vecs", [5, D], F32, kind="ExternalInput").ap()
    cst = dt("cst", [128, 1024], F32, kind="ExternalInput").ap()
    rmask = dt("rmask", [128, 3 * NH * 128], F32, kind="ExternalInput").ap()
    rot = dt("rot", [3, 2, 128, 1024], F32, kind="ExternalInput").ap()
    if with_moe:
        w_router = dt("w_router", [D, NE], F32, kind="ExternalInput").ap()
        b_router = dt("b_router", [1, NE], F32, kind="ExternalInput").ap()
        b_gu = dt("b_gu", [NE, 128, 32], F32, kind="ExternalInput").ap()
        if stage == "full":
            w_gu = dt("w_gu", [ne_run, 8, 128, 16, 512], F32, kind="ExternalInput").ap()
            w_dn = dt("w_dn", [ne_run, 4, 128, 16, 512], F32, kind="ExternalInput").ap()
        b_dn = dt("b_dn", [NE, D], F32, kind="ExternalInput").ap()
    y = dt("y", [1024, D], F32, kind="ExternalOutput").ap()

    AW = 53000
    with ExitStack() as st:
        arena_t = st.enter_context(nc.sbuf_tensor("arena", [128, AW], F32))
        ps = st.enter_context(nc.psum_tensor("ps", [128, 4096], F32))
        A = Arena(arena_t, AW)
        S = Sched(nc)
        OP = S.op

        def bank(b, n=512, off=0):
            return ps[:, b * 512 + off: b * 512 + off + n]

        def mm(out, lhsT, rhs, start, stop, reads, writes):
            OP("pe", "matmul", reads, writes, out=out, lhsT=lhsT, rhs=rhs, start=start, stop=stop)

        evac_rr = [0]

        def evac_copy(dst, src, reads, writes, eng=None):
            if eng is None:
                eng = ("act", "dve")[evac_rr[0] % 2]
                evac_rr[0] += 1
            if eng == "act":
                OP("act", "activation", reads, writes, out=dst, in_=src, func=AF.Copy)
            else:
                OP("dve", "tensor_copy", reads, writes, out=dst, in_=src)

        cst_f = A.f32(1024)
        ident_f = cst_f[:, 0:128]
        maskA = cst_f[:, 512:640]
        maskB = cst_f[:, 640:768]
        iota_f = cst_f[:, 768:1024]
        cst_b = A.bf16(4, 128)
        ident_b, triU_b, triL_b, ones_b = (cst_b[:, i, :] for i in range(4))
        S.dma("sp", "cst", [], ["cst"], out=cst_f, in_=cst)
        OP("dve", "tensor_copy", ["cst"], ["cstb"], out=cst_b,
           in_=cst_f[:, 0:512].rearrange("p (a b) -> p a b", a=4))
        swp_b = A.bf16(128)
        OP("dve", "tensor_copy", ["cstb"], ["swp"], out=swp_b[:, 0:64], in_=ident_b[:, 64:128])
        OP("dve", "tensor_copy", ["cstb"], ["swp"], out=swp_b[:, 64:128], in_=ident_b[:, 0:64])
        mb_b = A.bf16(2, 128)
        OP("dve", "tensor_scalar", ["cst"], ["mbb"], out=mb_b, in0=cst_f[:, 512:768].rearrange("p (a b) -> p a b", a=2),
           scalar1=-1.0, scalar2=1.0e4, op0=ALU.add, op1=ALU.mult)
        mix_raw = A._take(8192)
        mixT = mix_raw.bitcast(BF16).rearrange("p (a b) -> p a b", a=16)
        mix_hi_f32 = mix_raw[:, 4096:8192]
        m_phaseA = A.mark()

        wp_n = [0]

        def load_piece(wbufs, src_ap, tag, tiled=False):
            i = wp_n[0] % len(wbufs)
            wp_n[0] += 1
            key = (tag, i)
            S.dma("pool", f"{tag}{i}", [], [key], out=wbufs[i],
                  in_=src_ap if tiled else src_ap.rearrange("(k q) c -> q k c", q=128))
            return wbufs[i], key

        def load_xT(xT, p):
            S.dma("pool", "xT", [], ["xT"], out=xT, in_=xT3[p].rearrange("(k q) t -> q k t", q=128))

        pbank = [0]

        def next_banks(n):
            b = pbank[0]
            pbank[0] = (pbank[0] + n) % 4
            return [b + i for i in range(n)]

        def proj_feature_major(wbuf, wkey, col, xT, dst_fn, dkey_fn):
            bA, bB = next_banks(2)
            for k in range(16):
                for hf, b in ((0, bA), (1, bB)):
                    mm(bank(b), wbuf[:, k, col:col + 128], xT[:, k, hf * 512:(hf + 1) * 512],
                       k == 0, k == 15, [wkey, "xT"], [("ps", b)])
            for hf, b in ((0, bA), (1, bB)):
                evac_copy(dst_fn(hf), bank(b), [("ps", b)], [dkey_fn(hf)])

        def proj_token_major(wbuf, wkey, xT, tb, consume):
            (b,) = next_banks(1)
            for k in range(16):
                mm(bank(b), xT[:, k, tb * 128:(tb + 1) * 128], wbuf[:, k, :], k == 0, k == 15,
                   [wkey, "xT"], [("ps", b)])
            consume(b)

        KT = A.bf16(NH, SEQ)
        V = A.bf16(16, 1024)
        QT = A.bf16(NH, 1024)
        m_sbattn = A.mark()
        xT = A.bf16(16, 1024)
        wbufs = [A.bf16(16, 512) for _ in range(3)]

        for p in range(3):
            load_xT(xT, p)
            if p < 2:
                for pc in range(2):
                    wb, wk = load_piece(wbufs, w_ext[:, C_SBK + pc * 512: C_SBK + (pc + 1) * 512], "wp")
                    for hh in range(4):
                        h = pc * 4 + hh
                        proj_feature_major(
                            wb, wk, hh * 128, xT,
                            lambda hf, h=h, p=p: KT[:, h, p * 1024 + hf * 512: p * 1024 + (hf + 1) * 512],
                            lambda hf, h=h, p=p: ("KT", h, p * 2 + hf))
                for pc in range(2):
                    wb, wk = load_piece(wbufs, w_ext[:, C_SBV + pc * 512: C_SBV + (pc + 1) * 512], "wp")
                    for tb in range(8):
                        blk = p * 8 + tb
                        proj_token_major(
                            wb, wk, xT, tb,
                            lambda b, blk=blk, pc=pc: evac_copy(V[:, blk, pc * 512:(pc + 1) * 512], bank(b),
                                                                [("ps", b)], [("V", blk, pc)]))
            else:
                for pc in range(2):
                    wb, wk = load_piece(wbufs, w_ext[:, C_SBQ + pc * 512: C_SBQ + (pc + 1) * 512], "wp")
                    for hh in range(4):
                        h = pc * 4 + hh
                        proj_feature_major(
                            wb, wk, hh * 128, xT,
                            lambda hf, h=h: QT[:, h, hf * 512:(hf + 1) * 512],
                            lambda hf, h=h: ("QT", h, hf))

        S.barrier()
        A.release(m_sbattn)
        NS = 4
        e_buf = [A.f32(512) for _ in range(NS)]
        sp_buf = [A.bf16(512) for _ in range(NS)]
        t1_buf = [A.f32(512) for _ in range(NS)]
        w_buf = [A.bf16(512) for _ in range(NS)]
        acc_buf = [[A.bf16(512) for _ in range(2)] for _ in range(NS)]
        streams = []
        for s in range(NS):
            its = []
            for h in (s, s + 4):
                for q in range(2):
                    jmax = 8 * q + 7
                    for j in range(jmax, -1, -1):
                        its.append((h * 2 + q, h, q, j, jmax))
            streams.append(its)
        iters = []
        for h in range(NH):
            for q in range(2):
                for j in range(8 * q + 7, -1, -1):
                    iters.append((h * 2 + q, h, q, j, 8 * q + 7))

        def sb_info(q, j):
            r0 = max(0, j // 2 - 4 * q)
            ms = j // 2 - 4 * q
            return r0, (ms if ms >= 0 else None)

        def sb_parts(s, m):
            u, h, q, j, jmax = streams[s][m]
            r0, ms = sb_info(q, j)
            return u, h, q, j, jmax, r0 * 128, ms, jmax - j

        def sb_z(s, m):
            u, h, q, j, jmax, lo, ms, k = sb_parts(s, m)
            mm(bank(2 * s)[:, lo:512], KT[:, h, j * 128:(j + 1) * 128], QT[:, h, q * 512 + lo: (q + 1) * 512], True,
               ms is None, [("KT", h, j // 4), ("QT", h, q)], [("ps", 2 * s)])
            if ms is not None:
                mm(bank(2 * s)[:, ms * 128:(ms + 1) * 128], ident_b, mb_b[:, j % 2, :], False, True, ["mbb", "cstb"],
                   [("ps", 2 * s)])

        def sb_e(s, m):
            u, h, q, j, jmax, lo, ms, k = sb_parts(s, m)
            OP("act", "activation", [("ps", 2 * s)], [("e", s)], out=e_buf[s][:, lo:512], in_=bank(2 * s)[:, lo:512],
               func=AF.Exp, scale=QSCALE)

        def sb_sp(s, m):
            u, h, q, j, jmax, lo, ms, k = sb_parts(s, m)
            OP("act", "activation", [("e", s)], [("sp", s)], out=sp_buf[s][:, lo:512], in_=e_buf[s][:, lo:512],
               func=AF.Ln, bias=1.0, scale=1.0)

        def sb_t1(s, m):
            u, h, q, j, jmax, lo, ms, k = sb_parts(s, m)
            spb = sp_buf[s][:, lo:512]
            OP("dve", "scalar_tensor_tensor", [("ps", 2 * s), ("sp", s)], [("t1", s)],
               out=t1_buf[s][:, lo:512], in0=bank(2 * s)[:, lo:512], scalar=QSCALE, in1=spb, op0=ALU.mult, op1=ALU.subtract)
            ua = acc_buf[s]
            if j == jmax:
                for i2 in range(2):
                    OP("pool", "memset", [], [("acc", s, i2)], ap=ua[i2], constant=0.0)
            if j > 0:
                OP("pool", "tensor_tensor", [("acc", s, k % 2), ("sp", s)], [("acc", s, (k + 1) % 2)],
                   out=ua[(k + 1) % 2][:, lo:512], in0=ua[k % 2][:, lo:512], in1=spb, op=ALU.add)

        def sb_P(s, m):
            u, h, q, j, jmax, lo, ms, k = sb_parts(s, m)
            P = bank(2 * s)[:, lo:512]
            mm(P, triU_b, sp_buf[s][:, lo:512], True, False, [("sp", s), "cstb"], [("ps", 2 * s)])
            mm(P, ones_b, acc_buf[s][k % 2][:, lo:512], False, True, [("acc", s, k % 2), "cstb"], [("ps", 2 * s)])

        def sb_logw(s, m):
            u, h, q, j, jmax, lo, ms, k = sb_parts(s, m)
            t1b = t1_buf[s][:, lo:512]
            OP("dve", "tensor_tensor", [("t1", s), ("ps", 2 * s)], [("t1", s)], out=t1b, in0=t1b,
               in1=bank(2 * s)[:, lo:512], op=ALU.subtract)

        def sb_w(s, m):
            u, h, q, j, jmax, lo, ms, k = sb_parts(s, m)
            OP("act", "activation", [("t1", s)], [("w", s)], out=w_buf[s][:, lo:512], in_=t1_buf[s][:, lo:512],
               func=AF.Exp)

        def sb_av(s, m):
            u, h, q, j, jmax, lo, ms, k = sb_parts(s, m)
            Ob = 2 * s + 1
            mm(bank(Ob)[:, lo:512], V[:, j, h * 128:(h + 1) * 128], w_buf[s][:, lo:512], j == jmax, j == 0,
               [("V", j, h // 4), ("w", s)], [("ps", Ob)])
            if j == 0:
                evac_copy(mixT[:, h, q * 512:(q + 1) * 512], bank(Ob), [("ps", Ob)], [("mixT", h, q)])

        NIT = len(iters)
        nm = len(streams[0])
        grp = ((0, 1), (2, 3))
        for T in range(2 * nm + 1):
            F, mF = grp[T % 2], T // 2
            G, mG = grp[(T + 1) % 2], (T - 1) // 2
            for f1, f2 in ((sb_z, sb_P), (sb_e, sb_logw), (sb_sp, sb_w), (sb_t1, sb_av)):
                if mF < nm:
                    for s in F:
                        f1(s, mF)
                if T >= 1 and mG < nm:
                    for s in G:
                        f2(s, mG)

        if stage == "sb":
            return _finish_debug(nc, S, A, y, mixT, 8)

        S.barrier()
        A.release(m_phaseA)
        rKT = A.bf16(NH, SEQ)
        rV = A.bf16(16, 1024)
        rQT = A.bf16(NH, 1024)
        G = A.bf16(8, 1024)
        m_retattn = A.mark()
        xT = A.bf16(16, 1024)
        wbufs = [A.bf16(16, 512) for _ in range(2)]
        tab = mix_hi_f32[:, 0:2048].rearrange("p (a b) -> p a b", a=2)
        tA = [mix_hi_f32[:, 2048 + i * 512: 2048 + (i + 1) * 512] for i in range(2)]
        tB = [mix_hi_f32[:, 3072 + i * 512: 3072 + (i + 1) * 512] for i in range(2)]
        rr = [0]

        qb = [A.bf16(512) for _ in range(2)]

        def rot_proj(wb, wk, hh, xT, dst_fn, dkey_fn):
            for hf in range(2):
                bA, bB = next_banks(2)
                for k in range(16):
                    mm(bank(bA), wb[:, k, hh * 128:(hh + 1) * 128], xT[:, k, hf * 512:(hf + 1) * 512],
                       k == 0, k == 15, [wk, "xT"], [("ps", bA)])
                i = rr[0] % 2
                rr[0] += 1
                OP("act", "activation", [("ps", bA)], [("qb", i)], out=qb[i], in_=bank(bA), func=AF.Copy)
                mm(bank(bB), swp_b, qb[i], True, True, [("qb", i), "swp"], [("ps", bB)])
                cs, sn = tab[:, 0, hf * 512:(hf + 1) * 512], tab[:, 1, hf * 512:(hf + 1) * 512]
                OP("dve", "tensor_tensor", [("ps", bA), "tab", ("qb", i)], [("tA", i)], out=tA[i], in0=bank(bA), in1=cs,
                   op=ALU.mult)
                OP("dve", "tensor_tensor", [("ps", bB), "tab"], [("tB", i)], out=tB[i], in0=bank(bB), in1=sn, op=ALU.mult)
                OP("dve", "tensor_tensor", [("tA", i), ("tB", i)], [dkey_fn(hf)], out=dst_fn(hf), in0=tA[i], in1=tB[i],
                   op=ALU.add)

        for p in range(3):
            load_xT(xT, p)
            S.dma("sp", "tab", [], ["tab"], out=tab, in_=rot[p].rearrange("c d t -> d c t"))
            if p < 2:
                for pc in range(2):
                    wb, wk = load_piece(wbufs, w_ext[:, C_RKX + pc * 512: C_RKX + (pc + 1) * 512], "wr")
                    for hh in range(4):
                        h = pc * 4 + hh
                        rot_proj(wb, wk, hh, xT,
                                 lambda hf, h=h, p=p: rKT[:, h, p * 1024 + hf * 512: p * 1024 + (hf + 1) * 512],
                                 lambda hf, h=h, p=p: ("rKT", h, p * 2 + hf))
                for pc in range(2):
                    wb, wk = load_piece(wbufs, w_ext[:, C_RV + pc * 512: C_RV + (pc + 1) * 512], "wr")
                    for tb in range(8):
                        blk = p * 8 + tb
                        proj_token_major(
                            wb, wk, xT, tb,
                            lambda b, blk=blk, pc=pc: evac_copy(rV[:, blk, pc * 512:(pc + 1) * 512], bank(b),
                                                                [("ps", b)], [("rV", blk, pc)]))
            else:
                for pc in range(2):
                    wb, wk = load_piece(wbufs, w_ext[:, C_RQX + pc * 512: C_RQX + (pc + 1) * 512], "wr")
                    for hh in range(4):
                        h = pc * 4 + hh
                        rot_proj(wb, wk, hh, xT,
                                 lambda hf, h=h: rQT[:, h, hf * 512:(hf + 1) * 512],
                                 lambda hf, h=h: ("rQT", h, hf))
                for pc in range(2):
                    wb, wk = load_piece(wbufs, w_ext[:, C_RG + pc * 512: C_RG + (pc + 1) * 512], "wr")
                    for tb in range(8):
                        def cons(b, tb=tb, pc=pc):
                            OP("act", "activation", [("ps", b)], [("G", tb, pc)],
                               out=G[:, tb, pc * 512:(pc + 1) * 512], in_=bank(b), func=AF.Silu)
                        proj_token_major(wb, wk, xT, tb, cons)

        S.barrier()
        A.release(m_retattn)
        rm = A.f32(3, NH, 128)
        S.dma("sp", "rm", [], ["rm"], out=rm, in_=rmask.rearrange("p (a h c) -> p a h c", a=3, h=NH))
        gg = A.f32(1024)
        S.dma("sp", "gg", [], ["gg"], out=gg, in_=vecs[4, 0:1024].partition_broadcast(128))
        sc_buf = [A.bf16(512) for _ in range(2)]
        st6 = A.f32(4, 6)
        mv = A.f32(4, 2)
        rstd = A.f32(4)
        nmr = A.f32(4)
        nrm = [A.f32(128) for _ in range(2)]
        nrm2 = [A.f32(128) for _ in range(2)]
        rout = [A.bf16(128) for _ in range(2)]
        gam = [1.0 - 2.0 ** (-5.0 - h) for h in range(NH)]
        psT = ps[:, 4 * 512: 5 * 512].bitcast(BF16)

        def ret_s1(n):
            u, h, q, j, jmax = iters[n]
            r0, ms = sb_info(q, j)
            lo = r0 * 128
            sb_ = n % 2
            sT = bank(sb_)
            mm(sT[:, lo:512], rKT[:, h, j * 128:(j + 1) * 128], rQT[:, h, q * 512 + lo:(q + 1) * 512], True, True,
               [("rKT", h, j // 4), ("rQT", h, q)], [("ps", sb_)])
            for r in range(r0, 4):
                i_slot = 4 * q + r
                dstc = sc_buf[sb_][:, r * 128:(r + 1) * 128]
                src = sT[:, r * 128:(r + 1) * 128]
                if ms is not None and r == ms:
                    mk = rm[:, 1 if j % 2 == 0 else 2, h, :]
                    OP("dve", "tensor_tensor", [("ps", sb_), "rm"], [("sc", sb_)], out=dstc, in0=src, in1=mk, op=ALU.mult)
                else:
                    fac = gam[h] ** (128.0 * (2 * i_slot - j))
                    mk = rm[:, 0, h, :]
                    OP("dve", "scalar_tensor_tensor", [("ps", sb_), "rm"], [("sc", sb_)],
                       out=dstc, in0=src, scalar=fac, in1=mk, op0=ALU.mult, op1=ALU.mult)

        def ret_s2(n):
            u, h, q, j, jmax = iters[n]
            r0, ms = sb_info(q, j)
            sb_ = n % 2
            Ob = 2 + u % 2
            for r in range(r0, 4):
                first = (j == jmax)
                mm(bank(Ob)[:, r * 128:(r + 1) * 128], sc_buf[sb_][:, r * 128:(r + 1) * 128],
                   rV[:, j, h * 128:(h + 1) * 128], first, j == 0, [("sc", sb_), ("rV", j, h // 4)], [("ps", Ob)])
            if j == 0:
                for r in range(4):
                    o = bank(Ob)[:, r * 128:(r + 1) * 128]
                    OP("dve", "bn_stats", [("ps", Ob)], [("st6", r)], out=st6[:, r, :], in_=o)
                    OP("dve", "bn_aggr", [("st6", r)], [("mv",)], out=mv[:, r, :], in_=st6[:, r, :])
                OP("act", "activation", [("mv",)], [("rstd",)], out=rstd, in_=mv[:, :, 1], func=AF.Ln, bias=GN_EPS,
                   scale=1.0)
                OP("act", "activation", [("rstd",)], [("rstd",)], out=rstd, in_=rstd, func=AF.Exp, scale=-0.5)
                OP("dve", "scalar_tensor_tensor", [("mv",), ("rstd",)], [("nmr",)], out=nmr, in0=mv[:, :, 0], scalar=-1.0,
                   in1=rstd, op0=ALU.mult, op1=ALU.mult)
                for r in range(4):
                    i_slot = 4 * q + r
                    o = bank(Ob)[:, r * 128:(r + 1) * 128]
                    k2 = (u * 4 + r) % 2
                    OP("act", "activation", [("ps", Ob), ("nmr",), ("rstd",)], [("nrm", k2)], out=nrm[k2], in_=o,
                       func=AF.Identity, bias=nmr[:, r:r + 1], scale=rstd[:, r:r + 1])
                    OP("pool", "tensor_tensor", [("nrm", k2), "gg"], [("nrm2", k2)], out=nrm2[k2], in0=nrm[k2],
                       in1=gg[:, h * 128:(h + 1) * 128], op=ALU.mult)
                    OP("pool", "tensor_tensor", [("nrm2", k2), ("G", i_slot, h // 4)], [("rout", k2)], out=rout[k2],
                       in0=nrm2[k2], in1=G[:, i_slot, h * 128:(h + 1) * 128], op=ALU.mult)
                    OP("pe", "transpose", [("rout", k2), "cstb"], [("psT",)], out=psT[:, r * 128:(r + 1) * 128],
                       in_=rout[k2], identity=ident_b)
                evac_copy(mixT[:, 8 + h, q * 512:(q + 1) * 512], psT[:, 0:512], [("psT",)], [("mixT", 8 + h, q)])

        for step in range(NIT + 1):
            if step < NIT:
                ret_s1(step)
            if 0 <= step - 1 < NIT:
                ret_s2(step - 1)

        if stage == "attn":
            return _finish_debug(nc, S, A, y, mixT, 16)

        S.barrier()
        A.release(m_phaseA)
        yacc = A.f32(8, D)
        x1b = A.bf16(8, D)
        m_moe = A.mark()
        wob = [A.bf16(16, 512) for _ in range(2)]
        lng = A.f32(2, D)
        S.dma("sp", "lng", [], ["lng"], out=lng[:, 0, :], in_=vecs[0, :].partition_broadcast(128))
        S.dma("sp", "lng", [], ["lng"], out=lng[:, 1, :], in_=vecs[1, :].partition_broadcast(128))
        S.dma("sp", "xown", [], [("h1", s_) for s_ in range(8)], out=yacc, in_=x_own.rearrange("(s p) f -> p s f", p=128))
        for cp in range(4):
            wb, wk = load_piece(wob, w_out[:, cp * 512:(cp + 1) * 512], "wo")
            for s_ in range(8):
                (b,) = next_banks(1)
                for k in range(16):
                    mm(bank(b), mixT[:, k, s_ * 128:(s_ + 1) * 128], wb[:, k, :], k == 0, k == 15,
                       [wk, ("mixT", k, s_ // 4)], [("ps", b)])
                dst = yacc[:, s_, cp * 512:(cp + 1) * 512]
                OP("dve", "scalar_tensor_tensor", [("ps", b), ("h1", s_)], [("h1", s_)],
                   out=dst, in0=dst, scalar=ALPHA, in1=bank(b), op0=ALU.mult, op1=ALU.add)

        lst = A.f32(8, 24)
        lmv = A.f32(8, 2)
        lrs = A.f32(8)
        lnm = A.f32(8)

        def layer_norm(s_, lng, lst, lmv, lrs, lnm):
            xs = yacc[:, s_, :]
            for c in range(4):
                OP("dve", "bn_stats", [("h1", s_)], [("lst", s_)], out=lst[:, s_, c * 6:(c + 1) * 6],
                   in_=xs[:, c * 512:(c + 1) * 512])
            OP("dve", "bn_aggr", [("lst", s_)], [("lmv", s_)], out=lmv[:, s_, :], in_=lst[:, s_, :])
            OP("act", "activation", [("lmv", s_)], [("lrs", s_)], out=lrs[:, s_:s_ + 1], in_=lmv[:, s_, 1:2],
               func=AF.Ln, bias=LN_EPS, scale=1.0)
            OP("act", "activation", [("lrs", s_)], [("lrs", s_)], out=lrs[:, s_:s_ + 1], in_=lrs[:, s_:s_ + 1],
               func=AF.Exp, scale=-0.5)
            OP("dve", "scalar_tensor_tensor", [("lmv", s_), ("lrs", s_)], [("lnm", s_)], out=lnm[:, s_:s_ + 1],
               in0=lmv[:, s_, 0:1], scalar=-1.0, in1=lrs[:, s_:s_ + 1], op0=ALU.mult, op1=ALU.mult)
            OP("act", "activation", [("h1", s_), ("lnm", s_), ("lrs", s_)], [("h1", s_)], out=xs, in_=xs,
               func=AF.Identity, bias=lnm[:, s_:s_ + 1], scale=lrs[:, s_:s_ + 1])
            OP("pool", "tensor_tensor", [("h1", s_), "lng"], [("h1", s_)], out=xs, in0=xs, in1=lng[:, 0, :], op=ALU.mult)
            OP("pool", "tensor_tensor", [("h1", s_), "lng"], [("h1", s_)], out=xs, in0=xs, in1=lng[:, 1, :], op=ALU.add)

        for s_ in range(8):
            layer_norm(s_, lng, lst, lmv, lrs, lnm)
            OP("act", "activation", [("h1", s_)], [("x1b", s_)], out=x1b[:, s_, :], in_=yacc[:, s_, :], func=AF.Copy)

        if stage == "x1":
            S.dma("sp", "yout", [("h1", s_) for s_ in range(8)], ["y"], out=y.rearrange("(s p) f -> p s f", p=128),
                  in_=yacc)
            S.wait_all("sp", ["y"])
            S.emit()
            return nc

        S.barrier()
        A.release(m_moe)
        gates = A.f32(8, NE)
        posm = A.f32(8, NE)
        m_moe2 = A.mark()
        wr = A.f32(16, NE)
        br = A.f32(NE)
        onesrow = A.f32(128)
        bdn = A.f32(D)
        x1T = A.f32(16, 128)
        lg = A.f32(NE)
        mx8 = A.f32(8)
        msk = A.f32(8, NE)
        mskb = A.bf16(8, NE)
        msum = A.f32(NE)
        msumb = A.bf16(NE)
        ex = A.f32(NE)
        ssum = A.f32(1)
        nmx = A.f32(1)
        gT = A.f32(128)
        S.dma("sp", "wr", [], ["wr"], out=wr, in_=w_router.rearrange("(k q) n -> q k n", q=128))
        S.dma("sp", "br", [], ["br"], out=br[0:1, :], in_=b_router)
        S.dma("sp", "bdn", [], ["bdn"], out=bdn[0:NE, :], in_=b_dn)
        OP("dve", "memset", [], ["onesrow"], ap=onesrow[0:1, :], constant=1.0)
        OP("dve", "memset", [], ["msum"], ap=msum, constant=0.0)
        for s_ in range(8):
            for g4 in range(4):
                for c in range(4):
                    k = g4 * 4 + c
                    OP("pe", "transpose", [("h1", s_), "cst"], [("ps", g4)], out=bank(g4)[:, c * 128:(c + 1) * 128],
                       in_=yacc[:, s_, k * 128:(k + 1) * 128], identity=ident_f)
                evac_copy(x1T[:, g4 * 4:(g4 + 1) * 4, :], bank(g4).rearrange("p (a b) -> p a b", a=4),
                          [("ps", g4)], [("x1T", g4)])
            lgp = bank(4)[:, 0:NE]
            for k in range(16):
                mm(lgp, x1T[:, k, :], wr[:, k, :], k == 0, False, [("x1T", k // 4), "wr"], [("ps", 4)])
            mm(lgp, onesrow[0:1, :], br[0:1, :], False, True, ["onesrow", "br"], [("ps", 4)])
            OP("dve", "tensor_copy", [("ps", 4)], ["lg"], out=lg, in_=lgp)
            OP("dve", "max", ["lg"], ["mx8"], out=mx8, in_=lg)
            OP("dve", "tensor_scalar", ["lg", "mx8"], [("msk", s_)], out=msk[:, s_, :], in0=lg, scalar1=mx8[:, 3:4],
               scalar2=None, op0=ALU.is_ge)
            OP("dve", "tensor_scalar", ["mx8"], ["nmx"], out=nmx, in0=mx8[:, 0:1], scalar1=-1.0, scalar2=None, op0=ALU.mult)
            OP("act", "activation", ["lg", "nmx"], ["ex"], out=ex, in_=lg, func=AF.Exp, bias=nmx, scale=1.0)
            OP("dve", "tensor_tensor", ["ex", ("msk", s_)], ["ex"], out=ex, in0=ex, in1=msk[:, s_, :], op=ALU.mult)
            OP("dve", "reduce_sum", ["ex"], ["ssum"], out=ssum, in_=ex, axis=mybir.AxisListType.X)
            OP("dve", "reciprocal", ["ssum"], ["ssum"], out=ssum, in_=ssum)
            OP("dve", "tensor_scalar", ["ex", "ssum"], [("gates", s_)], out=gates[:, s_, :], in0=ex, scalar1=ssum[:, 0:1],
               scalar2=None, op0=ALU.mult)
            OP("dve", "tensor_copy", [("msk", s_)], [("mskb", s_)], out=mskb[:, s_, :], in_=msk[:, s_, :])
            OP("dve", "tensor_copy", ["msum"], ["msumb"], out=msumb, in_=msum)
            pp = bank(5)[:, 0:NE]
            mm(pp, triL_b, mskb[:, s_, :], True, False, [("mskb", s_), "cstb"], [("ps", 5)])
            mm(pp, ones_b, msumb, False, True, ["msumb", "cstb"], [("ps", 5)])
            OP("dve", "scalar_tensor_tensor", [("ps", 5), ("msk", s_)], [("posm", s_)], out=posm[:, s_, :], in0=pp,
               scalar=1.0, in1=msk[:, s_, :], op0=ALU.add, op1=ALU.mult)
            OP("dve", "tensor_scalar", [("posm", s_)], [("posm", s_)], out=posm[:, s_, :], in0=posm[:, s_, :],
               scalar1=-1.0, scalar2=None, op0=ALU.add)
            OP("dve", "tensor_tensor", ["msum", ("msk", s_)], ["msum"], out=msum, in0=msum, in1=msk[:, s_, :], op=ALU.add)
            OP("pe", "transpose", [("gates", s_), "cst"], [("ps", 6)], out=bank(6)[0:NE, 0:128], in_=gates[:, s_, :],
               identity=ident_f)
            OP("act", "activation", [("ps", 6)], ["gT"], out=gT[0:NE, :], in_=bank(6)[0:NE, 0:128], func=AF.Copy)
            for cp in range(4):
                bb = bank(7)
                mm(bb, gT[0:NE, :], bdn[0:NE, cp * 512:(cp + 1) * 512], True, True, ["gT", "bdn"], [("ps", 7)])
                dst = yacc[:, s_, cp * 512:(cp + 1) * 512]
                OP("dve", "scalar_tensor_tensor", [("ps", 7), ("h1", s_)], [("h1", s_)], out=dst, in0=dst, scalar=ALPHA,
                   in1=bb, op0=ALU.mult, op1=ALU.add)

        if stage == "router":
            S.barrier()
            yv = y.rearrange("(s p) f -> p s f", p=128)
            S.dma("sp", "yout", [], ["y"], out=yv[:, :, 0:NE], in_=gates)
            S.dma("sp", "yout", [], ["y"], out=yv[:, :, NE:2 * NE], in_=posm)
            S.dma("sp", "yout", [], ["y"], out=yv[:, :, 64:64 + 1024], in_=yacc[:, :, 0:1024])
            S.wait_all("sp", ["y"])
            S.emit()
            return nc

        S.barrier()
        A.release(m_moe2)
        wmb = [mix_raw[:, i * 4096:(i + 1) * 4096].bitcast(BF16).rearrange("p (a b) -> p a b", a=16) for i in range(2)]
        wmb.append(A.bf16(16, 512))
        sel = [A.bf16(8, CAP) for _ in range(2)]
        selT = [A.bf16(2, 1024) for _ in range(2)]
        XeT = [A.bf16(16, CAP) for _ in range(2)]
        actT = A.bf16(16, CAP)
        gsb = A.f32(4, CAP)
        utmp = [A.f32(CAP) for _ in range(2)]
        gtmp = utmp
        yrows = A.bf16(2, D)
        bgu = [A.f32(32) for _ in range(2)]
        psT7 = ps[:, 7 * 512: 8 * 512].bitcast(BF16)

        hb_n = [0]
        sc_n = [0]
        bgu1 = [A.f32(16) for _ in range(2)]

        def moe_front(ex_):
            eb = ex_ % 2
            S.dma("sp", f"bgu{eb}", [], [("bgu", eb)], out=bgu[eb], in_=b_gu[ex_])
            OP("dve", "tensor_scalar", [("bgu", eb)], [("bgu1", eb)], out=bgu1[eb], in0=bgu[eb][:, 16:32], scalar1=1.0,
               scalar2=None, op0=ALU.add)
            for s_ in range(8):
                OP("dve", "tensor_scalar", [("posm", s_), "cst"], [("sel", eb, s_)], out=sel[eb][:, s_, :], in0=iota_f[:, 0:CAP],
                   scalar1=posm[:, s_, ex_:ex_ + 1], scalar2=None, op0=ALU.is_equal)
            for rc, (r0_, rn) in enumerate(RCH):
                for s_ in range(8):
                    OP("pe", "transpose", [("sel", eb, s_), "cstb"], [("ps", 7)], out=psT7[0:rn, s_ * 128:(s_ + 1) * 128],
                       in_=sel[eb][:, s_, r0_:r0_ + rn], identity=ident_b)
                evac_copy(selT[eb][0:rn, rc, :], psT7[0:rn, :], [("ps", 7)], [("selT", eb, rc)], eng="act")
            for c in range(16):
                gb = (0, 7)[c % 2]
                for s_ in range(8):
                    mm(bank(gb, CAP), x1b[:, s_, c * 128:(c + 1) * 128], sel[eb][:, s_, :], s_ == 0, s_ == 7,
                       [("x1b", s_), ("sel", eb, s_)], [("ps", gb)])
                evac_copy(XeT[eb][:, c, :], bank(gb, CAP), [("ps", gb)], [("XeT", eb, c)], eng="act")

        def moe_hidden(ex_):
            eb = ex_ % 2
            for p4 in range(4):
                for gu in range(2):
                    wb, wk = load_piece(wmb, w_gu[ex_, p4 * 2 + gu], "wm", tiled=True)
                    for cc in range(4):
                        hbi = hb_n[0] % 2
                        hb_n[0] += 1
                        hbk = ("ps", 1 + hbi)
                        hp_ = bank(1 + hbi, CAP)
                        for k in range(16):
                            mm(hp_, wb[:, k, cc * 128:(cc + 1) * 128], XeT[eb][:, k, :], k == 0, k == 15,
                               [wk, ("XeT", eb, k)], [hbk])
                        fch = p4 * 4 + cc
                        if gu == 0:
                            OP("dve", "tensor_scalar", [hbk, ("bgu", eb)], [("gtmp", hbi)], out=gtmp[hbi], in0=hp_,
                               scalar1=bgu[eb][:, fch:fch + 1], scalar2=7.0, op0=ALU.add, op1=ALU.min)
                            OP("act", "activation", [("gtmp", hbi)], [("gsb", cc)], out=gsb[:, cc, :], in_=gtmp[hbi],
                               func=AF.Gelu_apprx_sigmoid)
                        else:
                            OP("dve", "tensor_scalar", [hbk, ("bgu1", eb)], [("utmp", hbi)], out=utmp[hbi], in0=hp_,
                               scalar1=bgu1[eb][:, fch:fch + 1], scalar2=8.0, op0=ALU.add, op1=ALU.min)
                            OP("dve", "scalar_tensor_tensor", [("utmp", hbi), ("gsb", cc)], [("actT", fch)],
                               out=actT[:, fch, :], in0=utmp[hbi], scalar=-6.0, in1=gsb[:, cc, :], op0=ALU.max, op1=ALU.mult)

        def moe_down(ex_):
            eb = ex_ % 2
            for cp in range(4):
                wb, wk = load_piece(wmb, w_dn[ex_, cp], "wm", tiled=True)
                for rc, (r0_, rn) in enumerate(RCH):
                    yb = 3 + rc
                    for k in range(16):
                        mm(bank(yb)[0:rn, :], actT[:, k, r0_:r0_ + rn], wb[:, k, :], k == 0, k == 15,
                           [wk, ("actT", k)], [("ps", yb)])
                    evac_copy(yrows[0:rn, rc, cp * 512:(cp + 1) * 512], bank(yb)[0:rn, :], [("ps", yb)],
                              [("yrows", rc, cp)], eng="act")
                for s_ in range(8):
                    sb_ = 5 + sc_n[0] % 2
                    sc_n[0] += 1
                    for rc, (r0_, rn) in enumerate(RCH):
                        mm(bank(sb_), selT[eb][0:rn, rc, s_ * 128:(s_ + 1) * 128], yrows[0:rn, rc, cp * 512:(cp + 1) * 512],
                           rc == 0, rc == 1, [("selT", eb, rc), ("yrows", rc, cp)], [("ps", sb_)])
                    dst = yacc[:, s_, cp * 512:(cp + 1) * 512]
                    OP("dve", "scalar_tensor_tensor", [("ps", sb_), ("gates", s_), ("h1", s_, cp)], [("h1", s_, cp)],
                       out=dst, in0=bank(sb_), scalar=gates[:, s_, ex_:ex_ + 1], in1=dst, op0=ALU.mult, op1=ALU.add)

        moe_front(0)
        for ex_ in range(ne_run):
            moe_hidden(ex_)
            if ex_ + 1 < ne_run:
                moe_front(ex_ + 1)
            moe_down(ex_)

        S.barrier()
        A.release(m_moe2)
        lng2 = A.f32(2, D)
        lst2 = A.f32(8, 24)
        lmv2 = A.f32(8, 2)
        lrs2 = A.f32(8)
        lnm2 = A.f32(8)
        S.dma("sp", "lng2", [], ["lng"], out=lng2[:, 0, :], in_=vecs[2, :].partition_broadcast(128))
        S.dma("sp", "lng2", [], ["lng"], out=lng2[:, 1, :], in_=vecs[3, :].partition_broadcast(128))
        yv = y.rearrange("(s p) f -> p s f", p=128)
        for s_ in range(8):
            layer_norm(s_, lng2, lst2, lmv2, lrs2, lnm2)
            S.dma("sp", "yout", [("h1", s_)], [("y", s_)], out=yv[:, s_, :], in_=yacc[:, s_, :])
        S.wait_all("sp", [("y", s_) for s_ in range(8)])
        S.emit()
    return nc


def _finish_debug(nc, S, A, y, mixT, nchunks):
    S.barrier()
    tmp = [A.f32(1024) for _ in range(2)]
    yv = y.rearrange("r (two t) -> (r two) t", two=2).rearrange("(c p) t -> p c t", p=128)
    for c in range(nchunks):
        S.op("dve", "tensor_copy", [], [("tmp", c % 2)], out=tmp[c % 2], in_=mixT[:, c, :])
        S.dma("sp", "dbg", [("tmp", c % 2)], ["ydbg"], out=yv[:, c, :], in_=tmp[c % 2])
    S.wait_all("sp", ["ydbg"])
    S.emit()
    return nc


def _constants(hp):
    p = np.arange(128)
    ident = np.eye(128, dtype=np.float32)
    triU = (p[:, None] > p[None, :]).astype(np.float32)
    triL = (p[:, None] < p[None, :]).astype(np.float32)
    ones = np.ones((128, 128), np.float32)
    strict = (p[:, None] < p[None, :]).astype(np.float32)
    if hp == 0:
        mA, mB = strict, np.zeros((128, 128), np.float32)
    else:
        mA, mB = ones, strict
    iota = np.broadcast_to(np.arange(256, dtype=np.float32)[None, :], (128, 256))
    cst = np.concatenate([ident, triU, triL, ones, mA, mB, iota], axis=1).astype(np.float32)
    sl = p[:, None].astype(np.float64)
    tl = p[None, :].astype(np.float64)
    rm = np.zeros((128, 3, NH, 128), np.float64)
    for h in range(NH):
        lg = math.log1p(-2.0 ** (-5.0 - h))
        gen = np.exp(lg * (128.0 * hp + tl - sl))
        diag = np.where((p[:, None] // 64) <= (p[None, :] // 64), np.exp(lg * np.abs(tl - sl)), 0.0)
        rm[:, 0, h, :] = gen
        if hp == 0:
            rm[:, 1, h, :] = diag
            rm[:, 2, h, :] = 0.0
        else:
            rm[:, 1, h, :] = np.exp(lg * (128.0 + tl - sl))
            rm[:, 2, h, :] = diag
    rm *= HD ** -0.5
    return cst, rm.reshape(128, 3 * NH * 128).astype(np.float32)


def _rot_tables(hp):
    inv = 10000.0 ** (-np.arange(0, HD, 2, dtype=np.float32) / HD)
    own = np.concatenate([np.arange(128) + (2 * i + hp) * 128 for i in range(8)])
    out = np.zeros((3, 2, 128, 1024), np.float32)
    for pi, pos in enumerate([np.arange(0, 1024), np.arange(1024, 2048), own]):
        ang = pos.astype(np.float32)[None, :] * inv[:, None]
        c, s = np.cos(ang), np.sin(ang)
        out[pi, 0] = np.concatenate([c, c], axis=0)
        out[pi, 1] = np.concatenate([-s, s], axis=0)
    return out


def _w_ext(w_in):
    return np.ascontiguousarray(w_in)


_PROG = {}


def _get_prog(stage="full", ne_run=NE):
    if (stage, ne_run) not in _PROG:
        _PROG[(stage, ne_run)] = build_program(stage, ne_run)
    return _PROG[(stage, ne_run)]


def make_in_maps(x, w_in, ret_gn_gain, w_out, ln1_gain, ln1_bias, w_router, b_router,
                 w_gate_up, b_gate_up, w_down, b_down, ln2_gain, ln2_bias, stage="full", ne_run=NE):
    f = lambda a: np.ascontiguousarray(np.asarray(a, dtype=np.float32))
    x = f(x)
    wext = _w_ext(f(w_in)[0])
    vecs = np.zeros((5, D), np.float32)
    vecs[0], vecs[1], vecs[2], vecs[3] = f(ln1_gain)[0], f(ln1_bias)[0], f(ln2_gain)[0], f(ln2_bias)[0]
    vecs[4, :1024] = f(ret_gn_gain)[0]
    shared = {"w_ext": wext, "w_out": f(w_out)[0], "vecs": vecs}
    if stage in ("full", "router"):
        shared.update({
            "w_router": f(w_router)[0], "b_router": f(b_router)[0].reshape(1, NE), "b_dn": f(b_down)[0],
            "b_gu": np.ascontiguousarray(f(b_gate_up)[0].reshape(NE, 32, 128).transpose(0, 2, 1)),
        })
    if stage == "full":
        wg = f(w_gate_up)[0][:ne_run].reshape(ne_run, 16, 128, 2, 4, 512)
        wd = f(w_down)[0][:ne_run].reshape(ne_run, 16, 128, 4, 512)
        shared.update({
            "w_gu": np.ascontiguousarray(wg.transpose(0, 4, 3, 2, 1, 5)).reshape(ne_run, 8, 128, 16, 512),
            "w_dn": np.ascontiguousarray(wd.transpose(0, 3, 2, 1, 4)),
        })
    consts = [(_constants(hp), _rot_tables(hp)) for hp in range(2)]
    in_maps = []
    for c in range(8):
        b, hp = c // 2, c % 2
        xb = x[b]
        own = np.concatenate([np.arange(128) + (2 * i + hp) * 128 for i in range(8)])
        xo = np.ascontiguousarray(xb[own])
        xT3 = np.stack([np.ascontiguousarray(xb[0:1024].T), np.ascontiguousarray(xb[1024:2048].T),
                        np.ascontiguousarray(xo.T)])
        (cst, rm), rot = consts[hp]
        m = dict(shared)
        m.update({"xT3": xT3, "x_own": xo, "cst": cst, "rmask": rm, "rot": rot})
        in_maps.append(m)
    return in_maps


def kernel(x, w_in, ret_gn_gain, w_out, ln1_gain, ln1_bias, w_router, b_router,
           w_gate_up, b_gate_up, w_down, b_down, ln2_gain, ln2_bias):
    in_maps = make_in_maps(x, w_in, ret_gn_gain, w_out, ln1_gain, ln1_bias, w_router, b_router,
                           w_gate_up, b_gate_up, w_down, b_down, ln2_gain, ln2_bias)
    nc = _get_prog("full")
    res = run_bass_kernel_spmd(nc, in_maps, core_ids=list(range(8)))
    out = np.zeros((NB, SEQ, D), np.float32)
    for c in range(8):
        b, hp = c // 2, c % 2
        yc = res.results[c]["y"]
        for i in range(8):
            blk = 2 * i + hp
            out[b, blk * 128:(blk + 1) * 128] = yc[i * 128:(i + 1) * 128]
    return out
```

```python
import math
from contextlib import ExitStack

import numpy as np
import concourse.bass as bass
import concourse.mybir as mybir
from concourse.bass_utils import run_bass_kernel_spmd

F32 = mybir.dt.float32
BF16 = mybir.dt.bfloat16
AF = mybir.ActivationFunctionType
ALU = mybir.AluOpType

D = 2048
SEQ = 2048
NB = 4
HD = 128
NH = 8
NE = 32
CAP = 192
RCH = ((0, 128), (128, 64))
ALPHA = 2.0 ** 0.25
LN_EPS = 1e-5
GN_EPS = 1e-5
QSCALE = 1.0 / math.sqrt(HD)
WEXT = 7168
EPOCH = 2000

C_SBQ, C_SBK, C_SBV, C_RQX, C_RKX, C_RV, C_RG = 0, 1024, 2048, 3072, 4096, 5120, 6144


class Sched:
    ENGS = ("pe", "act", "dve", "pool", "sp")

    def __init__(self, nc):
        self.nc = nc
        self.ops = {e: [] for e in self.ENGS}
        self.count = {e: 0 for e in self.ENGS}
        self.known = {e: {} for e in self.ENGS}
        self.last_w = {}
        self.readers = {}
        self.dma_slots = {}
        self.n_dma_sems = 0

    def _need(self, eng, key, val, waits):
        if key == ("eng", "pe") and eng == "pe":
            return
        if self.known[eng].get(key, 0) >= val:
            return
        self.known[eng][key] = val
        waits[key] = max(waits.get(key, 0), val)

    def _deps(self, eng, reads, writes):
        waits = {}
        for r in reads:
            t = self.last_w.get(r)
            if t is not None:
                self._need(eng, t[0], t[1], waits)
        for w in writes:
            t = self.last_w.get(w)
            if t is not None:
                self._need(eng, t[0], t[1], waits)
            for k, v in self.readers.get(w, {}).items():
                self._need(eng, k, v, waits)
        return list(waits.items())

    def _commit(self, tok, reads, writes):
        for r in reads:
            d = self.readers.setdefault(r, {})
            d[tok[0]] = max(d.get(tok[0], 0), tok[1])
        for w in writes:
            self.last_w[w] = tok
            self.readers[w] = {}

    def op(self, eng, method, reads=(), writes=(), **kw):
        fn = (method, kw)
        waits = self._deps(eng, reads, writes)
        idx = self.count[eng]
        self.count[eng] += 1
        tok = (("eng", eng), idx + 1)
        self.ops[eng].append((waits, fn, ("eng", idx)))
        self._commit(tok, reads, writes)

    def dma(self, eng, slot, reads=(), writes=(), **kw):
        fn = ("dma_start", kw)
        waits = self._deps(eng, reads, writes)
        if slot not in self.dma_slots:
            self.dma_slots[slot] = [self.n_dma_sems, 0]
            self.n_dma_sems += 1
        self.dma_slots[slot][1] += 16
        tok = (("dma", slot), self.dma_slots[slot][1])
        self.ops[eng].append((waits, fn, ("dma", slot)))
        self._commit(tok, reads, writes)

    def wait_all(self, eng, regions):
        waits = self._deps(eng, regions, ())
        self.ops[eng].append((waits, None, None))

    def barrier(self):
        toks = [(("eng", e), self.count[e]) for e in self.ENGS if self.count[e] > 0]
        toks += [(("dma", s), v[1]) for s, v in self.dma_slots.items()]
        for e in self.ENGS:
            waits = {}
            for k, v in toks:
                self._need(e, k, v, waits)
            self.ops[e].append((list(waits.items()), None, None))
        self.last_w.clear()
        self.readers.clear()

    def emit(self):
        nc = self.nc
        with ExitStack() as st:
            eng_sems = {}
            for e in self.ENGS:
                n_ep = self.count[e] // EPOCH + 1
                eng_sems[e] = [st.enter_context(nc.semaphore(f"s_{e}_{i}")) for i in range(n_ep)]
            dma_sems = [st.enter_context(nc.semaphore(f"s_dma_{i}")) for i in range(self.n_dma_sems)]
            block = st.enter_context(nc.Block())

            def run(ename):
                def body(eng):
                    for waits, fn, inc in self.ops[ename]:
                        for key, val in waits:
                            if key[0] == "eng":
                                idx = val - 1
                                eng.wait_ge(eng_sems[key[1]][idx // EPOCH], idx % EPOCH + 1)
                            else:
                                eng.wait_ge(dma_sems[self.dma_slots[key[1]][0]], val)
                        if fn is None:
                            continue
                        ins = getattr(eng, fn[0])(**fn[1])
                        if inc[0] == "eng":
                            idx = inc[1]
                            ins.then_inc(eng_sems[ename][idx // EPOCH], 1)
                        else:
                            ins.then_inc(dma_sems[self.dma_slots[inc[1]][0]], 16)
                return body

            block.tensor(run("pe"))
            block.scalar(run("act"))
            block.vector(run("dve"))
            block.gpsimd(run("pool"))
            block.sync(run("sp"))


class Arena:
    def __init__(self, t, words):
        self.t = t
        self.words = words
        self.off = 0

    def mark(self):
        return self.off

    def release(self, m):
        self.off = m

    def _take(self, nwords):
        a = self.off
        self.off += nwords
        assert self.off <= self.words, f"SBUF arena overflow {self.off} > {self.words}"
        return self.t[:, a:a + nwords]

    def f32(self, *dims):
        n = int(np.prod(dims))
        v = self._take(n)
        return self._shape(v, dims)

    def bf16(self, *dims):
        n = int(np.prod(dims))
        assert n % 2 == 0
        v = self._take(n // 2).bitcast(BF16)
        return self._shape(v, dims)

    @staticmethod
    def _shape(v, dims):
        if len(dims) == 1:
            return v
        if len(dims) == 2:
            return v.rearrange("p (a b) -> p a b", a=dims[0])
        if len(dims) == 3:
            return v.rearrange("p (a b c) -> p a b c", a=dims[0], b=dims[1])
        raise ValueError(dims)


def build_program(stage="full", ne_run=NE):
    nc = bass.Bass("TRN2", target_bir_lowering=False)
    dt = nc.dram_tensor
    with_moe = stage in ("full", "router")
    xT3 = dt("xT3", [3, D, 1024], F32, kind="ExternalInput").ap()
    x_own = dt("x_own", [1024, D], F32, kind="ExternalInput").ap()
    w_ext = dt("w_ext", [D, WEXT], F32, kind="ExternalInput").ap()
    w_out = dt("w_out", [D, D], F32, kind="ExternalInput").ap()
    vecs = dt("vecs", [5, D], F32, kind="ExternalInput").ap()
    cst = dt("cst", [128, 1024], F32, kind="ExternalInput").ap()
    rmask = dt("rmask", [128, 3 * NH * 128], F32, kind="ExternalInput").ap()
    rot = dt("rot", [3, 2, 128, 1024], F32, kind="ExternalInput").ap()
    if with_moe:
        w_router = dt("w_router", [D, NE], F32, kind="ExternalInput").ap()
        b_router = dt("b_router", [1, NE], F32, kind="ExternalInput").ap()
        b_gu = dt("b_gu", [NE, 128, 32], F32, kind="ExternalInput").ap()
        if stage == "full":
            w_gu = dt("w_gu", [ne_run, 8, 128, 16, 512], F32, kind="ExternalInput").ap()
            w_dn = dt("w_dn", [ne_run, 4, 128, 16, 512], F32, kind="ExternalInput").ap()
        b_dn = dt("b_dn", [NE, D], F32, kind="ExternalInput").ap()
    y = dt("y", [1024, D], F32, kind="ExternalOutput").ap()

    AW = 53000
    with ExitStack() as st:
        arena_t = st.enter_context(nc.sbuf_tensor("arena", [128, AW], F32))
        ps = st.enter_context(nc.psum_tensor("ps", [128, 4096], F32))
        A = Arena(arena_t, AW)
        S = Sched(nc)
        OP = S.op

        def bank(b, n=512, off=0):
            return ps[:, b * 512 + off: b * 512 + off + n]

        def mm(out, lhsT, rhs, start, stop, reads, writes):
            OP("pe", "matmul", reads, writes, out=out, lhsT=lhsT, rhs=rhs, start=start, stop=stop)

        evac_rr = [0]

        def evac_copy(dst, src, reads, writes, eng=None):
            if eng is None:
                eng = ("act", "dve")[evac_rr[0] % 2]
                evac_rr[0] += 1
            if eng == "act":
                OP("act", "activation", reads, writes, out=dst, in_=src, func=AF.Copy)
            else:
                OP("dve", "tensor_copy", reads, writes, out=dst, in_=src)

        cst_f = A.f32(1024)
        ident_f = cst_f[:, 0:128]
        maskA = cst_f[:, 512:640]
        maskB = cst_f[:, 640:768]
        iota_f = cst_f[:, 768:1024]
        cst_b = A.bf16(4, 128)
        ident_b, triU_b, triL_b, ones_b = (cst_b[:, i, :] for i in range(4))
        S.dma("sp", "cst", [], ["cst"], out=cst_f, in_=cst)
        OP("dve", "tensor_copy", ["cst"], ["cstb"], out=cst_b,
           in_=cst_f[:, 0:512].rearrange("p (a b) -> p a b", a=4))
        swp_b = A.bf16(128)
        OP("dve", "tensor_copy", ["cstb"], ["swp"], out=swp_b[:, 0:64], in_=ident_b[:, 64:128])
        OP("dve", "tensor_copy", ["cstb"], ["swp"], out=swp_b[:, 64:128], in_=ident_b[:, 0:64])
        mb_b = A.bf16(2, 128)
        OP("dve", "tensor_scalar", ["cst"], ["mbb"], out=mb_b, in0=cst_f[:, 512:768].rearrange("p (a b) -> p a b", a=2),
           scalar1=-1.0, scalar2=1.0e4, op0=ALU.add, op1=ALU.mult)
        mix_raw = A._take(8192)
        mixT = mix_raw.bitcast(BF16).rearrange("p (a b) -> p a b", a=16)
        mix_hi_f32 = mix_raw[:, 4096:8192]
        m_phaseA = A.mark()

        wp_n = [0]

        def load_piece(wbufs, src_ap, tag, tiled=False):
            i = wp_n[0] % len(wbufs)
            wp_n[0] += 1
            key = (tag, i)
            S.dma("pool", f"{tag}{i}", [], [key], out=wbufs[i],
                  in_=src_ap if tiled else src_ap.rearrange("(k q) c -> q k c", q=128))
            return wbufs[i], key

        def load_xT(xT, p):
            v = xT3[p].rearrange("(k q) t -> q k t", q=128)
            for kq in range(4):
                S.dma("pool", f"xT{kq}", [], [("xT", kq)], out=xT[:, kq * 4:(kq + 1) * 4, :], in_=v[:, kq * 4:(kq + 1) * 4, :])

        def run_proj(tasks, xT, wbufs, tag, on_pass=None):
            loaded = {}
            loaded[0] = load_piece(wbufs, tasks[0][1], tag)
            cur = None
            for i, (p, src_ap, fn) in enumerate(tasks):
                if p != cur:
                    load_xT(xT, p)
                    if on_pass is not None:
                        on_pass(p)
                    cur = p
                if i + 1 < len(tasks):
                    loaded[i + 1] = load_piece(wbufs, tasks[i + 1][1], tag)
                fn(*loaded.pop(i))

        pbank = [0]

        def next_banks(n):
            b = pbank[0]
            pbank[0] = (pbank[0] + n) % 4
            return [b + i for i in range(n)]

        def proj_feature_major(wbuf, wkey, col, xT, dst_fn, dkey_fn):
            bA, bB = next_banks(2)
            for k in range(16):
                for hf, b in ((0, bA), (1, bB)):
                    mm(bank(b), wbuf[:, k, col:col + 128], xT[:, k, hf * 512:(hf + 1) * 512],
                       k == 0, k == 15, [wkey, ("xT", k // 4)], [("ps", b)])
            for hf, b in ((0, bA), (1, bB)):
                evac_copy(dst_fn(hf), bank(b), [("ps", b)], [dkey_fn(hf)])

        def proj_token_major(wbuf, wkey, xT, tb, consume):
            (b,) = next_banks(1)
            for k in range(16):
                mm(bank(b), xT[:, k, tb * 128:(tb + 1) * 128], wbuf[:, k, :], k == 0, k == 15,
                   [wkey, ("xT", k // 4)], [("ps", b)])
            consume(b)

        KT = A.bf16(NH, SEQ)
        V = A.bf16(16, 1024)
        QT = A.bf16(NH, 1024)
        m_sbattn = A.mark()
        xT = A.bf16(16, 1024)
        wbufs = [A.bf16(16, 512) for _ in range(3)]

        def k_task(p, pc):
            def fn(wb, wk):
                for hh in range(4):
                    h = pc * 4 + hh
                    proj_feature_major(wb, wk, hh * 128, xT,
                                       lambda hf: KT[:, h, p * 1024 + hf * 512: p * 1024 + (hf + 1) * 512],
                                       lambda hf: ("KT", h, p * 2 + hf))
            return (p, w_ext[:, C_SBK + pc * 512: C_SBK + (pc + 1) * 512], fn)

        def v_task(p, pc):
            def fn(wb, wk):
                for tb in range(8):
                    blk = p * 8 + tb
                    proj_token_major(wb, wk, xT, tb,
                                     lambda b, blk=blk: evac_copy(V[:, blk, pc * 512:(pc + 1) * 512], bank(b),
                                                                  [("ps", b)], [("V", blk, pc)]))
            return (p, w_ext[:, C_SBV + pc * 512: C_SBV + (pc + 1) * 512], fn)

        def q_task(pc):
            def fn(wb, wk):
                for hh in range(4):
                    h = pc * 4 + hh
                    proj_feature_major(wb, wk, hh * 128, xT,
                                       lambda hf: QT[:, h, hf * 512:(hf + 1) * 512],
                                       lambda hf: ("QT", h, hf))
            return (2, w_ext[:, C_SBQ + pc * 512: C_SBQ + (pc + 1) * 512], fn)

        tasks = []
        for p in range(2):
            tasks += [k_task(p, 0), k_task(p, 1), v_task(p, 0), v_task(p, 1)]
        tasks += [q_task(0), q_task(1)]
        run_proj(tasks, xT, wbufs, "wp")

        S.barrier()
        A.release(m_sbattn)
        NS = 4
        e_buf = [A.f32(512) for _ in range(NS)]
        sp_buf = [A.bf16(512) for _ in range(NS)]
        t1_buf = [A.f32(512) for _ in range(NS)]
        w_buf = [A.bf16(512) for _ in range(NS)]
        acc_buf = [[A.bf16(512) for _ in range(2)] for _ in range(NS)]
        streams = []
        for s in range(NS):
            its = []
            for h in (s, s + 4):
                for q in range(2):
                    jmax = 8 * q + 7
                    for j in range(jmax, -1, -1):
                        its.append((h * 2 + q, h, q, j, jmax))
            streams.append(its)
        iters = []
        for h in range(NH):
            for q in range(2):
                for j in range(8 * q + 7, -1, -1):
                    iters.append((h * 2 + q, h, q, j, 8 * q + 7))

        def sb_info(q, j):
            r0 = max(0, j // 2 - 4 * q)
            ms = j // 2 - 4 * q
            return r0, (ms if ms >= 0 else None)

        def sb_parts(s, m):
            u, h, q, j, jmax = streams[s][m]
            r0, ms = sb_info(q, j)
            return u, h, q, j, jmax, r0 * 128, ms, jmax - j

        def sb_z(s, m):
            u, h, q, j, jmax, lo, ms, k = sb_parts(s, m)
            mm(bank(2 * s)[:, lo:512], KT[:, h, j * 128:(j + 1) * 128], QT[:, h, q * 512 + lo: (q + 1) * 512], True,
               ms is None, [("KT", h, j // 4), ("QT", h, q)], [("ps", 2 * s)])
            if ms is not None:
                mm(bank(2 * s)[:, ms * 128:(ms + 1) * 128], ident_b, mb_b[:, j % 2, :], False, True, ["mbb", "cstb"],
                   [("ps", 2 * s)])

        def sb_e(s, m):
            u, h, q, j, jmax, lo, ms, k = sb_parts(s, m)
            OP("act", "activation", [("ps", 2 * s)], [("e", s)], out=e_buf[s][:, lo:512], in_=bank(2 * s)[:, lo:512],
               func=AF.Exp, scale=QSCALE)

        def sb_sp(s, m):
            u, h, q, j, jmax, lo, ms, k = sb_parts(s, m)
            OP("act", "activation", [("e", s)], [("sp", s)], out=sp_buf[s][:, lo:512], in_=e_buf[s][:, lo:512],
               func=AF.Ln, bias=1.0, scale=1.0)

        def sb_t1(s, m):
            u, h, q, j, jmax, lo, ms, k = sb_parts(s, m)
            spb = sp_buf[s][:, lo:512]
            OP("dve", "scalar_tensor_tensor", [("ps", 2 * s), ("sp", s)], [("t1", s)],
               out=t1_buf[s][:, lo:512], in0=bank(2 * s)[:, lo:512], scalar=QSCALE, in1=spb, op0=ALU.mult, op1=ALU.subtract)
            ua = acc_buf[s]
            if j == jmax:
                for i2 in range(2):
                    OP("pool", "memset", [], [("acc", s, i2)], ap=ua[i2], constant=0.0)
            if j > 0:
                OP("pool", "tensor_tensor", [("acc", s, k % 2), ("sp", s)], [("acc", s, (k + 1) % 2)],
                   out=ua[(k + 1) % 2][:, lo:512], in0=ua[k % 2][:, lo:512], in1=spb, op=ALU.add)

        def sb_P(s, m):
            u, h, q, j, jmax, lo, ms, k = sb_parts(s, m)
            P = bank(2 * s)[:, lo:512]
            mm(P, triU_b, sp_buf[s][:, lo:512], True, False, [("sp", s), "cstb"], [("ps", 2 * s)])
            mm(P, ones_b, acc_buf[s][k % 2][:, lo:512], False, True, [("acc", s, k % 2), "cstb"], [("ps", 2 * s)])

        def sb_logw(s, m):
            u, h, q, j, jmax, lo, ms, k = sb_parts(s, m)
            t1b = t1_buf[s][:, lo:512]
            OP("dve", "tensor_tensor", [("t1", s), ("ps", 2 * s)], [("t1", s)], out=t1b, in0=t1b,
               in1=bank(2 * s)[:, lo:512], op=ALU.subtract)

        def sb_w(s, m):
            u, h, q, j, jmax, lo, ms, k = sb_parts(s, m)
            OP("act", "activation", [("t1", s)], [("w", s)], out=w_buf[s][:, lo:512], in_=t1_buf[s][:, lo:512],
               func=AF.Exp)

        def sb_av(s, m):
            u, h, q, j, jmax, lo, ms, k = sb_parts(s, m)
            Ob = 2 * s + 1
            mm(bank(Ob)[:, lo:512], V[:, j, h * 128:(h + 1) * 128], w_buf[s][:, lo:512], j == jmax, j == 0,
               [("V", j, h // 4), ("w", s)], [("ps", Ob)])
            if j == 0:
                evac_copy(mixT[:, h, q * 512:(q + 1) * 512], bank(Ob), [("ps", Ob)], [("mixT", h, q)])

        NIT = len(iters)
        nm = len(streams[0])
        grp = ((0, 1), (2, 3))
        for T in range(2 * nm + 1):
            F, mF = grp[T % 2], T // 2
            G, mG = grp[(T + 1) % 2], (T - 1) // 2
            for f1, f2 in ((sb_z, sb_P), (sb_e, sb_logw), (sb_sp, sb_w), (sb_t1, sb_av)):
                if mF < nm:
                    for s in F:
                        f1(s, mF)
                if T >= 1 and mG < nm:
                    for s in G:
                        f2(s, mG)

        if stage == "sb":
            return _finish_debug(nc, S, A, y, mixT, 8)

        S.barrier()
        A.release(m_phaseA)
        rKT = A.bf16(NH, SEQ)
        rV = A.bf16(16, 1024)
        rQT = A.bf16(NH, 1024)
        G = A.bf16(8, 1024)
        m_retattn = A.mark()
        xT = A.bf16(16, 1024)
        wbufs = [A.bf16(16, 512) for _ in range(2)]
        tab = mix_hi_f32[:, 0:2048].rearrange("p (a b) -> p a b", a=2)
        tA = [mix_hi_f32[:, 2048 + i * 512: 2048 + (i + 1) * 512] for i in range(2)]
        tB = [mix_hi_f32[:, 3072 + i * 512: 3072 + (i + 1) * 512] for i in range(2)]
        rr = [0]

        qb = [A.bf16(512) for _ in range(2)]

        def rot_proj(wb, wk, hh, xT, dst_fn, dkey_fn):
            for hf in range(2):
                bA, bB = next_banks(2)
                for k in range(16):
                    mm(bank(bA), wb[:, k, hh * 128:(hh + 1) * 128], xT[:, k, hf * 512:(hf + 1) * 512],
                       k == 0, k == 15, [wk, ("xT", k // 4)], [("ps", bA)])
                i = rr[0] % 2
                rr[0] += 1
                OP("act", "activation", [("ps", bA)], [("qb", i)], out=qb[i], in_=bank(bA), func=AF.Copy)
                mm(bank(bB), swp_b, qb[i], True, True, [("qb", i), "swp"], [("ps", bB)])
                cs, sn = tab[:, 0, hf * 512:(hf + 1) * 512], tab[:, 1, hf * 512:(hf + 1) * 512]
                OP("dve", "tensor_tensor", [("ps", bA), "tab", ("qb", i)], [("tA", i)], out=tA[i], in0=bank(bA), in1=cs,
                   op=ALU.mult)
                OP("dve", "tensor_tensor", [("ps", bB), "tab"], [("tB", i)], out=tB[i], in0=bank(bB), in1=sn, op=ALU.mult)
                OP("dve", "tensor_tensor", [("tA", i), ("tB", i)], [dkey_fn(hf)], out=dst_fn(hf), in0=tA[i], in1=tB[i],
                   op=ALU.add)

        def rk_task(p, pc):
            def fn(wb, wk):
                for hh in range(4):
                    h = pc * 4 + hh
                    rot_proj(wb, wk, hh, xT,
                             lambda hf: rKT[:, h, p * 1024 + hf * 512: p * 1024 + (hf + 1) * 512],
                             lambda hf: ("rKT", h, p * 2 + hf))
            return (p, w_ext[:, C_RKX + pc * 512: C_RKX + (pc + 1) * 512], fn)

        def rv_task(p, pc):
            def fn(wb, wk):
                for tb in range(8):
                    blk = p * 8 + tb
                    proj_token_major(wb, wk, xT, tb,
                                     lambda b, blk=blk: evac_copy(rV[:, blk, pc * 512:(pc + 1) * 512], bank(b),
                                                                  [("ps", b)], [("rV", blk, pc)]))
            return (p, w_ext[:, C_RV + pc * 512: C_RV + (pc + 1) * 512], fn)

        def rq_task(pc):
            def fn(wb, wk):
                for hh in range(4):
                    h = pc * 4 + hh
                    rot_proj(wb, wk, hh, xT,
                             lambda hf: rQT[:, h, hf * 512:(hf + 1) * 512],
                             lambda hf: ("rQT", h, hf))
            return (2, w_ext[:, C_RQX + pc * 512: C_RQX + (pc + 1) * 512], fn)

        def rg_task(pc):
            def fn(wb, wk):
                for tb in range(8):
                    def cons(b, tb=tb):
                        OP("act", "activation", [("ps", b)], [("G", tb, pc)],
                           out=G[:, tb, pc * 512:(pc + 1) * 512], in_=bank(b), func=AF.Silu)
                    proj_token_major(wb, wk, xT, tb, cons)
            return (2, w_ext[:, C_RG + pc * 512: C_RG + (pc + 1) * 512], fn)

        tasks = []
        for p in range(2):
            tasks += [rk_task(p, 0), rk_task(p, 1), rv_task(p, 0), rv_task(p, 1)]
        tasks += [rq_task(0), rq_task(1), rg_task(0), rg_task(1)]
        run_proj(tasks, xT, wbufs, "wr",
                 on_pass=lambda p: S.dma("sp", "tab", [], ["tab"], out=tab, in_=rot[p].rearrange("c d t -> d c t")))

        S.barrier()
        A.release(m_retattn)
        rm = A.f32(3, NH, 128)
        S.dma("sp", "rm", [], ["rm"], out=rm, in_=rmask.rearrange("p (a h c) -> p a h c", a=3, h=NH))
        gg = A.f32(1024)
        S.dma("sp", "gg", [], ["gg"], out=gg, in_=vecs[4, 0:1024].partition_broadcast(128))
        sc_buf = [A.bf16(512) for _ in range(2)]
        st6 = A.f32(4, 6)
        mv = A.f32(4, 2)
        rstd = A.f32(4)
        nmr = A.f32(4)
        nrm = [A.f32(128) for _ in range(2)]
        nrm2 = [A.f32(128) for _ in range(2)]
        rout = [A.bf16(128) for _ in range(2)]
        gam = [1.0 - 2.0 ** (-5.0 - h) for h in range(NH)]
        psT = ps[:, 4 * 512: 5 * 512].bitcast(BF16)

        def ret_s1(n):
            u, h, q, j, jmax = iters[n]
            r0, ms = sb_info(q, j)
            lo = r0 * 128
            sb_ = n % 2
            sT = bank(sb_)
            mm(sT[:, lo:512], rKT[:, h, j * 128:(j + 1) * 128], rQT[:, h, q * 512 + lo:(q + 1) * 512], True, True,
               [("rKT", h, j // 4), ("rQT", h, q)], [("ps", sb_)])
            for r in range(r0, 4):
                i_slot = 4 * q + r
                dstc = sc_buf[sb_][:, r * 128:(r + 1) * 128]
                src = sT[:, r * 128:(r + 1) * 128]
                if ms is not None and r == ms:
                    mk = rm[:, 1 if j % 2 == 0 else 2, h, :]
                    OP("dve", "tensor_tensor", [("ps", sb_), "rm"], [("sc", sb_)], out=dstc, in0=src, in1=mk, op=ALU.mult)
                else:
                    fac = gam[h] ** (128.0 * (2 * i_slot - j))
                    mk = rm[:, 0, h, :]
                    OP("dve", "scalar_tensor_tensor", [("ps", sb_), "rm"], [("sc", sb_)],
                       out=dstc, in0=src, scalar=fac, in1=mk, op0=ALU.mult, op1=ALU.mult)

        def ret_s2(n):
            u, h, q, j, jmax = iters[n]
            r0, ms = sb_info(q, j)
            sb_ = n % 2
            Ob = 2 + u % 2
            for r in range(r0, 4):
                first = (j == jmax)
                mm(bank(Ob)[:, r * 128:(r + 1) * 128], sc_buf[sb_][:, r * 128:(r + 1) * 128],
                   rV[:, j, h * 128:(h + 1) * 128], first, j == 0, [("sc", sb_), ("rV", j, h // 4)], [("ps", Ob)])
            if j == 0:
                for r in range(4):
                    o = bank(Ob)[:, r * 128:(r + 1) * 128]
                    OP("dve", "bn_stats", [("ps", Ob)], [("st6", r)], out=st6[:, r, :], in_=o)
                    OP("dve", "bn_aggr", [("st6", r)], [("mv",)], out=mv[:, r, :], in_=st6[:, r, :])
                OP("act", "activation", [("mv",)], [("rstd",)], out=rstd, in_=mv[:, :, 1], func=AF.Ln, bias=GN_EPS,
                   scale=1.0)
                OP("act", "activation", [("rstd",)], [("rstd",)], out=rstd, in_=rstd, func=AF.Exp, scale=-0.5)
                OP("dve", "scalar_tensor_tensor", [("mv",), ("rstd",)], [("nmr",)], out=nmr, in0=mv[:, :, 0], scalar=-1.0,
                   in1=rstd, op0=ALU.mult, op1=ALU.mult)
                for r in range(4):
                    i_slot = 4 * q + r
                    o = bank(Ob)[:, r * 128:(r + 1) * 128]
                    k2 = (u * 4 + r) % 2
                    OP("act", "activation", [("ps", Ob), ("nmr",), ("rstd",)], [("nrm", k2)], out=nrm[k2], in_=o,
                       func=AF.Identity, bias=nmr[:, r:r + 1], scale=rstd[:, r:r + 1])
                    OP("pool", "tensor_tensor", [("nrm", k2), "gg"], [("nrm2", k2)], out=nrm2[k2], in0=nrm[k2],
                       in1=gg[:, h * 128:(h + 1) * 128], op=ALU.mult)
                    OP("pool", "tensor_tensor", [("nrm2", k2), ("G", i_slot, h // 4)], [("rout", k2)], out=rout[k2],
                       in0=nrm2[k2], in1=G[:, i_slot, h * 128:(h + 1) * 128], op=ALU.mult)
                    OP("pe", "transpose", [("rout", k2), "cstb"], [("psT",)], out=psT[:, r * 128:(r + 1) * 128],
                       in_=rout[k2], identity=ident_b)
                evac_copy(mixT[:, 8 + h, q * 512:(q + 1) * 512], psT[:, 0:512], [("psT",)], [("mixT", 8 + h, q)])

        for step in range(NIT + 1):
            if step < NIT:
                ret_s1(step)
            if 0 <= step - 1 < NIT:
                ret_s2(step - 1)

        if stage == "attn":
            return _finish_debug(nc, S, A, y, mixT, 16)

        S.barrier()
        A.release(m_phaseA)
        yacc = A.f32(8, D)
        x1b = A.bf16(8, D)
        gates = A.f32(8, NE)
        posm = A.f32(8, NE)
        m_moe2 = A.mark()
        wob = [A.bf16(16, 512) for _ in range(2)]
        lng = A.f32(2, D)
        S.dma("sp", "lng", [], ["lng"], out=lng[:, 0, :], in_=vecs[0, :].partition_broadcast(128))
        S.dma("sp", "lng", [], ["lng"], out=lng[:, 1, :], in_=vecs[1, :].partition_broadcast(128))
        S.dma("sp", "xown", [], [("h1", s_) for s_ in range(8)], out=yacc, in_=x_own.rearrange("(s p) f -> p s f", p=128))
        for cp in range(4):
            wb, wk = load_piece(wob, w_out[:, cp * 512:(cp + 1) * 512], "wo")
            for s_ in range(8):
                (b,) = next_banks(1)
                for k in range(16):
                    mm(bank(b), mixT[:, k, s_ * 128:(s_ + 1) * 128], wb[:, k, :], k == 0, k == 15,
                       [wk, ("mixT", k, s_ // 4)], [("ps", b)])
                dst = yacc[:, s_, cp * 512:(cp + 1) * 512]
                OP("dve", "scalar_tensor_tensor", [("ps", b), ("h1", s_)], [("h1", s_)],
                   out=dst, in0=dst, scalar=ALPHA, in1=bank(b), op0=ALU.mult, op1=ALU.add)

        lst = A.f32(8, 24)
        lmv = A.f32(8, 2)
        lrs = A.f32(8)
        lnm = A.f32(8)

        def layer_norm(s_, lng, lst, lmv, lrs, lnm):
            xs = yacc[:, s_, :]
            for c in range(4):
                OP("dve", "bn_stats", [("h1", s_)], [("lst", s_)], out=lst[:, s_, c * 6:(c + 1) * 6],
                   in_=xs[:, c * 512:(c + 1) * 512])
            OP("dve", "bn_aggr", [("lst", s_)], [("lmv", s_)], out=lmv[:, s_, :], in_=lst[:, s_, :])
            OP("act", "activation", [("lmv", s_)], [("lrs", s_)], out=lrs[:, s_:s_ + 1], in_=lmv[:, s_, 1:2],
               func=AF.Ln, bias=LN_EPS, scale=1.0)
            OP("act", "activation", [("lrs", s_)], [("lrs", s_)], out=lrs[:, s_:s_ + 1], in_=lrs[:, s_:s_ + 1],
               func=AF.Exp, scale=-0.5)
            OP("dve", "scalar_tensor_tensor", [("lmv", s_), ("lrs", s_)], [("lnm", s_)], out=lnm[:, s_:s_ + 1],
               in0=lmv[:, s_, 0:1], scalar=-1.0, in1=lrs[:, s_:s_ + 1], op0=ALU.mult, op1=ALU.mult)
            OP("act", "activation", [("h1", s_), ("lnm", s_), ("lrs", s_)], [("h1", s_)], out=xs, in_=xs,
               func=AF.Identity, bias=lnm[:, s_:s_ + 1], scale=lrs[:, s_:s_ + 1])
            for hf_ in range(2):
                c0 = hf_ * 1024
                OP("dve", "tensor_tensor", [("h1", s_), "lng"], [("h1", s_, "n", hf_)], out=xs[:, c0:c0 + 1024],
                   in0=xs[:, c0:c0 + 1024], in1=lng[:, 0, c0:c0 + 1024], op=ALU.mult)
            for hf_ in range(2):
                c0 = hf_ * 1024
                OP("pool", "tensor_tensor", [("h1", s_, "n", hf_), "lng"], [("h1", s_)], out=xs[:, c0:c0 + 1024],
                   in0=xs[:, c0:c0 + 1024], in1=lng[:, 1, c0:c0 + 1024], op=ALU.add)

        def ln1_slot(s_):
            layer_norm(s_, lng, lst, lmv, lrs, lnm)
            OP("act", "activation", [("h1", s_)], [("x1b", s_)], out=x1b[:, s_, :], in_=yacc[:, s_, :], func=AF.Copy)

        if stage == "x1":
            for s_ in range(8):
                ln1_slot(s_)
            S.dma("sp", "yout", [("h1", s_) for s_ in range(8)], ["y"], out=y.rearrange("(s p) f -> p s f", p=128),
                  in_=yacc)
            S.wait_all("sp", ["y"])
            S.emit()
            return nc

        wr = A.f32(16, NE)
        br = A.f32(NE)
        onesrow = A.f32(128)
        bdn = A.f32(D)
        x1T = A.f32(16, 128)
        lg = A.f32(NE)
        mx8 = A.f32(8)
        msk = A.f32(8, NE)
        mskb = A.bf16(8, NE)
        msum = A.f32(NE)
        msumb = A.bf16(NE)
        ex = A.f32(NE)
        ssum = A.f32(1)
        nmx = A.f32(1)
        gT = A.f32(128)
        S.dma("sp", "wr", [], ["wr"], out=wr, in_=w_router.rearrange("(k q) n -> q k n", q=128))
        S.dma("sp", "br", [], ["br"], out=br[0:1, :], in_=b_router)
        S.dma("sp", "bdn", [], ["bdn"], out=bdn[0:NE, :], in_=b_dn)
        OP("dve", "memset", [], ["onesrow"], ap=onesrow[0:1, :], constant=1.0)
        OP("dve", "memset", [], ["msum"], ap=msum, constant=0.0)
        def router_slot(s_):
            for g4 in range(4):
                for c in range(4):
                    k = g4 * 4 + c
                    OP("pe", "transpose", [("h1", s_), "cst"], [("ps", g4)], out=bank(g4)[:, c * 128:(c + 1) * 128],
                       in_=yacc[:, s_, k * 128:(k + 1) * 128], identity=ident_f)
                evac_copy(x1T[:, g4 * 4:(g4 + 1) * 4, :], bank(g4).rearrange("p (a b) -> p a b", a=4),
                          [("ps", g4)], [("x1T", g4)])
            lgp = bank(4)[:, 0:NE]
            for k in range(16):
                mm(lgp, x1T[:, k, :], wr[:, k, :], k == 0, False, [("x1T", k // 4), "wr"], [("ps", 4)])
            mm(lgp, onesrow[0:1, :], br[0:1, :], False, True, ["onesrow", "br"], [("ps", 4)])
            OP("dve", "tensor_copy", [("ps", 4)], ["lg"], out=lg, in_=lgp)
            OP("dve", "max", ["lg"], ["mx8"], out=mx8, in_=lg)
            OP("dve", "tensor_scalar", ["lg", "mx8"], [("msk", s_)], out=msk[:, s_, :], in0=lg, scalar1=mx8[:, 3:4],
               scalar2=None, op0=ALU.is_ge)
            OP("dve", "tensor_scalar", ["mx8"], ["nmx"], out=nmx, in0=mx8[:, 0:1], scalar1=-1.0, scalar2=None, op0=ALU.mult)
            OP("act", "activation", ["lg", "nmx"], ["ex"], out=ex, in_=lg, func=AF.Exp, bias=nmx, scale=1.0)
            OP("dve", "tensor_tensor", ["ex", ("msk", s_)], ["ex"], out=ex, in0=ex, in1=msk[:, s_, :], op=ALU.mult)
            OP("dve", "reduce_sum", ["ex"], ["ssum"], out=ssum, in_=ex, axis=mybir.AxisListType.X)
            OP("dve", "reciprocal", ["ssum"], ["ssum"], out=ssum, in_=ssum)
            OP("dve", "tensor_scalar", ["ex", "ssum"], [("gates", s_)], out=gates[:, s_, :], in0=ex, scalar1=ssum[:, 0:1],
               scalar2=None, op0=ALU.mult)
            OP("dve", "tensor_copy", [("msk", s_)], [("mskb", s_)], out=mskb[:, s_, :], in_=msk[:, s_, :])
            OP("dve", "tensor_copy", ["msum"], ["msumb"], out=msumb, in_=msum)
            pp = bank(5)[:, 0:NE]
            mm(pp, triL_b, mskb[:, s_, :], True, False, [("mskb", s_), "cstb"], [("ps", 5)])
            mm(pp, ones_b, msumb, False, True, ["msumb", "cstb"], [("ps", 5)])
            OP("dve", "scalar_tensor_tensor", [("ps", 5), ("msk", s_)], [("posm", s_)], out=posm[:, s_, :], in0=pp,
               scalar=1.0, in1=msk[:, s_, :], op0=ALU.add, op1=ALU.mult)
            OP("dve", "tensor_scalar", [("posm", s_)], [("posm", s_)], out=posm[:, s_, :], in0=posm[:, s_, :],
               scalar1=-1.0, scalar2=None, op0=ALU.add)
            OP("dve", "tensor_tensor", ["msum", ("msk", s_)], ["msum"], out=msum, in0=msum, in1=msk[:, s_, :], op=ALU.add)
            OP("pe", "transpose", [("gates", s_), "cst"], [("ps", 6)], out=bank(6)[0:NE, 0:128], in_=gates[:, s_, :],
               identity=ident_f)
            OP("act", "activation", [("ps", 6)], ["gT"], out=gT[0:NE, :], in_=bank(6)[0:NE, 0:128], func=AF.Copy)
            for cp in range(4):
                bb = bank(7)
                mm(bb, gT[0:NE, :], bdn[0:NE, cp * 512:(cp + 1) * 512], True, True, ["gT", "bdn"], [("ps", 7)])
                dst = yacc[:, s_, cp * 512:(cp + 1) * 512]
                OP("dve", "scalar_tensor_tensor", [("ps", 7), ("h1", s_)], [("h1", s_)], out=dst, in0=dst, scalar=ALPHA,
                   in1=bb, op0=ALU.mult, op1=ALU.add)

        ln1_slot(0)
        for s_ in range(8):
            if s_ + 1 < 8:
                ln1_slot(s_ + 1)
            router_slot(s_)

        if stage == "router":
            S.barrier()
            yv = y.rearrange("(s p) f -> p s f", p=128)
            S.dma("sp", "yout", [], ["y"], out=yv[:, :, 0:NE], in_=gates)
            S.dma("sp", "yout", [], ["y"], out=yv[:, :, NE:2 * NE], in_=posm)
            S.dma("sp", "yout", [], ["y"], out=yv[:, :, 64:64 + 1024], in_=yacc[:, :, 0:1024])
            S.wait_all("sp", ["y"])
            S.emit()
            return nc

        S.barrier()
        A.release(m_moe2)
        wmb = [mix_raw[:, i * 4096:(i + 1) * 4096].bitcast(BF16).rearrange("p (a b) -> p a b", a=16) for i in range(2)]
        wmb.append(A.bf16(16, 512))
        sel = [A.bf16(8, CAP) for _ in range(2)]
        selT = [A.bf16(2, 1024) for _ in range(2)]
        XeT = [A.bf16(16, CAP) for _ in range(2)]
        actT = A.bf16(16, CAP)
        gsb = A.f32(4, CAP)
        utmp = [A.f32(CAP) for _ in range(2)]
        gtmp = utmp
        yrows = A.bf16(2, D)
        bgu = [A.f32(32) for _ in range(2)]
        psT7 = ps[:, 7 * 512: 8 * 512].bitcast(BF16)

        hb_n = [0]
        sc_n = [0]
        bgu1 = [A.f32(16) for _ in range(2)]

        def moe_front(ex_):
            eb = ex_ % 2
            S.dma("sp", f"bgu{eb}", [], [("bgu", eb)], out=bgu[eb], in_=b_gu[ex_])
            OP("dve", "tensor_scalar", [("bgu", eb)], [("bgu1", eb)], out=bgu1[eb], in0=bgu[eb][:, 16:32], scalar1=1.0,
               scalar2=None, op0=ALU.add)
            for s_ in range(8):
                OP("dve", "tensor_scalar", [("posm", s_), "cst"], [("sel", eb, s_)], out=sel[eb][:, s_, :], in0=iota_f[:, 0:CAP],
                   scalar1=posm[:, s_, ex_:ex_ + 1], scalar2=None, op0=ALU.is_equal)
            for rc, (r0_, rn) in enumerate(RCH):
                for s_ in range(8):
                    OP("pe", "transpose", [("sel", eb, s_), "cstb"], [("ps", 7)], out=psT7[0:rn, s_ * 128:(s_ + 1) * 128],
                       in_=sel[eb][:, s_, r0_:r0_ + rn], identity=ident_b)
                evac_copy(selT[eb][0:rn, rc, :], psT7[0:rn, :], [("ps", 7)], [("selT", eb, rc)], eng="act")
            for c in range(16):
                gb = (0, 7)[c % 2]
                for s_ in range(8):
                    mm(bank(gb, CAP), x1b[:, s_, c * 128:(c + 1) * 128], sel[eb][:, s_, :], s_ == 0, s_ == 7,
                       [("x1b", s_), ("sel", eb, s_)], [("ps", gb)])
                evac_copy(XeT[eb][:, c, :], bank(gb, CAP), [("ps", gb)], [("XeT", eb, c)], eng="act")

        def moe_hidden(ex_):
            eb = ex_ % 2
            for p4 in range(4):
                for gu in range(2):
                    wb, wk = load_piece(wmb, w_gu[ex_, p4 * 2 + gu], "wm", tiled=True)
                    for cc in range(4):
                        hbi = hb_n[0] % 2
                        hb_n[0] += 1
                        hbk = ("ps", 1 + hbi)
                        hp_ = bank(1 + hbi, CAP)
                        for k in range(16):
                            mm(hp_, wb[:, k, cc * 128:(cc + 1) * 128], XeT[eb][:, k, :], k == 0, k == 15,
                               [wk, ("XeT", eb, k)], [hbk])
                        fch = p4 * 4 + cc
                        if gu == 0:
                            OP("dve", "tensor_scalar", [hbk, ("bgu", eb)], [("gtmp", hbi)], out=gtmp[hbi], in0=hp_,
                               scalar1=bgu[eb][:, fch:fch + 1], scalar2=7.0, op0=ALU.add, op1=ALU.min)
                            OP("act", "activation", [("gtmp", hbi)], [("gsb", cc)], out=gsb[:, cc, :], in_=gtmp[hbi],
                               func=AF.Gelu_apprx_sigmoid)
                        else:
                            OP("dve", "tensor_scalar", [hbk, ("bgu1", eb)], [("utmp", hbi)], out=utmp[hbi], in0=hp_,
                               scalar1=bgu1[eb][:, fch:fch + 1], scalar2=8.0, op0=ALU.add, op1=ALU.min)
                            OP("dve", "scalar_tensor_tensor", [("utmp", hbi), ("gsb", cc)], [("actT", fch)],
                               out=actT[:, fch, :], in0=utmp[hbi], scalar=-6.0, in1=gsb[:, cc, :], op0=ALU.max, op1=ALU.mult)

        def moe_down(ex_):
            eb = ex_ % 2
            for cp in range(4):
                wb, wk = load_piece(wmb, w_dn[ex_, cp], "wm", tiled=True)
                for rc, (r0_, rn) in enumerate(RCH):
                    yb = 3 + rc
                    for k in range(16):
                        mm(bank(yb)[0:rn, :], actT[:, k, r0_:r0_ + rn], wb[:, k, :], k == 0, k == 15,
                           [wk, ("actT", k)], [("ps", yb)])
                    evac_copy(yrows[0:rn, rc, cp * 512:(cp + 1) * 512], bank(yb)[0:rn, :], [("ps", yb)],
                              [("yrows", rc, cp)], eng="act")
                for s_ in range(8):
                    sb_ = 5 + sc_n[0] % 2
                    sc_n[0] += 1
                    for rc, (r0_, rn) in enumerate(RCH):
                        mm(bank(sb_), selT[eb][0:rn, rc, s_ * 128:(s_ + 1) * 128], yrows[0:rn, rc, cp * 512:(cp + 1) * 512],
                           rc == 0, rc == 1, [("selT", eb, rc), ("yrows", rc, cp)], [("ps", sb_)])
                    dst = yacc[:, s_, cp * 512:(cp + 1) * 512]
                    OP("dve", "scalar_tensor_tensor", [("ps", sb_), ("gates", s_), ("h1", s_, cp)], [("h1", s_, cp)],
                       out=dst, in0=bank(sb_), scalar=gates[:, s_, ex_:ex_ + 1], in1=dst, op0=ALU.mult, op1=ALU.add)

        moe_front(0)
        for ex_ in range(ne_run):
            moe_hidden(ex_)
            if ex_ + 1 < ne_run:
                moe_front(ex_ + 1)
            moe_down(ex_)

        S.barrier()
        A.release(m_moe2)
        lng2 = A.f32(2, D)
        lst2 = A.f32(8, 24)
        lmv2 = A.f32(8, 2)
        lrs2 = A.f32(8)
        lnm2 = A.f32(8)
        S.dma("sp", "lng2", [], ["lng"], out=lng2[:, 0, :], in_=vecs[2, :].partition_broadcast(128))
        S.dma("sp", "lng2", [], ["lng"], out=lng2[:, 1, :], in_=vecs[3, :].partition_broadcast(128))
        yv = y.rearrange("(s p) f -> p s f", p=128)
        for s_ in range(8):
            layer_norm(s_, lng2, lst2, lmv2, lrs2, lnm2)
            S.dma("sp", "yout", [("h1", s_)], [("y", s_)], out=yv[:, s_, :], in_=yacc[:, s_, :])
        S.wait_all("sp", [("y", s_) for s_ in range(8)])
        S.emit()
    return nc


def _finish_debug(nc, S, A, y, mixT, nchunks):
    S.barrier()
    tmp = [A.f32(1024) for _ in range(2)]
    yv = y.rearrange("r (two t) -> (r two) t", two=2).rearrange("(c p) t -> p c t", p=128)
    for c in range(nchunks):
        S.op("dve", "tensor_copy", [], [("tmp", c % 2)], out=tmp[c % 2], in_=mixT[:, c, :])
        S.dma("sp", "dbg", [("tmp", c % 2)], ["ydbg"], out=yv[:, c, :], in_=tmp[c % 2])
    S.wait_all("sp", ["ydbg"])
    S.emit()
    return nc


def _constants(hp):
    p = np.arange(128)
    ident = np.eye(128, dtype=np.float32)
    triU = (p[:, None] > p[None, :]).astype(np.float32)
    triL = (p[:, None] < p[None, :]).astype(np.float32)
    ones = np.ones((128, 128), np.float32)
    strict = (p[:, None] < p[None, :]).astype(np.float32)
    if hp == 0:
        mA, mB = strict, np.zeros((128, 128), np.float32)
    else:
        mA, mB = ones, strict
    iota = np.broadcast_to(np.arange(256, dtype=np.float32)[None, :], (128, 256))
    cst = np.concatenate([ident, triU, triL, ones, mA, mB, iota], axis=1).astype(np.float32)
    sl = p[:, None].astype(np.float64)
    tl = p[None, :].astype(np.float64)
    rm = np.zeros((128, 3, NH, 128), np.float64)
    for h in range(NH):
        lg = math.log1p(-2.0 ** (-5.0 - h))
        gen = np.exp(lg * (128.0 * hp + tl - sl))
        diag = np.where((p[:, None] // 64) <= (p[None, :] // 64), np.exp(lg * np.abs(tl - sl)), 0.0)
        rm[:, 0, h, :] = gen
        if hp == 0:
            rm[:, 1, h, :] = diag
            rm[:, 2, h, :] = 0.0
        else:
            rm[:, 1, h, :] = np.exp(lg * (128.0 + tl - sl))
            rm[:, 2, h, :] = diag
    rm *= HD ** -0.5
    return cst, rm.reshape(128, 3 * NH * 128).astype(np.float32)


def _rot_tables(hp):
    inv = 10000.0 ** (-np.arange(0, HD, 2, dtype=np.float32) / HD)
    own = np.concatenate([np.arange(128) + (2 * i + hp) * 128 for i in range(8)])
    out = np.zeros((3, 2, 128, 1024), np.float32)
    for pi, pos in enumerate([np.arange(0, 1024), np.arange(1024, 2048), own]):
        ang = pos.astype(np.float32)[None, :] * inv[:, None]
        c, s = np.cos(ang), np.sin(ang)
        out[pi, 0] = np.concatenate([c, c], axis=0)
        out[pi, 1] = np.concatenate([-s, s], axis=0)
    return out


def _w_ext(w_in):
    return np.ascontiguousarray(w_in)


_PROG = {}


def _get_prog(stage="full", ne_run=NE):
    if (stage, ne_run) not in _PROG:
        _PROG[(stage, ne_run)] = build_program(stage, ne_run)
    return _PROG[(stage, ne_run)]


def make_in_maps(x, w_in, ret_gn_gain, w_out, ln1_gain, ln1_bias, w_router, b_router,
                 w_gate_up, b_gate_up, w_down, b_down, ln2_gain, ln2_bias, stage="full", ne_run=NE):
    f = lambda a: np.ascontiguousarray(np.asarray(a, dtype=np.float32))
    x = f(x)
    wext = _w_ext(f(w_in)[0])
    vecs = np.zeros((5, D), np.float32)
    vecs[0], vecs[1], vecs[2], vecs[3] = f(ln1_gain)[0], f(ln1_bias)[0], f(ln2_gain)[0], f(ln2_bias)[0]
    vecs[4, :1024] = f(ret_gn_gain)[0]
    shared = {"w_ext": wext, "w_out": f(w_out)[0], "vecs": vecs}
    if stage in ("full", "router"):
        shared.update({
            "w_router": f(w_router)[0], "b_router": f(b_router)[0].reshape(1, NE), "b_dn": f(b_down)[0],
            "b_gu": np.ascontiguousarray(f(b_gate_up)[0].reshape(NE, 32, 128).transpose(0, 2, 1)),
        })
    if stage == "full":
        wg = f(w_gate_up)[0][:ne_run].reshape(ne_run, 16, 128, 2, 4, 512)
        wd = f(w_down)[0][:ne_run].reshape(ne_run, 16, 128, 4, 512)
        shared.update({
            "w_gu": np.ascontiguousarray(wg.transpose(0, 4, 3, 2, 1, 5)).reshape(ne_run, 8, 128, 16, 512),
            "w_dn": np.ascontiguousarray(wd.transpose(0, 3, 2, 1, 4)),
        })
    consts = [(_constants(hp), _rot_tables(hp)) for hp in range(2)]
    in_maps = []
    for c in range(8):
        b, hp = c // 2, c % 2
        xb = x[b]
        own = np.concatenate([np.arange(128) + (2 * i + hp) * 128 for i in range(8)])
        xo = np.ascontiguousarray(xb[own])
        xT3 = np.stack([np.ascontiguousarray(xb[0:1024].T), np.ascontiguousarray(xb[1024:2048].T),
                        np.ascontiguousarray(xo.T)])
        (cst, rm), rot = consts[hp]
        m = dict(shared)
        m.update({"xT3": xT3, "x_own": xo, "cst": cst, "rmask": rm, "rot": rot})
        in_maps.append(m)
    return in_maps


def kernel(x, w_in, ret_gn_gain, w_out, ln1_gain, ln1_bias, w_router, b_router,
           w_gate_up, b_gate_up, w_down, b_down, ln2_gain, ln2_bias):
    in_maps = make_in_maps(x, w_in, ret_gn_gain, w_out, ln1_gain, ln1_bias, w_router, b_router,
                           w_gate_up, b_gate_up, w_down, b_down, ln2_gain, ln2_bias)
    nc = _get_prog("full")
    res = run_bass_kernel_spmd(nc, in_maps, core_ids=list(range(8)))
    out = np.zeros((NB, SEQ, D), np.float32)
    for c in range(8):
        b, hp = c // 2, c % 2
        yc = res.results[c]["y"]
        for i in range(8):
            blk = 2 * i + hp
            out[b, blk * 128:(blk + 1) * 128] = yc[i * 128:(i + 1) * 128]
    return out
```

```python
import math
from contextlib import ExitStack

import numpy as np
import concourse.bass as bass
import concourse.mybir as mybir
from concourse.bass_utils import run_bass_kernel_spmd

F32 = mybir.dt.float32
BF16 = mybir.dt.bfloat16
AF = mybir.ActivationFunctionType
ALU = mybir.AluOpType

D = 2048
SEQ = 2048
NB = 4
HD = 128
NH = 8
NE = 32
CAP = 192
RCH = ((0, 128), (128, 64))
ALPHA = 2.0 ** 0.25
LN_EPS = 1e-5
GN_EPS = 1e-5
QSCALE = 1.0 / math.sqrt(HD)
WEXT = 7168
EPOCH = 2000

C_SBQ, C_SBK, C_SBV, C_RQX, C_RKX, C_RV, C_RG = 0, 1024, 2048, 3072, 4096, 5120, 6144


class Sched:
    ENGS = ("pe", "act", "dve", "pool", "sp")

    def __init__(self, nc):
        self.nc = nc
        self.ops = {e: [] for e in self.ENGS}
        self.count = {e: 0 for e in self.ENGS}
        self.known = {e: {} for e in self.ENGS}
        self.last_w = {}
        self.readers = {}
        self.dma_slots = {}
        self.n_dma_sems = 0

    def _need(self, eng, key, val, waits):
        if key == ("eng", "pe") and eng == "pe":
            return
        if self.known[eng].get(key, 0) >= val:
            return
        self.known[eng][key] = val
        waits[key] = max(waits.get(key, 0), val)

    def _deps(self, eng, reads, writes):
        waits = {}
        for r in reads:
            t = self.last_w.get(r)
            if t is not None:
                self._need(eng, t[0], t[1], waits)
        for w in writes:
            t = self.last_w.get(w)
            if t is not None:
                self._need(eng, t[0], t[1], waits)
            for k, v in self.readers.get(w, {}).items():
                self._need(eng, k, v, waits)
        return list(waits.items())

    def _commit(self, tok, reads, writes):
        for r in reads:
            d = self.readers.setdefault(r, {})
            d[tok[0]] = max(d.get(tok[0], 0), tok[1])
        for w in writes:
            self.last_w[w] = tok
            self.readers[w] = {}

    def op(self, eng, method, reads=(), writes=(), **kw):
        fn = (method, kw)
        waits = self._deps(eng, reads, writes)
        idx = self.count[eng]
        self.count[eng] += 1
        tok = (("eng", eng), idx + 1)
        self.ops[eng].append((waits, fn, ("eng", idx)))
        self._commit(tok, reads, writes)

    def dma(self, eng, slot, reads=(), writes=(), **kw):
        fn = ("dma_start", kw)
        waits = self._deps(eng, reads, writes)
        if slot not in self.dma_slots:
            self.dma_slots[slot] = [self.n_dma_sems, 0]
            self.n_dma_sems += 1
        self.dma_slots[slot][1] += 16
        tok = (("dma", slot), self.dma_slots[slot][1])
        self.ops[eng].append((waits, fn, ("dma", slot)))
        self._commit(tok, reads, writes)

    def wait_all(self, eng, regions):
        waits = self._deps(eng, regions, ())
        self.ops[eng].append((waits, None, None))

    def barrier(self):
        toks = [(("eng", e), self.count[e]) for e in self.ENGS if self.count[e] > 0]
        toks += [(("dma", s), v[1]) for s, v in self.dma_slots.items()]
        for e in self.ENGS:
            waits = {}
            for k, v in toks:
                self._need(e, k, v, waits)
            self.ops[e].append((list(waits.items()), None, None))
        self.last_w.clear()
        self.readers.clear()

    def emit(self):
        nc = self.nc
        with ExitStack() as st:
            eng_sems = {}
            for e in self.ENGS:
                n_ep = self.count[e] // EPOCH + 1
                eng_sems[e] = [st.enter_context(nc.semaphore(f"s_{e}_{i}")) for i in range(n_ep)]
            dma_sems = [st.enter_context(nc.semaphore(f"s_dma_{i}")) for i in range(self.n_dma_sems)]
            block = st.enter_context(nc.Block())

            def run(ename):
                def body(eng):
                    for waits, fn, inc in self.ops[ename]:
                        for key, val in waits:
                            if key[0] == "eng":
                                idx = val - 1
                                eng.wait_ge(eng_sems[key[1]][idx // EPOCH], idx % EPOCH + 1)
                            else:
                                eng.wait_ge(dma_sems[self.dma_slots[key[1]][0]], val)
                        if fn is None:
                            continue
                        ins = getattr(eng, fn[0])(**fn[1])
                        if inc[0] == "eng":
                            idx = inc[1]
                            ins.then_inc(eng_sems[ename][idx // EPOCH], 1)
                        else:
                            ins.then_inc(dma_sems[self.dma_slots[inc[1]][0]], 16)
                return body

            block.tensor(run("pe"))
            block.scalar(run("act"))
            block.vector(run("dve"))
            block.gpsimd(run("pool"))
            block.sync(run("sp"))


class Arena:
    def __init__(self, t, words):
        self.t = t
        self.words = words
        self.off = 0

    def mark(self):
        return self.off

    def release(self, m):
        self.off = m

    def _take(self, nwords):
        a = self.off
        self.off += nwords
        assert self.off <= self.words, f"SBUF arena overflow {self.off} > {self.words}"
        return self.t[:, a:a + nwords]

    def f32(self, *dims):
        n = int(np.prod(dims))
        v = self._take(n)
        return self._shape(v, dims)

    def bf16(self, *dims):
        n = int(np.prod(dims))
        assert n % 2 == 0
        v = self._take(n // 2).bitcast(BF16)
        return self._shape(v, dims)

    @staticmethod
    def _shape(v, dims):
        if len(dims) == 1:
            return v
        if len(dims) == 2:
            return v.rearrange("p (a b) -> p a b", a=dims[0])
        if len(dims) == 3:
            return v.rearrange("p (a b c) -> p a b c", a=dims[0], b=dims[1])
        raise ValueError(dims)


def build_program(stage="full", ne_run=NE):
    nc = bass.Bass("TRN2", target_bir_lowering=False)
    dt = nc.dram_tensor
    with_moe = stage in ("full", "router")
    xT3 = dt("xT3", [3, D, 1024], F32, kind="ExternalInput").ap()
    x_own = dt("x_own", [1024, D], F32, kind="ExternalInput").ap()
    w_ext = dt("w_ext", [D, WEXT], F32, kind="ExternalInput").ap()
    w_out = dt("w_out", [D, D], F32, kind="ExternalInput").ap()
    vecs = dt("vecs", [5, D], F32, kind="ExternalInput").ap()
    cst = dt("cst", [128, 1024], F32, kind="ExternalInput").ap()
    rmask = dt("rmask", [128, 3 * NH * 128], F32, kind="ExternalInput").ap()
    rot = dt("rot", [3, 2, 128, 1024], F32, kind="ExternalInput").ap()
    if with_moe:
        w_router = dt("w_router", [D, NE], F32, kind="ExternalInput").ap()
        b_router = dt("b_router", [1, NE], F32, kind="ExternalInput").ap()
        b_gu = dt("b_gu", [NE, 128, 32], F32, kind="ExternalInput").ap()
        if stage == "full":
            w_gu = dt("w_gu", [ne_run, 8, 128, 16, 512], F32, kind="ExternalInput").ap()
            w_dn = dt("w_dn", [ne_run, 4, 128, 16, 512], F32, kind="ExternalInput").ap()
        b_dn = dt("b_dn", [NE, D], F32, kind="ExternalInput").ap()
    y = dt("y", [1024, D], F32, kind="ExternalOutput").ap()

    AW = 53000
    with ExitStack() as st:
        arena_t = st.enter_context(nc.sbuf_tensor("arena", [128, AW], F32))
        ps = st.enter_context(nc.psum_tensor("ps", [128, 4096], F32))
        A = Arena(arena_t, AW)
        S = Sched(nc)
        OP = S.op

        def bank(b, n=512, off=0):
            return ps[:, b * 512 + off: b * 512 + off + n]

        def mm(out, lhsT, rhs, start, stop, reads, writes):
            OP("pe", "matmul", reads, writes, out=out, lhsT=lhsT, rhs=rhs, start=start, stop=stop)

        evac_rr = [0]

        def evac_copy(dst, src, reads, writes, eng=None):
            if eng is None:
                eng = ("act", "dve")[evac_rr[0] % 2]
                evac_rr[0] += 1
            if eng == "act":
                OP("act", "activation", reads, writes, out=dst, in_=src, func=AF.Copy)
            else:
                OP("dve", "tensor_copy", reads, writes, out=dst, in_=src)

        cst_f = A.f32(1024)
        ident_f = cst_f[:, 0:128]
        maskA = cst_f[:, 512:640]
        maskB = cst_f[:, 640:768]
        iota_f = cst_f[:, 768:1024]
        cst_b = A.bf16(4, 128)
        ident_b, triU_b, triL_b, ones_b = (cst_b[:, i, :] for i in range(4))
        S.dma("sp", "cst", [], ["cst"], out=cst_f, in_=cst)
        OP("dve", "tensor_copy", ["cst"], ["cstb"], out=cst_b,
           in_=cst_f[:, 0:512].rearrange("p (a b) -> p a b", a=4))
        swp_b = A.bf16(128)
        OP("dve", "tensor_copy", ["cstb"], ["swp"], out=swp_b[:, 0:64], in_=ident_b[:, 64:128])
        OP("dve", "tensor_copy", ["cstb"], ["swp"], out=swp_b[:, 64:128], in_=ident_b[:, 0:64])
        mb_b = A.bf16(2, 128)
        OP("dve", "tensor_scalar", ["cst"], ["mbb"], out=mb_b, in0=cst_f[:, 512:768].rearrange("p (a b) -> p a b", a=2),
           scalar1=-1.0, scalar2=1.0e4, op0=ALU.add, op1=ALU.mult)
        mix_raw = A._take(8192)
        mixT = mix_raw.bitcast(BF16).rearrange("p (a b) -> p a b", a=16)
        mix_hi_f32 = mix_raw[:, 4096:8192]
        m_phaseA = A.mark()

        wp_n = [0]

        def load_piece(wbufs, src_ap, tag, tiled=False):
            i = wp_n[0] % len(wbufs)
            wp_n[0] += 1
            key = (tag, i)
            S.dma("pool", f"{tag}{i}", [], [key], out=wbufs[i],
                  in_=src_ap if tiled else src_ap.rearrange("(k q) c -> q k c", q=128))
            return wbufs[i], key

        def load_xT(xT, p):
            v = xT3[p].rearrange("(k q) t -> q k t", q=128)
            for kq in range(4):
                S.dma("pool", f"xT{kq}", [], [("xT", kq)], out=xT[:, kq * 4:(kq + 1) * 4, :], in_=v[:, kq * 4:(kq + 1) * 4, :])

        def run_proj(tasks, xT, wbufs, tag, on_pass=None):
            loaded = {}
            loaded[0] = load_piece(wbufs, tasks[0][1], tag)
            cur = None
            for i, (p, src_ap, fn) in enumerate(tasks):
                if p != cur:
                    load_xT(xT, p)
                    if on_pass is not None:
                        on_pass(p)
                    cur = p
                if i + 1 < len(tasks):
                    loaded[i + 1] = load_piece(wbufs, tasks[i + 1][1], tag)
                fn(*loaded.pop(i))

        pbank = [0]

        def next_banks(n):
            b = pbank[0]
            pbank[0] = (pbank[0] + n) % 4
            return [b + i for i in range(n)]

        def proj_feature_major(wbuf, wkey, col, xT, dst_fn, dkey_fn):
            bA, bB = next_banks(2)
            for k in range(16):
                for hf, b in ((0, bA), (1, bB)):
                    mm(bank(b), wbuf[:, k, col:col + 128], xT[:, k, hf * 512:(hf + 1) * 512],
                       k == 0, k == 15, [wkey, ("xT", k // 4)], [("ps", b)])
            for hf, b in ((0, bA), (1, bB)):
                evac_copy(dst_fn(hf), bank(b), [("ps", b)], [dkey_fn(hf)])

        def proj_token_major(wbuf, wkey, xT, tb, consume):
            (b,) = next_banks(1)
            for k in range(16):
                mm(bank(b), xT[:, k, tb * 128:(tb + 1) * 128], wbuf[:, k, :], k == 0, k == 15,
                   [wkey, ("xT", k // 4)], [("ps", b)])
            consume(b)

        KT = A.bf16(NH, SEQ)
        V = A.bf16(16, 1024)
        QT = A.bf16(NH, 1024)
        m_sbattn = A.mark()
        xT = A.bf16(16, 1024)
        wbufs = [A.bf16(16, 512) for _ in range(3)]

        def k_task(p, pc):
            def fn(wb, wk):
                for hh in range(4):
                    h = pc * 4 + hh
                    proj_feature_major(wb, wk, hh * 128, xT,
                                       lambda hf: KT[:, h, p * 1024 + hf * 512: p * 1024 + (hf + 1) * 512],
                                       lambda hf: ("KT", h, p * 2 + hf))
            return (p, w_ext[:, C_SBK + pc * 512: C_SBK + (pc + 1) * 512], fn)

        def v_task(p, pc):
            def fn(wb, wk):
                for tb in range(8):
                    blk = p * 8 + tb
                    proj_token_major(wb, wk, xT, tb,
                                     lambda b, blk=blk: evac_copy(V[:, blk, pc * 512:(pc + 1) * 512], bank(b),
                                                                  [("ps", b)], [("V", blk, pc)]))
            return (p, w_ext[:, C_SBV + pc * 512: C_SBV + (pc + 1) * 512], fn)

        def q_task(pc):
            def fn(wb, wk):
                for hh in range(4):
                    h = pc * 4 + hh
                    proj_feature_major(wb, wk, hh * 128, xT,
                                       lambda hf: QT[:, h, hf * 512:(hf + 1) * 512],
                                       lambda hf: ("QT", h, hf))
            return (2, w_ext[:, C_SBQ + pc * 512: C_SBQ + (pc + 1) * 512], fn)

        tasks = []
        for p in range(2):
            tasks += [k_task(p, 0), k_task(p, 1), v_task(p, 0), v_task(p, 1)]
        tasks += [q_task(0), q_task(1)]
        run_proj(tasks, xT, wbufs, "wp")

        S.barrier()
        A.release(m_sbattn)
        NS = 4
        e_buf = [A.f32(512) for _ in range(NS)]
        sp_buf = [A.bf16(512) for _ in range(NS)]
        t1_buf = [A.f32(512) for _ in range(NS)]
        w_buf = [A.bf16(512) for _ in range(NS)]
        acc_buf = [[A.bf16(512) for _ in range(2)] for _ in range(NS)]
        streams = []
        for s in range(NS):
            its = []
            for h in (s, s + 4):
                for q in range(2):
                    jmax = 8 * q + 7
                    for j in range(jmax, -1, -1):
                        its.append((h * 2 + q, h, q, j, jmax))
            streams.append(its)
        iters = []
        for h in range(NH):
            for q in range(2):
                for j in range(8 * q + 7, -1, -1):
                    iters.append((h * 2 + q, h, q, j, 8 * q + 7))

        def sb_info(q, j):
            r0 = max(0, j // 2 - 4 * q)
            ms = j // 2 - 4 * q
            return r0, (ms if ms >= 0 else None)

        def sb_parts(s, m):
            u, h, q, j, jmax = streams[s][m]
            r0, ms = sb_info(q, j)
            return u, h, q, j, jmax, r0 * 128, ms, jmax - j

        def sb_z(s, m):
            u, h, q, j, jmax, lo, ms, k = sb_parts(s, m)
            mm(bank(2 * s)[:, lo:512], KT[:, h, j * 128:(j + 1) * 128], QT[:, h, q * 512 + lo: (q + 1) * 512], True,
               ms is None, [("KT", h, j // 4), ("QT", h, q)], [("ps", 2 * s)])
            if ms is not None:
                mm(bank(2 * s)[:, ms * 128:(ms + 1) * 128], ident_b, mb_b[:, j % 2, :], False, True, ["mbb", "cstb"],
                   [("ps", 2 * s)])

        def sb_e(s, m):
            u, h, q, j, jmax, lo, ms, k = sb_parts(s, m)
            OP("act", "activation", [("ps", 2 * s)], [("e", s)], out=e_buf[s][:, lo:512], in_=bank(2 * s)[:, lo:512],
               func=AF.Exp, scale=QSCALE)

        def sb_sp(s, m):
            u, h, q, j, jmax, lo, ms, k = sb_parts(s, m)
            OP("act", "activation", [("e", s)], [("sp", s)], out=sp_buf[s][:, lo:512], in_=e_buf[s][:, lo:512],
               func=AF.Ln, bias=1.0, scale=1.0)

        def sb_t1(s, m):
            u, h, q, j, jmax, lo, ms, k = sb_parts(s, m)
            spb = sp_buf[s][:, lo:512]
            OP("dve", "scalar_tensor_tensor", [("ps", 2 * s), ("sp", s)], [("t1", s)],
               out=t1_buf[s][:, lo:512], in0=bank(2 * s)[:, lo:512], scalar=QSCALE, in1=spb, op0=ALU.mult, op1=ALU.subtract)
            ua = acc_buf[s]
            if j == jmax:
                for i2 in range(2):
                    OP("pool", "memset", [], [("acc", s, i2)], ap=ua[i2], constant=0.0)
            if j > 0:
                OP("pool", "tensor_tensor", [("acc", s, k % 2), ("sp", s)], [("acc", s, (k + 1) % 2)],
                   out=ua[(k + 1) % 2][:, lo:512], in0=ua[k % 2][:, lo:512], in1=spb, op=ALU.add)

        def sb_P(s, m):
            u, h, q, j, jmax, lo, ms, k = sb_parts(s, m)
            P = bank(2 * s)[:, lo:512]
            mm(P, triU_b, sp_buf[s][:, lo:512], True, False, [("sp", s), "cstb"], [("ps", 2 * s)])
            mm(P, ones_b, acc_buf[s][k % 2][:, lo:512], False, True, [("acc", s, k % 2), "cstb"], [("ps", 2 * s)])

        def sb_logw(s, m):
            u, h, q, j, jmax, lo, ms, k = sb_parts(s, m)
            t1b = t1_buf[s][:, lo:512]
            OP("dve", "tensor_tensor", [("t1", s), ("ps", 2 * s)], [("t1", s)], out=t1b, in0=t1b,
               in1=bank(2 * s)[:, lo:512], op=ALU.subtract)

        def sb_w(s, m):
            u, h, q, j, jmax, lo, ms, k = sb_parts(s, m)
            OP("act", "activation", [("t1", s)], [("w", s)], out=w_buf[s][:, lo:512], in_=t1_buf[s][:, lo:512],
               func=AF.Exp)

        def sb_av(s, m):
            u, h, q, j, jmax, lo, ms, k = sb_parts(s, m)
            Ob = 2 * s + 1
            mm(bank(Ob)[:, lo:512], V[:, j, h * 128:(h + 1) * 128], w_buf[s][:, lo:512], j == jmax, j == 0,
               [("V", j, h // 4), ("w", s)], [("ps", Ob)])
            if j == 0:
                evac_copy(mixT[:, h, q * 512:(q + 1) * 512], bank(Ob), [("ps", Ob)], [("mixT", h, q)])

        NIT = len(iters)
        nm = len(streams[0])
        grp = ((0, 1), (2, 3))
        for T in range(2 * nm + 1):
            F, mF = grp[T % 2], T // 2
            G, mG = grp[(T + 1) % 2], (T - 1) // 2
            for f1, f2 in ((sb_z, sb_P), (sb_e, sb_logw), (sb_sp, sb_w), (sb_t1, sb_av)):
                if mF < nm:
                    for s in F:
                        f1(s, mF)
                if T >= 1 and mG < nm:
                    for s in G:
                        f2(s, mG)

        if stage == "sb":
            return _finish_debug(nc, S, A, y, mixT, 8)

        S.barrier()
        A.release(m_phaseA)
        rKT = A.bf16(NH, SEQ)
        rV = A.bf16(16, 1024)
        rQT = A.bf16(NH, 1024)
        G = A.bf16(8, 1024)
        m_retattn = A.mark()
        xT = A.bf16(16, 1024)
        wbufs = [A.bf16(16, 512) for _ in range(2)]
        tab = mix_hi_f32[:, 0:2048].rearrange("p (a b) -> p a b", a=2)
        tA = [mix_hi_f32[:, 2048 + i * 512: 2048 + (i + 1) * 512] for i in range(2)]
        tB = [mix_hi_f32[:, 3072 + i * 512: 3072 + (i + 1) * 512] for i in range(2)]
        rr = [0]

        qb = [A.bf16(512) for _ in range(2)]

        def rot_proj(wb, wk, hh, xT, dst_fn, dkey_fn):
            for hf in range(2):
                bA, bB = next_banks(2)
                for k in range(16):
                    mm(bank(bA), wb[:, k, hh * 128:(hh + 1) * 128], xT[:, k, hf * 512:(hf + 1) * 512],
                       k == 0, k == 15, [wk, ("xT", k // 4)], [("ps", bA)])
                i = rr[0] % 2
                rr[0] += 1
                OP("act", "activation", [("ps", bA)], [("qb", i)], out=qb[i], in_=bank(bA), func=AF.Copy)
                mm(bank(bB), swp_b, qb[i], True, True, [("qb", i), "swp"], [("ps", bB)])
                cs, sn = tab[:, 0, hf * 512:(hf + 1) * 512], tab[:, 1, hf * 512:(hf + 1) * 512]
                OP("dve", "tensor_tensor", [("ps", bA), "tab", ("qb", i)], [("tA", i)], out=tA[i], in0=bank(bA), in1=cs,
                   op=ALU.mult)
                OP("dve", "tensor_tensor", [("ps", bB), "tab"], [("tB", i)], out=tB[i], in0=bank(bB), in1=sn, op=ALU.mult)
                OP("dve", "tensor_tensor", [("tA", i), ("tB", i)], [dkey_fn(hf)], out=dst_fn(hf), in0=tA[i], in1=tB[i],
                   op=ALU.add)

        def rk_task(p, pc):
            def fn(wb, wk):
                for hh in range(4):
                    h = pc * 4 + hh
                    rot_proj(wb, wk, hh, xT,
                             lambda hf: rKT[:, h, p * 1024 + hf * 512: p * 1024 + (hf + 1) * 512],
                             lambda hf: ("rKT", h, p * 2 + hf))
            return (p, w_ext[:, C_RKX + pc * 512: C_RKX + (pc + 1) * 512], fn)

        def rv_task(p, pc):
            def fn(wb, wk):
                for tb in range(8):
                    blk = p * 8 + tb
                    proj_token_major(wb, wk, xT, tb,
                                     lambda b, blk=blk: evac_copy(rV[:, blk, pc * 512:(pc + 1) * 512], bank(b),
                                                                  [("ps", b)], [("rV", blk, pc)]))
            return (p, w_ext[:, C_RV + pc * 512: C_RV + (pc + 1) * 512], fn)

        def rq_task(pc):
            def fn(wb, wk):
                for hh in range(4):
                    h = pc * 4 + hh
                    rot_proj(wb, wk, hh, xT,
                             lambda hf: rQT[:, h, hf * 512:(hf + 1) * 512],
                             lambda hf: ("rQT", h, hf))
            return (2, w_ext[:, C_RQX + pc * 512: C_RQX + (pc + 1) * 512], fn)

        def rg_task(pc):
            def fn(wb, wk):
                for tb in range(8):
                    def cons(b, tb=tb):
                        OP("act", "activation", [("ps", b)], [("G", tb, pc)],
                           out=G[:, tb, pc * 512:(pc + 1) * 512], in_=bank(b), func=AF.Silu)
                    proj_token_major(wb, wk, xT, tb, cons)
            return (2, w_ext[:, C_RG + pc * 512: C_RG + (pc + 1) * 512], fn)

        tasks = []
        for p in range(2):
            tasks += [rk_task(p, 0), rk_task(p, 1), rv_task(p, 0), rv_task(p, 1)]
        tasks += [rq_task(0), rq_task(1), rg_task(0), rg_task(1)]
        run_proj(tasks, xT, wbufs, "wr",
                 on_pass=lambda p: S.dma("sp", "tab", [], ["tab"], out=tab, in_=rot[p].rearrange("c d t -> d c t")))

        S.barrier()
        A.release(m_retattn)
        rm = A.f32(3, NH, 128)
        S.dma("sp", "rm", [], ["rm"], out=rm, in_=rmask.rearrange("p (a h c) -> p a h c", a=3, h=NH))
        gg = A.f32(1024)
        S.dma("sp", "gg", [], ["gg"], out=gg, in_=vecs[4, 0:1024].partition_broadcast(128))
        sc_buf = [A.bf16(512) for _ in range(2)]
        st6 = A.f32(4, 6)
        mv = A.f32(4, 2)
        rstd = A.f32(4)
        nmr = A.f32(4)
        nrm = [A.f32(128) for _ in range(2)]
        nrm2 = [A.f32(128) for _ in range(2)]
        rout = [A.bf16(128) for _ in range(2)]
        gam = [1.0 - 2.0 ** (-5.0 - h) for h in range(NH)]
        psT = ps[:, 4 * 512: 5 * 512].bitcast(BF16)

        def ret_s1(n):
            u, h, q, j, jmax = iters[n]
            r0, ms = sb_info(q, j)
            lo = r0 * 128
            sb_ = n % 2
            sT = bank(sb_)
            mm(sT[:, lo:512], rKT[:, h, j * 128:(j + 1) * 128], rQT[:, h, q * 512 + lo:(q + 1) * 512], True, True,
               [("rKT", h, j // 4), ("rQT", h, q)], [("ps", sb_)])
            for r in range(r0, 4):
                i_slot = 4 * q + r
                dstc = sc_buf[sb_][:, r * 128:(r + 1) * 128]
                src = sT[:, r * 128:(r + 1) * 128]
                if ms is not None and r == ms:
                    mk = rm[:, 1 if j % 2 == 0 else 2, h, :]
                    OP("dve", "tensor_tensor", [("ps", sb_), "rm"], [("sc", sb_)], out=dstc, in0=src, in1=mk, op=ALU.mult)
                else:
                    fac = gam[h] ** (128.0 * (2 * i_slot - j))
                    mk = rm[:, 0, h, :]
                    OP("dve", "scalar_tensor_tensor", [("ps", sb_), "rm"], [("sc", sb_)],
                       out=dstc, in0=src, scalar=fac, in1=mk, op0=ALU.mult, op1=ALU.mult)

        def ret_s2(n):
            u, h, q, j, jmax = iters[n]
            r0, ms = sb_info(q, j)
            sb_ = n % 2
            Ob = 2 + u % 2
            for r in range(r0, 4):
                first = (j == jmax)
                mm(bank(Ob)[:, r * 128:(r + 1) * 128], sc_buf[sb_][:, r * 128:(r + 1) * 128],
                   rV[:, j, h * 128:(h + 1) * 128], first, j == 0, [("sc", sb_), ("rV", j, h // 4)], [("ps", Ob)])
            if j == 0:
                for r in range(4):
                    o = bank(Ob)[:, r * 128:(r + 1) * 128]
                    OP("dve", "bn_stats", [("ps", Ob)], [("st6", r)], out=st6[:, r, :], in_=o)
                    OP("dve", "bn_aggr", [("st6", r)], [("mv",)], out=mv[:, r, :], in_=st6[:, r, :])
                OP("act", "activation", [("mv",)], [("rstd",)], out=rstd, in_=mv[:, :, 1], func=AF.Ln, bias=GN_EPS,
                   scale=1.0)
                OP("act", "activation", [("rstd",)], [("rstd",)], out=rstd, in_=rstd, func=AF.Exp, scale=-0.5)
                OP("dve", "scalar_tensor_tensor", [("mv",), ("rstd",)], [("nmr",)], out=nmr, in0=mv[:, :, 0], scalar=-1.0,
                   in1=rstd, op0=ALU.mult, op1=ALU.mult)
                for r in range(4):
                    i_slot = 4 * q + r
                    o = bank(Ob)[:, r * 128:(r + 1) * 128]
                    k2 = (u * 4 + r) % 2
                    OP("act", "activation", [("ps", Ob), ("nmr",), ("rstd",)], [("nrm", k2)], out=nrm[k2], in_=o,
                       func=AF.Identity, bias=nmr[:, r:r + 1], scale=rstd[:, r:r + 1])
                    OP("pool", "tensor_tensor", [("nrm", k2), "gg"], [("nrm2", k2)], out=nrm2[k2], in0=nrm[k2],
                       in1=gg[:, h * 128:(h + 1) * 128], op=ALU.mult)
                    OP("pool", "tensor_tensor", [("nrm2", k2), ("G", i_slot, h // 4)], [("rout", k2)], out=rout[k2],
                       in0=nrm2[k2], in1=G[:, i_slot, h * 128:(h + 1) * 128], op=ALU.mult)
                    OP("pe", "transpose", [("rout", k2), "cstb"], [("psT",)], out=psT[:, r * 128:(r + 1) * 128],
                       in_=rout[k2], identity=ident_b)
                evac_copy(mixT[:, 8 + h, q * 512:(q + 1) * 512], psT[:, 0:512], [("psT",)], [("mixT", 8 + h, q)])

        for step in range(NIT + 1):
            if step < NIT:
                ret_s1(step)
            if 0 <= step - 1 < NIT:
                ret_s2(step - 1)

        if stage == "attn":
            return _finish_debug(nc, S, A, y, mixT, 16)

        S.barrier()
        A.release(m_phaseA)
        yacc = A.f32(8, D)
        x1b = A.bf16(8, D)
        gates = A.f32(8, NE)
        posm = A.f32(8, NE)
        m_moe2 = A.mark()
        wob = [A.bf16(16, 512) for _ in range(2)]
        lng = A.f32(2, D)
        S.dma("sp", "lng", [], ["lng"], out=lng[:, 0, :], in_=vecs[0, :].partition_broadcast(128))
        S.dma("sp", "lng", [], ["lng"], out=lng[:, 1, :], in_=vecs[1, :].partition_broadcast(128))
        S.dma("sp", "xown", [], [("h1", s_) for s_ in range(8)], out=yacc, in_=x_own.rearrange("(s p) f -> p s f", p=128))
        for cp in range(4):
            wb, wk = load_piece(wob, w_out[:, cp * 512:(cp + 1) * 512], "wo")
            for s_ in range(8):
                (b,) = next_banks(1)
                for k in range(16):
                    mm(bank(b), mixT[:, k, s_ * 128:(s_ + 1) * 128], wb[:, k, :], k == 0, k == 15,
                       [wk, ("mixT", k, s_ // 4)], [("ps", b)])
                dst = yacc[:, s_, cp * 512:(cp + 1) * 512]
                OP("dve", "scalar_tensor_tensor", [("ps", b), ("h1", s_)], [("h1", s_)],
                   out=dst, in0=dst, scalar=ALPHA, in1=bank(b), op0=ALU.mult, op1=ALU.add)

        lst = A.f32(8, 24)
        lmv = A.f32(8, 2)
        lrs = A.f32(8)
        lnm = A.f32(8)

        def layer_norm(s_, lng, lst, lmv, lrs, lnm):
            xs = yacc[:, s_, :]
            for c in range(4):
                OP("dve", "bn_stats", [("h1", s_)], [("lst", s_)], out=lst[:, s_, c * 6:(c + 1) * 6],
                   in_=xs[:, c * 512:(c + 1) * 512])
            OP("dve", "bn_aggr", [("lst", s_)], [("lmv", s_)], out=lmv[:, s_, :], in_=lst[:, s_, :])
            OP("act", "activation", [("lmv", s_)], [("lrs", s_)], out=lrs[:, s_:s_ + 1], in_=lmv[:, s_, 1:2],
               func=AF.Ln, bias=LN_EPS, scale=1.0)
            OP("act", "activation", [("lrs", s_)], [("lrs", s_)], out=lrs[:, s_:s_ + 1], in_=lrs[:, s_:s_ + 1],
               func=AF.Exp, scale=-0.5)
            OP("dve", "scalar_tensor_tensor", [("lmv", s_), ("lrs", s_)], [("lnm", s_)], out=lnm[:, s_:s_ + 1],
               in0=lmv[:, s_, 0:1], scalar=-1.0, in1=lrs[:, s_:s_ + 1], op0=ALU.mult, op1=ALU.mult)
            OP("act", "activation", [("h1", s_), ("lnm", s_), ("lrs", s_)], [("h1", s_)], out=xs, in_=xs,
               func=AF.Identity, bias=lnm[:, s_:s_ + 1], scale=lrs[:, s_:s_ + 1])
            for hf_ in range(2):
                c0 = hf_ * 1024
                OP("dve", "tensor_tensor", [("h1", s_), "lng"], [("h1", s_, "n", hf_)], out=xs[:, c0:c0 + 1024],
                   in0=xs[:, c0:c0 + 1024], in1=lng[:, 0, c0:c0 + 1024], op=ALU.mult)
            for hf_ in range(2):
                c0 = hf_ * 1024
                OP("pool", "tensor_tensor", [("h1", s_, "n", hf_), "lng"], [("h1", s_)], out=xs[:, c0:c0 + 1024],
                   in0=xs[:, c0:c0 + 1024], in1=lng[:, 1, c0:c0 + 1024], op=ALU.add)

        def ln1_slot(s_):
            layer_norm(s_, lng, lst, lmv, lrs, lnm)
            OP("act", "activation", [("h1", s_)], [("x1b", s_)], out=x1b[:, s_, :], in_=yacc[:, s_, :], func=AF.Copy)

        if stage == "x1":
            for s_ in range(8):
                ln1_slot(s_)
            S.dma("sp", "yout", [("h1", s_) for s_ in range(8)], ["y"], out=y.rearrange("(s p) f -> p s f", p=128),
                  in_=yacc)
            S.wait_all("sp", ["y"])
            S.emit()
            return nc

        wr = A.f32(16, NE)
        br = A.f32(NE)
        onesrow = A.f32(128)
        bdn = A.f32(D)
        x1T = A.f32(16, 128)
        lg = A.f32(NE)
        mx8 = A.f32(8)
        msk = A.f32(8, NE)
        mskb = A.bf16(8, NE)
        msum = A.f32(NE)
        msumb = A.bf16(NE)
        ex = A.f32(NE)
        ssum = A.f32(1)
        nmx = A.f32(1)
        gT = A.f32(128)
        S.dma("sp", "wr", [], ["wr"], out=wr, in_=w_router.rearrange("(k q) n -> q k n", q=128))
        S.dma("sp", "br", [], ["br"], out=br[0:1, :], in_=b_router)
        S.dma("sp", "bdn", [], ["bdn"], out=bdn[0:NE, :], in_=b_dn)
        OP("dve", "memset", [], ["onesrow"], ap=onesrow[0:1, :], constant=1.0)
        OP("dve", "memset", [], ["msum"], ap=msum, constant=0.0)
        def router_slot(s_):
            for g4 in range(4):
                for c in range(4):
                    k = g4 * 4 + c
                    OP("pe", "transpose", [("h1", s_), "cst"], [("ps", g4)], out=bank(g4)[:, c * 128:(c + 1) * 128],
                       in_=yacc[:, s_, k * 128:(k + 1) * 128], identity=ident_f)
                evac_copy(x1T[:, g4 * 4:(g4 + 1) * 4, :], bank(g4).rearrange("p (a b) -> p a b", a=4),
                          [("ps", g4)], [("x1T", g4)])
            lgp = bank(4)[:, 0:NE]
            for k in range(16):
                mm(lgp, x1T[:, k, :], wr[:, k, :], k == 0, False, [("x1T", k // 4), "wr"], [("ps", 4)])
            mm(lgp, onesrow[0:1, :], br[0:1, :], False, True, ["onesrow", "br"], [("ps", 4)])
            OP("dve", "tensor_copy", [("ps", 4)], ["lg"], out=lg, in_=lgp)
            OP("dve", "max", ["lg"], ["mx8"], out=mx8, in_=lg)
            OP("dve", "tensor_scalar", ["lg", "mx8"], [("msk", s_)], out=msk[:, s_, :], in0=lg, scalar1=mx8[:, 3:4],
               scalar2=None, op0=ALU.is_ge)
            OP("dve", "tensor_scalar", ["mx8"], ["nmx"], out=nmx, in0=mx8[:, 0:1], scalar1=-1.0, scalar2=None, op0=ALU.mult)
            OP("act", "activation", ["lg", "nmx"], ["ex"], out=ex, in_=lg, func=AF.Exp, bias=nmx, scale=1.0)
            OP("dve", "tensor_tensor", ["ex", ("msk", s_)], ["ex"], out=ex, in0=ex, in1=msk[:, s_, :], op=ALU.mult)
            OP("dve", "reduce_sum", ["ex"], ["ssum"], out=ssum, in_=ex, axis=mybir.AxisListType.X)
            OP("dve", "reciprocal", ["ssum"], ["ssum"], out=ssum, in_=ssum)
            OP("dve", "tensor_scalar", ["ex", "ssum"], [("gates", s_)], out=gates[:, s_, :], in0=ex, scalar1=ssum[:, 0:1],
               scalar2=None, op0=ALU.mult)
            OP("dve", "tensor_copy", [("msk", s_)], [("mskb", s_)], out=mskb[:, s_, :], in_=msk[:, s_, :])
            OP("dve", "tensor_copy", ["msum"], ["msumb"], out=msumb, in_=msum)
            pp = bank(5)[:, 0:NE]
            mm(pp, triL_b, mskb[:, s_, :], True, False, [("mskb", s_), "cstb"], [("ps", 5)])
            mm(pp, ones_b, msumb, False, True, ["msumb", "cstb"], [("ps", 5)])
            OP("dve", "scalar_tensor_tensor", [("ps", 5), ("msk", s_)], [("posm", s_)], out=posm[:, s_, :], in0=pp,
               scalar=1.0, in1=msk[:, s_, :], op0=ALU.add, op1=ALU.mult)
            OP("dve", "tensor_scalar", [("posm", s_)], [("posm", s_)], out=posm[:, s_, :], in0=posm[:, s_, :],
               scalar1=-1.0, scalar2=None, op0=ALU.add)
            OP("dve", "tensor_tensor", ["msum", ("msk", s_)], ["msum"], out=msum, in0=msum, in1=msk[:, s_, :], op=ALU.add)
            OP("pe", "transpose", [("gates", s_), "cst"], [("ps", 6)], out=bank(6)[0:NE, 0:128], in_=gates[:, s_, :],
               identity=ident_f)
            OP("act", "activation", [("ps", 6)], ["gT"], out=gT[0:NE, :], in_=bank(6)[0:NE, 0:128], func=AF.Copy)
            for cp in range(4):
                bb = bank(7)
                mm(bb, gT[0:NE, :], bdn[0:NE, cp * 512:(cp + 1) * 512], True, True, ["gT", "bdn"], [("ps", 7)])
                dst = yacc[:, s_, cp * 512:(cp + 1) * 512]
                OP("dve", "scalar_tensor_tensor", [("ps", 7), ("h1", s_)], [("h1", s_)], out=dst, in0=dst, scalar=ALPHA,
                   in1=bb, op0=ALU.mult, op1=ALU.add)

        ln1_slot(0)
        for s_ in range(8):
            if s_ + 1 < 8:
                ln1_slot(s_ + 1)
            router_slot(s_)

        if stage == "router":
            S.barrier()
            yv = y.rearrange("(s p) f -> p s f", p=128)
            S.dma("sp", "yout", [], ["y"], out=yv[:, :, 0:NE], in_=gates)
            S.dma("sp", "yout", [], ["y"], out=yv[:, :, NE:2 * NE], in_=posm)
            S.dma("sp", "yout", [], ["y"], out=yv[:, :, 64:64 + 1024], in_=yacc[:, :, 0:1024])
            S.wait_all("sp", ["y"])
            S.emit()
            return nc

        S.barrier()
        A.release(m_moe2)
        wmb = [mix_raw[:, i * 4096:(i + 1) * 4096].bitcast(BF16).rearrange("p (a b) -> p a b", a=16) for i in range(2)]
        wmb.append(A.bf16(16, 512))
        sel = [A.bf16(8, CAP) for _ in range(2)]
        selT = [A.bf16(2, 1024) for _ in range(2)]
        XeT = [A.bf16(16, CAP) for _ in range(2)]
        actT = A.bf16(16, CAP)
        gsb = A.f32(4, CAP)
        utmp = [A.f32(CAP) for _ in range(2)]
        gtmp = utmp
        yrows = A.bf16(2, D)
        bgu = [A.f32(32) for _ in range(2)]
        psT7 = ps[:, 7 * 512: 8 * 512].bitcast(BF16)

        hb_n = [0]
        sc_n = [0]
        bgu1 = [A.f32(16) for _ in range(2)]

        def moe_front(ex_):
            eb = ex_ % 2
            S.dma("sp", f"bgu{eb}", [], [("bgu", eb)], out=bgu[eb], in_=b_gu[ex_])
            OP("dve", "tensor_scalar", [("bgu", eb)], [("bgu1", eb)], out=bgu1[eb], in0=bgu[eb][:, 16:32], scalar1=1.0,
               scalar2=None, op0=ALU.add)
            for s_ in range(8):
                OP("dve", "tensor_scalar", [("posm", s_), "cst"], [("sel", eb, s_)], out=sel[eb][:, s_, :], in0=iota_f[:, 0:CAP],
                   scalar1=posm[:, s_, ex_:ex_ + 1], scalar2=None, op0=ALU.is_equal)
            for rc, (r0_, rn) in enumerate(RCH):
                for s_ in range(8):
                    OP("pe", "transpose", [("sel", eb, s_), "cstb"], [("ps", 7)], out=psT7[0:rn, s_ * 128:(s_ + 1) * 128],
                       in_=sel[eb][:, s_, r0_:r0_ + rn], identity=ident_b)
                evac_copy(selT[eb][0:rn, rc, :], psT7[0:rn, :], [("ps", 7)], [("selT", eb, rc)], eng="act")
            for c in range(16):
                gb = (0, 7, 3, 4, 5, 6)[c % 6]
                for s_ in range(8):
                    mm(bank(gb, CAP), x1b[:, s_, c * 128:(c + 1) * 128], sel[eb][:, s_, :], s_ == 0, s_ == 7,
                       [("x1b", s_), ("sel", eb, s_)], [("ps", gb)])
                evac_copy(XeT[eb][:, c, :], bank(gb, CAP), [("ps", gb)], [("XeT", eb, c)], eng="act")

        def moe_hidden(ex_):
            eb = ex_ % 2
            for p4 in range(4):
                for gu in range(2):
                    wb, wk = load_piece(wmb, w_gu[ex_, p4 * 2 + gu], "wm", tiled=True)
                    for cc in range(4):
                        hbi = hb_n[0] % 2
                        hb_n[0] += 1
                        hbk = ("ps", 1 + hbi)
                        hp_ = bank(1 + hbi, CAP)
                        for k in range(16):
                            mm(hp_, wb[:, k, cc * 128:(cc + 1) * 128], XeT[eb][:, k, :], k == 0, k == 15,
                               [wk, ("XeT", eb, k)], [hbk])
                        fch = p4 * 4 + cc
                        if gu == 0:
                            OP("dve", "tensor_scalar", [hbk, ("bgu", eb)], [("gtmp", hbi)], out=gtmp[hbi], in0=hp_,
                               scalar1=bgu[eb][:, fch:fch + 1], scalar2=7.0, op0=ALU.add, op1=ALU.min)
                            OP("act", "activation", [("gtmp", hbi)], [("gsb", cc)], out=gsb[:, cc, :], in_=gtmp[hbi],
                               func=AF.Gelu_apprx_sigmoid)
                        else:
                            OP("dve", "tensor_scalar", [hbk, ("bgu1", eb)], [("utmp", hbi)], out=utmp[hbi], in0=hp_,
                               scalar1=bgu1[eb][:, fch:fch + 1], scalar2=8.0, op0=ALU.add, op1=ALU.min)
                            OP("dve", "scalar_tensor_tensor", [("utmp", hbi), ("gsb", cc)], [("actT", fch)],
                               out=actT[:, fch, :], in0=utmp[hbi], scalar=-6.0, in1=gsb[:, cc, :], op0=ALU.max, op1=ALU.mult)

        def moe_down(ex_):
            eb = ex_ % 2
            for cp in range(4):
                wb, wk = load_piece(wmb, w_dn[ex_, cp], "wm", tiled=True)
                for rc, (r0_, rn) in enumerate(RCH):
                    yb = 3 + rc
                    for k in range(16):
                        mm(bank(yb)[0:rn, :], actT[:, k, r0_:r0_ + rn], wb[:, k, :], k == 0, k == 15,
                           [wk, ("actT", k)], [("ps", yb)])
                    evac_copy(yrows[0:rn, rc, cp * 512:(cp + 1) * 512], bank(yb)[0:rn, :], [("ps", yb)],
                              [("yrows", rc, cp)], eng="act")
                for s_ in range(8):
                    sb_ = (5, 6, 0, 7)[sc_n[0] % 4]
                    sc_n[0] += 1
                    for rc, (r0_, rn) in enumerate(RCH):
                        mm(bank(sb_), selT[eb][0:rn, rc, s_ * 128:(s_ + 1) * 128], yrows[0:rn, rc, cp * 512:(cp + 1) * 512],
                           rc == 0, rc == 1, [("selT", eb, rc), ("yrows", rc, cp)], [("ps", sb_)])
                    dst = yacc[:, s_, cp * 512:(cp + 1) * 512]
                    OP("dve", "scalar_tensor_tensor", [("ps", sb_), ("gates", s_), ("h1", s_, cp)], [("h1", s_, cp)],
                       out=dst, in0=bank(sb_), scalar=gates[:, s_, ex_:ex_ + 1], in1=dst, op0=ALU.mult, op1=ALU.add)

        moe_front(0)
        for ex_ in range(ne_run):
            moe_hidden(ex_)
            if ex_ + 1 < ne_run:
                moe_front(ex_ + 1)
            moe_down(ex_)

        S.barrier()
        A.release(m_moe2)
        lng2 = A.f32(2, D)
        lst2 = A.f32(8, 24)
        lmv2 = A.f32(8, 2)
        lrs2 = A.f32(8)
        lnm2 = A.f32(8)
        S.dma("sp", "lng2", [], ["lng"], out=lng2[:, 0, :], in_=vecs[2, :].partition_broadcast(128))
        S.dma("sp", "lng2", [], ["lng"], out=lng2[:, 1, :], in_=vecs[3, :].partition_broadcast(128))
        yv = y.rearrange("(s p) f -> p s f", p=128)
        for s_ in range(8):
            layer_norm(s_, lng2, lst2, lmv2, lrs2, lnm2)
            S.dma("sp", "yout", [("h1", s_)], [("y", s_)], out=yv[:, s_, :], in_=yacc[:, s_, :])
        S.wait_all("sp", [("y", s_) for s_ in range(8)])
        S.emit()
    return nc


def _finish_debug(nc, S, A, y, mixT, nchunks):
    S.barrier()
    tmp = [A.f32(1024) for _ in range(2)]
    yv = y.rearrange("r (two t) -> (r two) t", two=2).rearrange("(c p) t -> p c t", p=128)
    for c in range(nchunks):
        S.op("dve", "tensor_copy", [], [("tmp", c % 2)], out=tmp[c % 2], in_=mixT[:, c, :])
        S.dma("sp", "dbg", [("tmp", c % 2)], ["ydbg"], out=yv[:, c, :], in_=tmp[c % 2])
    S.wait_all("sp", ["ydbg"])
    S.emit()
    return nc


def _constants(hp):
    p = np.arange(128)
    ident = np.eye(128, dtype=np.float32)
    triU = (p[:, None] > p[None, :]).astype(np.float32)
    triL = (p[:, None] < p[None, :]).astype(np.float32)
    ones = np.ones((128, 128), np.float32)
    strict = (p[:, None] < p[None, :]).astype(np.float32)
    if hp == 0:
        mA, mB = strict, np.zeros((128, 128), np.float32)
    else:
        mA, mB = ones, strict
    iota = np.broadcast_to(np.arange(256, dtype=np.float32)[None, :], (128, 256))
    cst = np.concatenate([ident, triU, triL, ones, mA, mB, iota], axis=1).astype(np.float32)
    sl = p[:, None].astype(np.float64)
    tl = p[None, :].astype(np.float64)
    rm = np.zeros((128, 3, NH, 128), np.float64)
    for h in range(NH):
        lg = math.log1p(-2.0 ** (-5.0 - h))
        gen = np.exp(lg * (128.0 * hp + tl - sl))
        diag = np.where((p[:, None] // 64) <= (p[None, :] // 64), np.exp(lg * np.abs(tl - sl)), 0.0)
        rm[:, 0, h, :] = gen
        if hp == 0:
            rm[:, 1, h, :] = diag
            rm[:, 2, h, :] = 0.0
        else:
            rm[:, 1, h, :] = np.exp(lg * (128.0 + tl - sl))
            rm[:, 2, h, :] = diag
    rm *= HD ** -0.5
    return cst, rm.reshape(128, 3 * NH * 128).astype(np.float32)


def _rot_tables(hp):
    inv = 10000.0 ** (-np.arange(0, HD, 2, dtype=np.float32) / HD)
    own = np.concatenate([np.arange(128) + (2 * i + hp) * 128 for i in range(8)])
    out = np.zeros((3, 2, 128, 1024), np.float32)
    for pi, pos in enumerate([np.arange(0, 1024), np.arange(1024, 2048), own]):
        ang = pos.astype(np.float32)[None, :] * inv[:, None]
        c, s = np.cos(ang), np.sin(ang)
        out[pi, 0] = np.concatenate([c, c], axis=0)
        out[pi, 1] = np.concatenate([-s, s], axis=0)
    return out


def _w_ext(w_in):
    return np.ascontiguousarray(w_in)


_PROG = {}


def _get_prog(stage="full", ne_run=NE):
    if (stage, ne_run) not in _PROG:
        _PROG[(stage, ne_run)] = build_program(stage, ne_run)
    return _PROG[(stage, ne_run)]


def make_in_maps(x, w_in, ret_gn_gain, w_out, ln1_gain, ln1_bias, w_router, b_router,
                 w_gate_up, b_gate_up, w_down, b_down, ln2_gain, ln2_bias, stage="full", ne_run=NE):
    f = lambda a: np.ascontiguousarray(np.asarray(a, dtype=np.float32))
    x = f(x)
    wext = _w_ext(f(w_in)[0])
    vecs = np.zeros((5, D), np.float32)
    vecs[0], vecs[1], vecs[2], vecs[3] = f(ln1_gain)[0], f(ln1_bias)[0], f(ln2_gain)[0], f(ln2_bias)[0]
    vecs[4, :1024] = f(ret_gn_gain)[0]
    shared = {"w_ext": wext, "w_out": f(w_out)[0], "vecs": vecs}
    if stage in ("full", "router"):
        shared.update({
            "w_router": f(w_router)[0], "b_router": f(b_router)[0].reshape(1, NE), "b_dn": f(b_down)[0],
            "b_gu": np.ascontiguousarray(f(b_gate_up)[0].reshape(NE, 32, 128).transpose(0, 2, 1)),
        })
    if stage == "full":
        wg = f(w_gate_up)[0][:ne_run].reshape(ne_run, 16, 128, 2, 4, 512)
        wd = f(w_down)[0][:ne_run].reshape(ne_run, 16, 128, 4, 512)
        shared.update({
            "w_gu": np.ascontiguousarray(wg.transpose(0, 4, 3, 2, 1, 5)).reshape(ne_run, 8, 128, 16, 512),
            "w_dn": np.ascontiguousarray(wd.transpose(0, 3, 2, 1, 4)),
        })
    consts = [(_constants(hp), _rot_tables(hp)) for hp in range(2)]
    in_maps = []
    for c in range(8):
        b, hp = c // 2, c % 2
        xb = x[b]
        own = np.concatenate([np.arange(128) + (2 * i + hp) * 128 for i in range(8)])
        xo = np.ascontiguousarray(xb[own])
        xT3 = np.stack([np.ascontiguousarray(xb[0:1024].T), np.ascontiguousarray(xb[1024:2048].T),
                        np.ascontiguousarray(xo.T)])
        (cst, rm), rot = consts[hp]
        m = dict(shared)
        m.update({"xT3": xT3, "x_own": xo, "cst": cst, "rmask": rm, "rot": rot})
        in_maps.append(m)
    return in_maps


def kernel(x, w_in, ret_gn_gain, w_out, ln1_gain, ln1_bias, w_router, b_router,
           w_gate_up, b_gate_up, w_down, b_down, ln2_gain, ln2_bias):
    in_maps = make_in_maps(x, w_in, ret_gn_gain, w_out, ln1_gain, ln1_bias, w_router, b_router,
                           w_gate_up, b_gate_up, w_down, b_down, ln2_gain, ln2_bias)
    nc = _get_prog("full")
    res = run_bass_kernel_spmd(nc, in_maps, core_ids=list(range(8)))
    out = np.zeros((NB, SEQ, D), np.float32)
    for c in range(8):
        b, hp = c // 2, c % 2
        yc = res.results[c]["y"]
        for i in range(8):
            blk = 2 * i + hp
            out[b, blk * 128:(blk + 1) * 128] = yc[i * 128:(i + 1) * 128]
    return out
```
